# Optimizing a Trainium2 kernel written in Bass

```python
import math
import jax, jax.numpy as jnp
from jax import lax
import numpy as np

D_MODEL = 1024
BATCH = 2
SEQ = 8192
DEPTH = 1

CHUNK = 64
EPS = 1e-6
A_HEAD_DIM = 128
A_WIDTH = D_MODEL // 2
A_HEADS = A_WIDTH // A_HEAD_DIM
CONV_K = 4
B_HEAD_DIM = 64
B_WIDTH = D_MODEL - A_WIDTH
B_HEADS = B_WIDTH // B_HEAD_DIM
B_PREV_CHUNKS = 8
B_BAND = (B_PREV_CHUNKS + 1) * CHUNK
REL_CLIP = 128
MIX_WIDTH = A_WIDTH + B_WIDTH
OFF_A_QKV = 0
OFF_A_Z = 3 * A_WIDTH
OFF_A_ALPHA = 4 * A_WIDTH
OFF_A_BETA = OFF_A_ALPHA + A_HEADS
OFF_B_QKV = OFF_A_BETA + A_HEADS
IN_COLS = OFF_B_QKV + 3 * B_WIDTH
N_EXPERTS = 32
TOP_K = 4
D_EXPERT = D_MODEL
SWIGLU_ALPHA = 1.702
SWIGLU_LIMIT = 7.0
MOE_BLOCK = 256

kernel_name = 'hybrid_deltanet_chunkattn_moe_block'


def rms_norm(x, w):
    xf = x.astype(jnp.float32)
    y = xf * lax.rsqrt(jnp.mean(xf * xf, axis=-1, keepdims=True) + EPS)
    return (y * w.astype(jnp.float32)).astype(x.dtype)


def l2_norm(x):
    xf = x.astype(jnp.float32)
    return xf * lax.rsqrt(jnp.sum(xf * xf, axis=-1, keepdims=True) + EPS)


def causal_depthwise_conv(x, w):
    ch = x.shape[-1]
    return lax.conv_general_dilated(x, w[:, None, :].astype(x.dtype), window_strides=(1,),
                                    padding=[(CONV_K - 1, 0)],
                                    dimension_numbers=('NWC', 'WIO', 'NWC'),
                                    feature_group_count=ch)


def gated_delta_rule(q, k, v, g, beta):
    bsz, t_len, n_h, dk = q.shape
    dv = v.shape[-1]
    n_ch = t_len // CHUNK

    def to_chunks(t):
        t = t.astype(jnp.float32).reshape((bsz, n_ch, CHUNK, n_h) + t.shape[3:])
        return jnp.moveaxis(t, 3, 1)

    q = to_chunks(q) * (dk ** -0.5)
    k = to_chunks(k)
    v = to_chunks(v)
    beta = to_chunks(beta)
    g = jnp.cumsum(to_chunks(g), axis=-1)
    causal = jnp.tril(jnp.ones((CHUNK, CHUNK), dtype=bool))
    decay = jnp.exp(jnp.where(causal, g[..., :, None] - g[..., None, :], -jnp.inf))
    k_beta = k * beta[..., None]
    strict = jnp.tril(jnp.ones((CHUNK, CHUNK), jnp.float32), -1)
    lower = jnp.einsum('bhnid,bhnjd->bhnij', k_beta, k) * decay * strict
    eye = jnp.eye(CHUNK, dtype=jnp.float32)
    tmat = lax.linalg.triangular_solve(lower + eye, jnp.broadcast_to(eye, lower.shape),
                                       left_side=True, lower=True, unit_diagonal=True)
    u = tmat @ (v * beta[..., None])
    w = tmat @ (k_beta * jnp.exp(g)[..., None])
    attn = jnp.einsum('bhnid,bhnjd->bhnij', q, k) * decay
    q_dec = q * jnp.exp(g)[..., None]
    g_last = g[..., -1]
    k_dec = k * jnp.exp(g_last[..., None] - g)[..., None]

    def step(state, inp):
        u_n, w_n, attn_n, q_n, k_n, gl_n = inp
        v_new = u_n - w_n @ state
        out = q_n @ state + attn_n @ v_new
        state = state * jnp.exp(gl_n)[..., None, None] + jnp.swapaxes(k_n, -1, -2) @ v_new
        return state, out

    xs = tuple(jnp.moveaxis(t, 2, 0) for t in (u, w, attn, q_dec, k_dec, g_last))
    state0 = jnp.zeros((bsz, n_h, dk, dv), jnp.float32)
    _, out = lax.scan(step, state0, xs)
    return out.transpose(1, 0, 3, 2, 4).reshape(bsz, t_len, n_h, dv)


def chunk_band_attention(q, k, v, rel_bias):
    bsz, t_len, n_h, hd = q.shape
    n_ch = t_len // CHUNK
    qc = q.reshape(bsz, n_ch, CHUNK, n_h, hd)

    def band(t):
        t = t.reshape(bsz, n_ch, CHUNK, n_h, hd)
        t = jnp.pad(t, ((0, 0), (B_PREV_CHUNKS, 0), (0, 0), (0, 0), (0, 0)))
        return jnp.concatenate([t[:, s:s + n_ch] for s in range(B_PREV_CHUNKS + 1)], axis=2)

    kb, vb = band(k), band(v)
    q_off = jnp.arange(CHUNK)[:, None] + B_PREV_CHUNKS * CHUNK
    k_off = jnp.arange(B_BAND)[None, :]
    idx = jnp.clip(q_off - k_off, -REL_CLIP, REL_CLIP) + REL_CLIP
    bias = rel_bias[:, idx].astype(jnp.float32)
    valid = (jnp.arange(n_ch)[:, None] - B_PREV_CHUNKS + k_off // CHUNK) >= 0
    s = jnp.einsum('bnqhd,bnkhd->bnhqk', qc, kb).astype(jnp.float32) * (hd ** -0.5) + bias
    s = jnp.where(valid[None, :, None, None, :], s, -jnp.inf)
    p = jax.nn.softmax(s, axis=-1).astype(v.dtype)
    o = jnp.einsum('bnhqk,bnkhd->bnqhd', p, vb)
    return o.reshape(bsz, t_len, n_h * hd)


def hybrid_mixer(h, w_in, conv_w, a_log, dt_bias, a_norm_w, rel_bias, w_out):
    bsz, t_len, _ = h.shape
    proj = h @ w_in
    qkv_a = jax.nn.silu(causal_depthwise_conv(proj[..., OFF_A_QKV:OFF_A_Z], conv_w))
    q_a, k_a, v_a = [t.reshape(bsz, t_len, A_HEADS, A_HEAD_DIM) for t in jnp.split(qkv_a, 3, axis=-1)]
    z_a = proj[..., OFF_A_Z:OFF_A_ALPHA].reshape(bsz, t_len, A_HEADS, A_HEAD_DIM)
    g = -jnp.exp(a_log.astype(jnp.float32)) * jax.nn.softplus(
        proj[..., OFF_A_ALPHA:OFF_A_BETA].astype(jnp.float32) + dt_bias.astype(jnp.float32))
    beta = jax.nn.sigmoid(proj[..., OFF_A_BETA:OFF_B_QKV].astype(jnp.float32))
    o_a = gated_delta_rule(l2_norm(q_a), l2_norm(k_a), v_a, g, beta)
    o_a = (rms_norm(o_a, a_norm_w) * jax.nn.silu(z_a.astype(jnp.float32))).astype(h.dtype)
    o_a = o_a.reshape(bsz, t_len, A_WIDTH)
    q_b, k_b, v_b = [t.reshape(bsz, t_len, B_HEADS, B_HEAD_DIM)
                     for t in jnp.split(proj[..., OFF_B_QKV:], 3, axis=-1)]
    o_b = chunk_band_attention(q_b, k_b, v_b, rel_bias).astype(h.dtype)
    return jnp.concatenate([o_a, o_b], axis=-1) @ w_out


def moe_ffn(h, w_router, b_router, w_up, b_up, w_down, b_down):
    bsz, t_len, d = h.shape
    x = h.reshape(-1, d)
    n_tok = x.shape[0]
    logits = (x @ w_router + b_router).astype(jnp.float32)
    top_val, top_idx = lax.top_k(logits, TOP_K)
    gates = jax.nn.softmax(top_val, axis=-1)
    n_assign = n_tok * TOP_K
    flat_e = top_idx.reshape(-1)
    flat_tok = jnp.repeat(jnp.arange(n_tok, dtype=jnp.int32), TOP_K)
    flat_g = gates.reshape(-1)
    order = jnp.argsort(flat_e, stable=True)
    sorted_e = flat_e[order]
    counts = jnp.bincount(flat_e, length=N_EXPERTS)
    padded = (counts + MOE_BLOCK - 1) // MOE_BLOCK * MOE_BLOCK
    start = jnp.cumsum(counts) - counts
    pend = jnp.cumsum(padded)
    pstart = pend - padded
    dest = pstart[sorted_e] + (jnp.arange(n_assign) - start[sorted_e])
    n_blocks = -(-n_assign // MOE_BLOCK) + N_EXPERTS
    cap = n_blocks * MOE_BLOCK
    slot_tok = jnp.zeros((cap,), jnp.int32).at[dest].set(flat_tok[order])
    slot_g = jnp.zeros((cap,), jnp.float32).at[dest].set(flat_g[order])
    block_e = jnp.minimum(jnp.searchsorted(pend, jnp.arange(n_blocks) * MOE_BLOCK, side='right'),
                          N_EXPERTS - 1)
    xb = x[slot_tok].reshape(n_blocks, MOE_BLOCK, d)

    def expert_block(args):
        xe, e = args
        hu = xe @ w_up[e] + b_up[e]
        glu = jnp.minimum(hu[:, :D_EXPERT], SWIGLU_LIMIT)
        lin = jnp.clip(hu[:, D_EXPERT:], -SWIGLU_LIMIT, SWIGLU_LIMIT)
        act = glu * jax.nn.sigmoid(SWIGLU_ALPHA * glu) * (lin + 1)
        return act @ w_down[e] + b_down[e]

    yb = lax.map(expert_block, (xb, block_e))
    y = yb.reshape(cap, d) * slot_g[:, None].astype(yb.dtype)
    out = jnp.zeros((n_tok, d), y.dtype).at[slot_tok].add(y)
    return out.reshape(bsz, t_len, d)


def setup_inputs(seed: int = 0) -> dict:
    key = jax.random.key(seed)
    ks = jax.random.split(key, 18)
    f32 = jnp.float32
    L, D, E, F = DEPTH, D_MODEL, N_EXPERTS, D_EXPERT

    def nrm(k, shape, s):
        return jax.random.normal(k, shape, f32) * s

    dt = jnp.exp(jax.random.uniform(ks[8], (L, A_HEADS), f32, math.log(1e-3), math.log(1e-1)))
    return {
        'x': nrm(ks[0], (BATCH, SEQ, D), 1.0),
        'c': nrm(ks[1], (BATCH, D), 1.0),
        'w_ada': nrm(ks[2], (L, D, 6 * D), 0.5 * D ** -0.5),
        'b_ada': nrm(ks[3], (L, 6 * D), 0.02),
        'norm_w': 1.0 + nrm(ks[4], (L, 4, D), 0.02),
        'w_in': nrm(ks[5], (L, D, IN_COLS), D ** -0.5),
        'conv_w': nrm(ks[6], (L, CONV_K, 3 * A_WIDTH), CONV_K ** -0.5),
        'a_log': jnp.log(jax.random.uniform(ks[7], (L, A_HEADS), f32, 1.0, 16.0)),
        'dt_bias': dt + jnp.log(-jnp.expm1(-dt)),
        'a_norm_w': 1.0 + nrm(ks[9], (L, A_HEAD_DIM), 0.02),
        'rel_bias': nrm(ks[10], (L, B_HEADS, 2 * REL_CLIP + 1), 0.1),
        'w_out': nrm(ks[11], (L, MIX_WIDTH, D), MIX_WIDTH ** -0.5),
        'w_router': nrm(ks[12], (L, D, E), D ** -0.5),
        'b_router': nrm(ks[13], (L, E), 0.01),
        'w_up': nrm(ks[14], (L, E, D, 2 * F), D ** -0.5),
        'b_up': nrm(ks[15], (L, E, 2 * F), 0.01),
        'w_down': nrm(ks[16], (L, E, F, D), F ** -0.5),
        'b_down': nrm(ks[17], (L, E, D), 0.01),
    }


def reference(x, c, w_ada, b_ada, norm_w, w_in, conv_w, a_log, dt_bias, a_norm_w, rel_bias, w_out,
              w_router, b_router, w_up, b_up, w_down, b_down):
    for l in range(DEPTH):
        mod = jax.nn.silu(c) @ w_ada[l] + b_ada[l]
        sh1, sc1, ga1, sh2, sc2, ga2 = [m[:, None, :] for m in jnp.split(mod, 6, axis=-1)]
        h = rms_norm(x, norm_w[l, 0]) * (1 + sc1) + sh1
        y = hybrid_mixer(h, w_in[l], conv_w[l], a_log[l], dt_bias[l], a_norm_w[l], rel_bias[l], w_out[l])
        x = x + ga1 * rms_norm(y, norm_w[l, 1])
        h = rms_norm(x, norm_w[l, 2]) * (1 + sc2) + sh2
        y = moe_ffn(h, w_router[l], b_router[l], w_up[l], b_up[l], w_down[l], b_down[l])
        x = x + ga2 * rms_norm(y, norm_w[l, 3])
    return x
```

```python
import numpy as np
from contextlib import ExitStack
import concourse.bass as bass
import concourse.mybir as mybir
from concourse.bass_utils import run_bass_kernel_spmd

F32 = mybir.dt.float32
BF16 = mybir.dt.bfloat16
AF = mybir.ActivationFunctionType
ALU = mybir.AluOpType
AX = mybir.AxisListType

ENGS = ("pe", "act", "dve", "pool", "sp")
DS = 1
NEG = -30000.0
EPS = 1e-6


class Prog:
    def __init__(self, nc, n_dma_sems=6):
        self.nc = nc
        self.ops = []
        self.last_w = {}
        self.readers = {}
        self.n_dma_sems = n_dma_sems

    def add(self, eng, fn, r=(), w=(), dma=False, after_all=False):
        banks = set()
        for k in list(r) + list(w):
            if k[0] == "q" and "_" in k and k[1:k.index("_")].isdigit():
                banks.add("BK" + k[1:k.index("_")])
        deps = set()
        for k in r:
            if k in self.last_w:
                deps.add(self.last_w[k])
        for k in w:
            if k in self.last_w:
                deps.add(self.last_w[k])
            deps.update(self.readers.get(k, ()))
        tdeps = set()
        for k in banks:
            if k in self.last_w and self.last_w[k] not in deps:
                tdeps.add(self.last_w[k])
        deps |= tdeps
        w = list(w) + sorted(banks)
        idx = len(self.ops)
        if after_all:
            deps = set(range(idx))
        self.ops.append(dict(eng=eng, fn=fn, deps=deps, dma=dma, sig=False, tdeps=tdeps))
        for k in r:
            self.readers.setdefault(k, []).append(idx)
        for k in w:
            self.last_w[k] = idx
            self.readers[k] = []
        return idx

    def emit(self, stack, final_wait_keys=()):
        nc = self.nc
        ops = self.ops
        final_deps = set()
        for k in final_wait_keys:
            if k in self.last_w:
                final_deps.add(self.last_w[k])
        for o in ops:
            for d in o["deps"]:
                ops[d]["sig"] = True
        for d in final_deps:
            ops[d]["sig"] = True
        esem = {e: stack.enter_context(nc.semaphore("s_" + e)) for e in ENGS}
        dsem = {e: [stack.enter_context(nc.semaphore("d_%s%d" % (e, i))) for i in range(self.n_dma_sems)]
                for e in ENGS if e != "pe"}
        ecount = {e: 0 for e in ENGS}
        dcount = {e: [0] * self.n_dma_sems for e in dsem}
        drr = {e: 0 for e in dsem}
        for o in ops:
            e = o["eng"]
            if o["dma"]:
                j = drr[e]
                drr[e] = (j + 1) % self.n_dma_sems
                o["prev_on_sem"] = (dsem[e][j], dcount[e][j]) if dcount[e][j] else None
                dcount[e][j] += 16
                o["sem"] = dsem[e][j]
                o["val"] = dcount[e][j]
            elif o["sig"]:
                ecount[e] += 1
                o["sem"] = esem[e]
                o["val"] = ecount[e]
        per_eng = {e: [] for e in ENGS}
        for i, o in enumerate(ops):
            per_eng[o["eng"]].append(i)
        block = stack.enter_context(nc.Block())

        def run(e, eng):
            waited = {}

            def wait(sem, val):
                key = id(sem)
                if waited.get(key, 0) >= val:
                    return
                waited[key] = val
                eng.wait_ge(sem, val)

            for i in per_eng[e]:
                o = ops[i]
                for d in sorted(o["deps"]):
                    p = ops[d]
                    if p["eng"] == "pe" and e == "pe" and not p["dma"] and not o["dma"]:
                        continue
                    if d in o["tdeps"] and p["eng"] == e and not p["dma"] and not o["dma"]:
                        continue
                    wait(p["sem"], p["val"])
                if o["dma"] and o["prev_on_sem"] is not None:
                    wait(*o["prev_on_sem"])
                ins = o["fn"](eng)
                if o["dma"]:
                    ins.then_inc(o["sem"], 16)
                elif o["sig"]:
                    ins.then_inc(o["sem"], 1)
            if e == "sp":
                for d in sorted(final_deps):
                    wait(ops[d]["sem"], ops[d]["val"])

        @block.tensor
        def _(eng):
            run("pe", eng)

        @block.scalar
        def _(eng):
            run("act", eng)

        @block.vector
        def _(eng):
            run("dve", eng)

        @block.gpsimd
        def _(eng):
            run("pool", eng)

        @block.sync
        def _(eng):
            run("sp", eng)


D = 1024
NTOK = 2048
WIN = 8192
ST = 256
NST = WIN // ST
OWN0 = (WIN - NTOK) // ST
HALO0 = OWN0 - 2
C_Q, C_K, C_V, C_Z, C_AB, C_QB, C_KB, C_VB = 0, 512, 1024, 1536, 2048, 2056, 2568, 3080
NE = 32


def _grp(fns):
    def f(e):
        ins = None
        for g in fns:
            ins = g(e)
        return ins
    return f


def build(n_exp=NE, dbg=False, a_list=None, do_b=True, do_c=True, stop=99):
    a_list = list(range(NST)) if a_list is None else a_list
    nc = bass.Bass("TRN2", target_bir_lowering=False)
    DI = lambda name, shape, dt=F32: nc.dram_tensor(name, shape, dt, kind="ExternalInput").ap()
    xw = DI("xw", [WIN, D]); qvalid_d = DI("qvalid", [128, 3]); kmask_d = DI("kmask", [128, 1])
    ccol_d = DI("ccol", [128, 8]); w_ada = DI("w_ada", [D, 6 * D]); b_ada = DI("b_ada", [1, 6 * D])
    norm_w = DI("norm_w", [1, 4 * D]); w_in = DI("w_in", [D, 3592]); cw_d = DI("cw", [128, 12, 4])
    imk_d = DI("imask", [128, 5, 128]); alog_d = DI("alog", [128, 4]); dtb_d = DI("dtb", [128, 4]); anw_d = DI("anw", [128, 1])
    bm5_d = DI("bm5", [128, 8, 640]); w_out = DI("w_out", [D, D]); w_router = DI("w_router", [D, NE])
    br_d = DI("br", [128, NE]); w_up = DI("w_up", [n_exp, D, 2 * D]); bu_d = DI("bu", [128, NE, 16])
    w_down = DI("w_down", [n_exp, D, D]); b_down = DI("b_down", [n_exp, D])
    out = nc.dram_tensor("out", [NTOK, D], F32, kind="ExternalOutput").ap()
    x1s = nc.dram_tensor("x1s", [NTOK, D], F32).ap()
    h2s = nc.dram_tensor("h2s", [128, 8, NTOK], BF16).ap()
    dbg_o = {}
    if dbg:
        dbg_o["ota"] = nc.dram_tensor("dbg_ota", [128, 4, NTOK], F32, kind="ExternalOutput").ap()
        dbg_o["otb"] = nc.dram_tensor("dbg_otb", [128, 4, NTOK], F32, kind="ExternalOutput").ap()
        dbg_o["x1"] = nc.dram_tensor("dbg_x1", [NTOK, D], F32, kind="ExternalOutput").ap()
        dbg_o["lg"] = nc.dram_tensor("dbg_lg", [128, 16, NE], F32, kind="ExternalOutput").ap()

    with ExitStack() as st0:
        def mk(st):
            return lambda name, shape, dt=F32: st.enter_context(nc.sbuf_tensor(name, shape, dt))
        S0 = mk(st0)
        IDF = S0("IDF", [128, 128]); IDB = S0("IDB", [128, 128], BF16)
        ONF = S0("ONF", [128, 128]); ONB = S0("ONB", [128, 128], BF16)
        LG = S0("LG", [128, 16, NE])
        G2R = S0("G2R", [1, D])
        A1C = S0("A1C", [128, 8]); SH1C = S0("SH1C", [128, 8]); A2C = S0("A2C", [128, 8]); SH2C = S0("SH2C", [128, 8])
        EPSC = S0("EPSC", [128, 1])
        PB = [st0.enter_context(nc.psum_tensor("pb%d" % i, [128, 512], F32)) for i in range(8)]
        stAB = ExitStack()
        SAB = mk(stAB)
        OTA = SAB("OTA", [128, 4, NTOK], BF16)
        G1R = SAB("G1R", [1, D])

        def bank(b):
            return ["q%d_%d" % (b, i) for i in range(4)]

        def qs(b, q0, n=1):
            return ["q%d_%d" % (b, i) for i in range(q0, q0 + n)]

        with ExitStack() as st:
            S = mk(st)
            P = Prog(nc)
            UF = S("UF", [128, 128]); NM2 = S("NM2", [128, 128]); NMT = S("NMT", [128, 128]); IMK = S("IMK", [128, 5, 128])
            WA = S("WA", [128, 8, 2056], BF16)
            QV = S("QV", [128, 3]); CW = S("CW", [128, 12, 4]); NEGA = S("NEGA", [128, 4]); DTB = S("DTB", [128, 4])
            ANW = S("ANW", [128, 1]); CC = S("CC", [128, 8]); SC = S("SC", [128, 8])
            WST = S("WST", [128, 8, 512])
            BST = S("BST", [1, 512]); NWT = S("NWT", [1, 512]); ROWT = S("ROWT", [1, 512])
            P.add("pool", lambda e: e.memset(IDF[:], 0.0), w=["IDF"])
            P.add("pool", lambda e: e.affine_select(out=IDF[:], in_=IDF[:], pattern=[[-1, 128]], compare_op=ALU.not_equal,
                                                     fill=1.0, base=0, channel_multiplier=1), r=["IDF"], w=["IDF"])
            P.add("pool", lambda e: e.tensor_copy(out=IDB[:], in_=IDF[:]), r=["IDF"], w=["IDB"])
            P.add("pool", lambda e: e.memset(ONF[:], 1.0), w=["ONF"])
            P.add("pool", lambda e: e.memset(ONB[:], 1.0), w=["ONB"])
            P.add("pool", lambda e: e.memset(EPSC[:], EPS), w=["EPSC"])
            P.add("pool", lambda e: e.memset(UF[:], 1.0), w=["UF"])
            P.add("pool", lambda e: e.affine_select(out=UF[:], in_=UF[:], pattern=[[1, 128]], compare_op=ALU.is_ge,
                                                     fill=0.0, base=0, channel_multiplier=-1), r=["UF"], w=["UF"])
            P.add("pool", lambda e: e.memset(NM2[:], -NEG), w=["NM2"])
            P.add("pool", lambda e: e.affine_select(out=NM2[:], in_=NM2[:], pattern=[[1, 128]], compare_op=ALU.is_ge,
                                                     fill=0.0, base=0, channel_multiplier=-1), r=["NM2"], w=["NM2"])
            P.add("pool", lambda e: e.memset(NMT[:], NEG), w=["NMT"])
            P.add("pool", lambda e: e.affine_select(out=NMT[:], in_=NMT[:], pattern=[[-1, 128]], compare_op=ALU.is_gt,
                                                     fill=0.0, base=0, channel_multiplier=1), r=["NMT"], w=["NMT"])
            for dst, src, key in ((IMK, imk_d, "IMK"), (QV, qvalid_d, "QV"), (CW, cw_d, "CW"), (NEGA, alog_d, "NEGA"), (DTB, dtb_d, "DTB"),
                                  (ANW, anw_d, "ANW"), (CC, ccol_d, "CC")):
                P.add("sp", (lambda dst, src: lambda e: e.dma_start(out=dst[:], in_=src))(dst, src), w=[key], dma=True)
            w_in_r = w_in.rearrange("(k p) c -> p k c", p=128)
            P.add("pool", lambda e: e.dma_start(out=WA[:, :, 0:2048], in_=w_in_r[:, :, 0:2048]), w=["WA0"], dma=True)
            P.add("pool", lambda e: e.dma_start(out=WA[:, :, 2048:2056], in_=w_in_r[:, :, 2048:2056]), w=["WA1"], dma=True)
            P.add("act", lambda e: e.activation(out=NEGA[:], in_=NEGA[:], func=AF.Exp), r=["NEGA"], w=["NEGA"])
            P.add("dve", lambda e: e.tensor_scalar(out=NEGA[:], in0=NEGA[:], scalar1=-1.0, scalar2=None, op0=ALU.mult), r=["NEGA"], w=["NEGA"])
            P.add("act", lambda e: e.activation(out=SC[:], in_=CC[:], func=AF.Silu), r=["CC"], w=["SC"])
            w_ada_r = w_ada.rearrange("(k p) n -> p k n", p=128)
            for nb in range(12):
                kind, half = nb // 2, nb % 2
                P.add("sp", (lambda nb: lambda e: e.dma_start(out=WST[:], in_=w_ada_r[:, :, nb * 512:(nb + 1) * 512]))(nb), w=["WST"], dma=True)
                P.add("sp", (lambda nb: lambda e: e.dma_start(out=BST[:], in_=b_ada[0:1, nb * 512:(nb + 1) * 512]))(nb), w=["BST"], dma=True)
                if kind in (1, 2, 4, 5):
                    nwi = {1: 0, 2: 1, 4: 2, 5: 3}[kind]
                    P.add("sp", (lambda o_: lambda e: e.dma_start(out=NWT[:], in_=norm_w[0:1, o_:o_ + 512]))(nwi * D + half * 512), w=["NWT"], dma=True)
                P.add("pe", _grp([(lambda k: lambda e: e.matmul(PB[0][0:1, :], lhsT=SC[:, k:k + 1], rhs=WST[:, k, :], start=(k == 0), stop=(k == 7)))(k) for k in range(8)]),
                      r=["SC", "WST"], w=bank(0))
                P.add("dve", lambda e: e.tensor_tensor(out=ROWT[:], in0=PB[0][0:1, :], in1=BST[:], op=ALU.add), r=bank(0) + ["BST"], w=["ROWT"])
                if kind in (1, 4):
                    P.add("dve", lambda e: e.scalar_tensor_tensor(out=ROWT[:], in0=ROWT[:], scalar=1.0, in1=NWT[:], op0=ALU.add, op1=ALU.mult), r=["ROWT", "NWT"], w=["ROWT"])
                elif kind in (2, 5):
                    P.add("dve", lambda e: e.tensor_tensor(out=ROWT[:], in0=ROWT[:], in1=NWT[:], op=ALU.mult), r=["ROWT", "NWT"], w=["ROWT"])
                if kind in (0, 1, 3, 4):
                    dst, key = {0: (SH1C, "SH1C"), 1: (A1C, "A1C"), 3: (SH2C, "SH2C"), 4: (A2C, "A2C")}[kind]
                    P.add("pe", _grp([(lambda k: lambda e: e.matmul(PB[1][:, k:k + 1], lhsT=ROWT[0:1, k * 128:(k + 1) * 128], rhs=ONF[0:1, 0:1], start=True, stop=True))(k)
                                      for k in range(4)]), r=["ROWT", "ONF"], w=bank(1))
                    P.add("dve", (lambda dst, half: lambda e: e.tensor_copy(out=dst[:, half * 4:half * 4 + 4], in_=PB[1][:, 0:4]))(dst, half), r=bank(1), w=[key])
                else:
                    dst, key = (G1R, "G1R") if kind == 2 else (G2R, "G2R")
                    P.add("dve", (lambda dst, half: lambda e: e.tensor_copy(out=dst[0:1, half * 512:(half + 1) * 512], in_=ROWT[:]))(dst, half), r=["ROWT"], w=[key])

            XT = [S("XT%d" % i, [128, D]) for i in range(2)]
            JNK = S("JNK", [128, D], BF16)
            XS = [S("XS%d" % i, [128, D], BF16) for i in range(2)]
            HT = S("HT", [128, 8, ST], BF16)
            RAW = S("RAW", [128, 12, ST + 3], BF16)
            CV = S("CV", [128, 12, ST])
            SIL = S("SIL", [128, 12, ST], BF16)
            SQ = S("SQ", [128, 8, ST], BF16)
            LNT = S("LNT", [128, 8, ST])
            QKVL = [S("QKV%d" % i, [128, 12, ST], BF16) for i in range(2)]
            ZSL = [S("ZS%d" % i, [128, 4, ST], BF16) for i in range(2)]
            ABTL = [S("ABT%d" % i, [128, 2, 8]) for i in range(2)]
            GSTL = [S("GST%d" % i, [128, 2, 4]) for i in range(2)]; BETL = [S("BET%d" % i, [128, 2, 4]) for i in range(2)]
            NBETL = [S("NBET%d" % i, [128, 2, 4]) for i in range(2)]
            SSC = S("SSC", [128, 4]); RSC = S("RSC", [128, 4])
            Sm = [S("Sm%d" % h, [128, 128]) for h in range(4)]
            Sb = [S("Sb%d" % h, [128, 128], BF16) for h in range(4)]
            NSET = 4
            def tset(i):
                t = {}
                for nm in ("gsb", "decS", "decT", "egb", "o1"):
                    t[nm] = S("%s_%d" % (nm, i), [128, 128])
                for nm in ("A", "AT", "P0", "PT0", "P1", "PT1", "RT", "TM", "BKm", "Ym", "kbg", "kdec", "vb", "nwT", "vnew", "attnT", "qdT", "sq"):
                    t[nm] = S("%s_%d" % (nm, i), [128, 128], BF16)
                t["gc"] = S("gc_%d" % i, [128, 8])
                return t
            TS = [tset(i) for i in range(NSET)]
            P.add("pool", lambda e: e.memset(RAW[:], 0.0), w=["RAW%d" % c for c in range(12)])
            for h in range(4):
                P.add("pool", (lambda h: lambda e: e.memset(Sm[h][:], 0.0))(h), w=["Sm%d" % h])
                P.add("pool", (lambda h: lambda e: e.memset(Sb[h][:], 0.0))(h), w=["Sb%d" % h])

            xw_t = xw.rearrange("(n p) d -> n p d", p=128)
            PBb0 = PB[0][:].bitcast(BF16)
            PBbH = [PB[4 + h][:].bitcast(BF16) for h in range(4)]

            def do_st(s):
                own = s >= OWN0
                q = s // 8
                par = s % 2; kp = "p%d" % par
                QKVc, ZSc, ABTc, GSTc, BETc, NBETc = QKVL[par], ZSL[par], ABTL[par], GSTL[par], BETL[par], NBETL[par]
                for u in range(2):
                    ti = 2 * s + u
                    xt = XT[u]; xs_ = XS[u]
                    P.add("sp", (lambda xt, ti: lambda e: e.dma_start(out=xt[:], in_=xw_t[ti]))(xt, ti), w=["XT%d" % u], dma=True)
                    if stop <= 0.25:
                        continue
                    P.add("act", (lambda xt, u: lambda e: e.activation(out=JNK[:], in_=xt[:], func=AF.Square, accum_out=SSC[:, u:u + 1]))(xt, u),
                          r=["XT%d" % u], w=["JNK", "SSC%d" % u])
                    P.add("act", (lambda u: lambda e: e.activation(out=RSC[:, u:u + 1], in_=SSC[:, u:u + 1], func=AF.Ln, bias=EPSC[:], scale=1.0 / D))(u),
                          r=["SSC%d" % u, "EPSC"], w=["RSC%d" % u])
                    P.add("act", (lambda u: lambda e: e.activation(out=RSC[:, u:u + 1], in_=RSC[:, u:u + 1], func=AF.Exp, scale=-0.5))(u),
                          r=["RSC%d" % u], w=["RSC%d" % u])
                    P.add("dve", (lambda xt, xs_, u: lambda e: e.tensor_scalar(out=xs_[:], in0=xt[:], scalar1=RSC[:, u:u + 1], scalar2=None, op0=ALU.mult))(xt, xs_, u),
                          r=["XT%d" % u, "RSC%d" % u], w=["XS%d" % u])
                    if stop <= 0.5:
                        continue
                    P.add("pe", (lambda xs_: _grp([(lambda k: lambda e: e.transpose(PBb0[:, k * 128:(k + 1) * 128], xs_[:, k * 128:(k + 1) * 128], IDB[:]))(k)
                                                   for k in range(8)]))(xs_), r=["XS%d" % u, "IDB"], w=bank(0))
                    for k in range(8):
                        eng = "act"
                        if eng == "act":
                            f = (lambda k, u: lambda e: e.activation(out=HT[:, k, u * 128:(u + 1) * 128], in_=PBb0[:, k * 128:(k + 1) * 128],
                                                                     func=AF.Identity, bias=SH1C[:, k:k + 1], scale=A1C[:, k:k + 1]))(k, u)
                        else:
                            f = (lambda k, u: lambda e: e.tensor_scalar(out=HT[:, k, u * 128:(u + 1) * 128], in0=PBb0[:, k * 128:(k + 1) * 128],
                                                                        scalar1=A1C[:, k:k + 1], scalar2=SH1C[:, k:k + 1], op0=ALU.mult, op1=ALU.add))(k, u)
                        P.add(eng, f, r=bank(0) + ["A1C", "SH1C"], w=["HT%d_%d" % (k, u)])
                HTK = ["HT%d_%d" % (k, u) for k in range(8) for u in range(2)]
                if stop <= 1:
                    return
                chunks = list(range(4, 12)) + (list(range(0, 4)) + list(range(12, 16)) if own else ([0, 1, 2, 3] if s == OWN0 - 1 else []))
                for ci, c in enumerate(chunks):
                    slot_b, slot_q = 1 + (ci % 2), 0
                    pso = PB[slot_b][:, 0:ST]
                    P.add("pe", (lambda c, pso: _grp([(lambda k: lambda e: e.matmul(pso, lhsT=WA[:, k, c * 128:(c + 1) * 128], rhs=HT[:, k, :],
                                                                                start=(k == 0), stop=(k == 7)))(k) for k in range(8)]))(c, pso),
                          r=HTK + ["WA0"], w=qs(slot_b, 0, 2))
                    if c < 12:
                        dst = RAW[:, c, 3:3 + ST]
                        if own:
                            P.add("act", (lambda dst, pso: lambda e: e.copy(out=dst, in_=pso))(dst, pso), r=qs(slot_b, 0, 2), w=["RAW%d" % c])
                        else:
                            P.add("dve", (lambda dst, pso, q: lambda e: e.tensor_scalar(out=dst, in0=pso, scalar1=QV[:, q:q + 1], scalar2=None, op0=ALU.mult))(dst, pso, q),
                                  r=qs(slot_b, 0, 2) + ["QV"], w=["RAW%d" % c])
                    else:
                        P.add("act", (lambda c, pso: lambda e: e.activation(out=ZSc[:, c - 12, :], in_=pso, func=AF.Silu))(c, pso),
                              r=qs(slot_b, 0, 2), w=["ZS%d" % (c - 12) + kp])
                if stop <= 2:
                    return
                for u in range(2):
                    P.add("pe", (lambda u: _grp([(lambda k: lambda e: e.matmul(PB[3][:, 0:8], lhsT=HT[:, k, u * 128:(u + 1) * 128], rhs=WA[:, k, 2048:2056],
                                                                            start=(k == 0), stop=(k == 7)))(k) for k in range(8)]))(u),
                          r=HTK + ["WA1"], w=qs(3, 0))
                    if own:
                        P.add("dve", (lambda u: lambda e: e.tensor_copy(out=ABTc[:, u, :], in_=PB[3][:, 0:8]))(u), r=qs(3, 0), w=["ABT%d" % u + kp])
                    else:
                        P.add("dve", (lambda u, q: lambda e: e.tensor_scalar(out=ABTc[:, u, :], in0=PB[3][:, 0:8], scalar1=QV[:, q:q + 1], scalar2=None, op0=ALU.mult))(u, q),
                              r=qs(3, 0) + ["QV"], w=["ABT%d" % u + kp])
                    P.add("dve", (lambda u: lambda e: e.tensor_tensor(out=GSTc[:, u, :], in0=ABTc[:, u, 0:4], in1=DTB[:], op=ALU.add))(u), r=["ABT%d" % u + kp, "DTB"], w=["GST%d" % u + kp])
                    P.add("act", (lambda u: lambda e: e.activation(out=GSTc[:, u, :], in_=GSTc[:, u, :], func=AF.Exp))(u), r=["GST%d" % u + kp], w=["GST%d" % u + kp])
                    P.add("act", (lambda u: lambda e: e.activation(out=GSTc[:, u, :], in_=GSTc[:, u, :], func=AF.Ln, bias=ONF[:, 0:1]))(u), r=["GST%d" % u + kp, "ONF"], w=["GST%d" % u + kp])
                    P.add("dve", (lambda u: lambda e: e.tensor_tensor(out=GSTc[:, u, :], in0=GSTc[:, u, :], in1=NEGA[:], op=ALU.mult))(u), r=["GST%d" % u + kp, "NEGA"], w=["GST%d" % u + kp])
                    P.add("act", (lambda u: lambda e: e.activation(out=BETc[:, u, :], in_=ABTc[:, u, 4:8], func=AF.Sigmoid))(u), r=["ABT%d" % u + kp], w=["BET%d" % u + kp])
                    P.add("dve", (lambda u: lambda e: e.tensor_scalar(out=NBETc[:, u, :], in0=BETc[:, u, :], scalar1=-1.0, scalar2=None, op0=ALU.mult))(u), r=["BET%d" % u + kp], w=["NBET%d" % u + kp])
                if stop <= 3:
                    return
                for c in chunks:
                    if c >= 12:
                        continue
                    rk = "RAW%d" % c
                    P.add("pool", (lambda c: lambda e: e.tensor_scalar(out=CV[:, c, :], in0=RAW[:, c, 0:ST], scalar1=CW[:, c, 0:1], scalar2=None, op0=ALU.mult))(c),
                          r=[rk, "CW"], w=["CV%d" % c])
                    for tp in range(1, 4):
                        P.add("dve", (lambda c, tp: lambda e: e.scalar_tensor_tensor(out=CV[:, c, :], in0=RAW[:, c, tp:tp + ST], scalar=CW[:, c, tp:tp + 1],
                                                                                      in1=CV[:, c, :], op0=ALU.mult, op1=ALU.add))(c, tp),
                              r=[rk, "CW", "CV%d" % c], w=["CV%d" % c])
                    P.add("pool", (lambda c: lambda e: e.tensor_copy(out=RAW[:, c, 0:3], in_=RAW[:, c, ST:ST + 3]))(c), r=[rk], w=[rk])
                    if c >= 8:
                        P.add("act", (lambda c: lambda e: e.activation(out=QKVc[:, c, :], in_=CV[:, c, :], func=AF.Silu))(c), r=["CV%d" % c], w=["QKV%d" % c + kp])
                    else:
                        P.add("act", (lambda c: lambda e: e.activation(out=SIL[:, c, :], in_=CV[:, c, :], func=AF.Silu))(c), r=["CV%d" % c], w=["SIL%d" % c])
                        P.add("act", (lambda c: lambda e: e.activation(out=SQ[:, c, :], in_=SIL[:, c, :], func=AF.Square))(c), r=["SIL%d" % c], w=["SQ%d" % c])
                        P.add("pe", (lambda c: lambda e: e.matmul(PB[3][:, ST:2 * ST], lhsT=ONB[:], rhs=SQ[:, c, :], start=True, stop=True))(c),
                              r=["SQ%d" % c, "ONB"], w=qs(3, 2, 2))
                        P.add("act", (lambda c: lambda e: e.activation(out=LNT[:, c, :], in_=PB[3][:, ST:2 * ST], func=AF.Ln, bias=EPSC[:]))(c),
                              r=qs(3, 2, 2) + ["EPSC"], w=["LNT%d" % c])
                        P.add("act", (lambda c: lambda e: e.activation(out=LNT[:, c, :], in_=LNT[:, c, :], func=AF.Exp, scale=-0.5))(c), r=["LNT%d" % c], w=["LNT%d" % c])
                        sc_ = (128.0 ** -0.5) if c < 4 else 1.0
                        P.add("dve", (lambda c, sc_: lambda e: e.scalar_tensor_tensor(out=QKVc[:, c, :], in0=SIL[:, c, :], scalar=sc_, in1=LNT[:, c, :],
                                                                                      op0=ALU.mult, op1=ALU.mult))(c, sc_),
                              r=["SIL%d" % c, "LNT%d" % c], w=["QKV%d" % c + kp])
                if stop <= 4:
                    return
                def chain(u, h):
                    cs = slice(u * 128, (u + 1) * 128)
                    t = TS[h]; tk = "_%d" % h
                    K = lambda n: n + tk
                    hb = 4 + h; Hf = PB[hb]; H16 = PBbH[h]
                    Q = lambda q_: Hf[:, q_ * 128:(q_ + 1) * 128]
                    QK = lambda q_: qs(hb, q_)
                    kT = QKVc[:, 4 + h, cs]; vT = QKVc[:, 8 + h, cs]; qT = QKVc[:, h, cs]
                    kk, vk, qk = "QKV%d" % (4 + h) + kp, "QKV%d" % (8 + h) + kp, "QKV%d" % h + kp
                    gcol = GSTc[:, u, h:h + 1]
                    gk, bk_, nbk = "GST%d" % u + kp, "BET%d" % u + kp, "NBET%d" % u + kp
                    gc = t["gc"]
                    BETl, NBETl, ZSl = BETc, NBETc, ZSc
                    P.add("dve", lambda e: e.tensor_scalar(out=t["gsb"][:], in0=ONF[:], scalar1=gcol, scalar2=None, op0=ALU.mult), r=["ONF", gk], w=[K("gsb")])
                    yield
                    P.add("pe", _grp([lambda e: e.matmul(Q(0), lhsT=t["gsb"][:], rhs=UF[:], start=True, stop=True),
                                      lambda e: e.matmul(Hf[:, 128:129], lhsT=UF[:], rhs=gcol, start=True, stop=True),
                                      lambda e: e.matmul(Hf[:, 129:130], lhsT=ONF[:], rhs=gcol, start=True, stop=True)]),
                          r=[K("gsb"), "UF", "ONF", gk], w=QK(0) + QK(1))
                    P.add("dve", lambda e: e.tensor_copy(out=gc[:, 0:2], in_=Hf[:, 128:130]), r=QK(1), w=[K("gc")])
                    if own:
                        P.add("act", lambda e: e.activation(out=t["egb"][:], in_=Q(0), func=AF.Exp), r=QK(0), w=[K("egb")])
                    yield
                    P.add("dve", lambda e: e.tensor_scalar(out=gc[:, 2:3], in0=gc[:, 0:1], scalar1=-1.0, scalar2=None, op0=ALU.mult), r=[K("gc")], w=[K("gc2")])
                    P.add("dve", lambda e: e.tensor_tensor(out=gc[:, 3:4], in0=gc[:, 1:2], in1=gc[:, 0:1], op=ALU.subtract), r=[K("gc")], w=[K("gc3")])
                    P.add("act", lambda e: e.activation(out=gc[:, 4:5], in_=gc[:, 0:1], func=AF.Exp), r=[K("gc")], w=[K("gc4")])
                    P.add("act", lambda e: e.activation(out=gc[:, 6:7], in_=gc[:, 1:2], func=AF.Exp), r=[K("gc")], w=[K("gc6")])
                    yield
                    P.add("dve", lambda e: e.tensor_tensor(out=gc[:, 4:5], in0=gc[:, 4:5], in1=BETl[:, u, h:h + 1], op=ALU.mult), r=[K("gc4"), bk_], w=[K("gc4")])
                    P.add("act", lambda e: e.activation(out=gc[:, 5:6], in_=gc[:, 3:4], func=AF.Exp), r=[K("gc3")], w=[K("gc5")])
                    P.add("pe", _grp([lambda e: e.matmul(Q(2), lhsT=t["gsb"][:], rhs=UF[:], start=True, stop=False),
                                      lambda e: e.matmul(Q(2), lhsT=IDF[:], rhs=NM2[:], start=False, stop=True)]),
                          r=[K("gsb"), "UF", "IDF", "NM2"], w=QK(2))
                    P.add("act", lambda e: e.activation(out=t["decS"][:], in_=Q(2), func=AF.Exp, bias=gc[:, 0:1], scale=-1.0), r=QK(2) + [K("gc")], w=[K("decS")])
                    yield
                    P.add("pe", _grp([lambda e: e.transpose(H16[:, 768:896], kT, IDB[:]), lambda e: e.transpose(H16[:, 896:1024], vT, IDB[:])]),
                          r=[kk, vk, "IDB"], w=QK(3))
                    P.add("dve", lambda e: e.tensor_scalar(out=t["kbg"][:], in0=H16[:, 768:896], scalar1=gc[:, 4:5], scalar2=None, op0=ALU.mult), r=QK(3) + [K("gc4")], w=[K("kbg")])
                    P.add("dve", lambda e: e.tensor_scalar(out=t["kdec"][:], in0=H16[:, 768:896], scalar1=gc[:, 5:6], scalar2=None, op0=ALU.mult), r=QK(3) + [K("gc5")], w=[K("kdec")])
                    P.add("dve", lambda e: e.tensor_scalar(out=t["vb"][:], in0=H16[:, 896:1024], scalar1=BETl[:, u, h:h + 1], scalar2=None, op0=ALU.mult), r=QK(3) + [bk_], w=[K("vb")])
                    yield
                    P.add("pe", lambda e: e.matmul(Q(2), lhsT=kT, rhs=kT, start=True, stop=True), r=[kk], w=QK(2))
                    P.add("dve", lambda e: e.scalar_tensor_tensor(out=t["A"][:], in0=Q(2), scalar=NBETl[:, u, h:h + 1], in1=t["decS"][:], op0=ALU.mult, op1=ALU.mult),
                          r=QK(2) + [nbk, K("decS")], w=[K("A")])
                    yield
                    P.add("pe", lambda e: e.transpose(H16[:, 768:896], t["A"][:], IDB[:]), r=[K("A"), "IDB"], w=QK(3))
                    P.add("dve", lambda e: e.tensor_copy(out=t["AT"][:], in_=H16[:, 768:896]), r=QK(3), w=[K("AT")])
                    P.add("pool", lambda e: e.tensor_tensor(out=t["P0"][:], in0=t["A"][:], in1=IMK[:, 0, :], op=ALU.mult), r=[K("A"), "IMK"], w=[K("P0")])
                    P.add("pool", lambda e: e.tensor_tensor(out=t["TM"][:], in0=t["P0"][:], in1=IDB[:], op=ALU.add), r=[K("P0"), "IDB"], w=[K("TM")])
                    yield
                    P.add("pool", lambda e: e.tensor_tensor(out=t["PT0"][:], in0=t["AT"][:], in1=IMK[:, 0, :], op=ALU.mult), r=[K("AT"), "IMK"], w=[K("PT0")])
                    P.add("pool", lambda e: e.tensor_tensor(out=t["RT"][:], in0=t["PT0"][:], in1=IDB[:], op=ALU.add), r=[K("PT0"), "IDB"], w=[K("RT")])
                    yield

                    def upd(ln, with_T):
                        P.add("pe", lambda e: e.matmul(Q(2), lhsT=t[ln][:], rhs=t["RT"][:], start=True, stop=True), r=[K(ln), K("RT")], w=QK(2))
                        if with_T:
                            P.add("pe", lambda e: e.matmul(Q(3), lhsT=t["RT"][:], rhs=t[ln][:], start=True, stop=True), r=[K(ln), K("RT")], w=QK(3))
                        P.add("dve", lambda e: e.tensor_tensor(out=t["RT"][:], in0=t["RT"][:], in1=Q(2), op=ALU.add), r=QK(2) + [K("RT")], w=[K("RT")])
                        if with_T:
                            P.add("dve", lambda e: e.tensor_tensor(out=t["TM"][:], in0=t["TM"][:], in1=Q(3), op=ALU.add), r=QK(3) + [K("TM")], w=[K("TM")])
                    P.add("pe", lambda e: e.matmul(Q(0), lhsT=t["PT0"][:], rhs=t["P0"][:], start=True, stop=True), r=[K("P0"), K("PT0")], w=QK(0))
                    P.add("pe", lambda e: e.matmul(Q(1), lhsT=t["P0"][:], rhs=t["PT0"][:], start=True, stop=True), r=[K("P0"), K("PT0")], w=QK(1))
                    P.add("act", lambda e: e.copy(out=t["P1"][:], in_=Q(0)), r=QK(0), w=[K("P1")])
                    P.add("act", lambda e: e.copy(out=t["PT1"][:], in_=Q(1)), r=QK(1), w=[K("PT1")])
                    yield
                    upd("P1", True)
                    yield
                    P.add("pe", lambda e: e.matmul(Q(0), lhsT=t["PT1"][:], rhs=t["P1"][:], start=True, stop=True), r=[K("P1"), K("PT1")], w=QK(0))
                    P.add("act", lambda e: e.copy(out=t["P0"][:], in_=Q(0)), r=QK(0), w=[K("P0")])
                    yield
                    upd("P0", True)
                    yield
                    for lv in range(1, 5):
                        P.add("pool", (lambda lv: lambda e: e.tensor_tensor(out=t["BKm"][:], in0=t["AT"][:], in1=IMK[:, lv, :], op=ALU.mult))(lv), r=[K("AT"), "IMK"], w=[K("BKm")])
                        P.add("pe", lambda e: e.matmul(Q(0), lhsT=t["BKm"][:], rhs=t["TM"][:], start=True, stop=True), r=[K("BKm"), K("TM")], w=QK(0))
                        P.add("act", lambda e: e.copy(out=t["Ym"][:], in_=Q(0)), r=QK(0), w=[K("Ym")])
                        yield
                        upd("Ym", lv < 4)
                        yield
                    P.add("pe", lambda e: e.matmul(Q(0), lhsT=t["kbg"][:], rhs=t["RT"][:], start=True, stop=True), r=[K("kbg"), K("RT")], w=QK(0))
                    P.add("act", lambda e: e.activation(out=t["nwT"][:], in_=Q(0), func=AF.Copy, scale=-1.0), r=QK(0), w=[K("nwT")])
                    yield
                    P.add("pe", _grp([lambda e: e.matmul(Q(1), lhsT=t["RT"][:], rhs=t["vb"][:], start=True, stop=False),
                                      lambda e: e.matmul(Q(1), lhsT=t["nwT"][:], rhs=Sb[h][:], start=False, stop=True)]),
                          r=[K("RT"), K("vb"), K("nwT"), "Sb%d" % h], w=QK(1))
                    P.add("act", lambda e: e.copy(out=t["vnew"][:], in_=Q(1)), r=QK(1), w=[K("vnew")])
                    yield
                    if own:
                        qt_ = (s - OWN0) * 2 + u
                        ocs = slice(qt_ * 128, (qt_ + 1) * 128)
                        P.add("pe", _grp([lambda e: e.matmul(Q(2), lhsT=t["gsb"][:], rhs=UF[:], start=True, stop=False),
                                          lambda e: e.matmul(Q(2), lhsT=IDF[:], rhs=NMT[:], start=False, stop=True)]),
                              r=[K("gsb"), "UF", "IDF", "NMT"], w=QK(2))
                        P.add("pe", lambda e: e.matmul(Q(3), lhsT=kT, rhs=qT, start=True, stop=True), r=[kk, qk], w=QK(3))
                        P.add("act", lambda e: e.activation(out=t["decT"][:], in_=Q(2), func=AF.Exp, bias=gc[:, 2:3]), r=QK(2) + [K("gc2")], w=[K("decT")])
                        P.add("pool", lambda e: e.tensor_tensor(out=t["qdT"][:], in0=qT, in1=t["egb"][:], op=ALU.mult), r=[qk, K("egb")], w=[K("qdT")])
                        yield
                        P.add("dve", lambda e: e.tensor_tensor(out=t["attnT"][:], in0=Q(3), in1=t["decT"][:], op=ALU.mult), r=QK(3) + [K("decT")], w=[K("attnT")])
                        yield
                        P.add("pe", _grp([lambda e: e.matmul(Q(0), lhsT=Sb[h][:], rhs=t["qdT"][:], start=True, stop=False),
                                          lambda e: e.matmul(Q(0), lhsT=t["vnew"][:], rhs=t["attnT"][:], start=False, stop=True)]),
                              r=["Sb%d" % h, K("qdT"), K("vnew"), K("attnT")], w=QK(0))
                        P.add("act", lambda e: e.activation(out=t["sq"][:], in_=Q(0), func=AF.Square), r=QK(0), w=[K("sq")])
                        P.add("act", lambda e: e.copy(out=t["o1"][:], in_=Q(0)), r=QK(0), w=[K("o1")])
                        yield
                        P.add("pe", lambda e: e.matmul(Q(1), lhsT=ONB[:], rhs=t["sq"][:], start=True, stop=True), r=[K("sq"), "ONB"], w=QK(1))
                        P.add("act", lambda e: e.activation(out=t["egb"][:], in_=Q(1), func=AF.Ln, bias=EPSC[:], scale=1.0 / 128), r=QK(1) + ["EPSC", K("qdT")], w=[K("egb")])
                        P.add("act", lambda e: e.activation(out=t["egb"][:], in_=t["egb"][:], func=AF.Exp, scale=-0.5), r=[K("egb")], w=[K("egb")])
                        yield
                        P.add("dve", lambda e: e.tensor_tensor(out=t["o1"][:], in0=t["o1"][:], in1=t["egb"][:], op=ALU.mult), r=[K("o1"), K("egb")], w=[K("o1")])
                        P.add("dve", lambda e: e.scalar_tensor_tensor(out=OTA[:, h, ocs], in0=t["o1"][:], scalar=ANW[:, 0:1], in1=ZSl[:, h, cs], op0=ALU.mult, op1=ALU.mult),
                              r=[K("o1"), "ANW", "ZS%d" % h + kp], w=["OTA%d_%d" % (h, qt_)])
                        yield
                    P.add("pe", lambda e: e.matmul(Q(2), lhsT=t["kdec"][:], rhs=t["vnew"][:], start=True, stop=True), r=[K("kdec"), K("vnew")], w=QK(2))
                    P.add("dve", lambda e: e.scalar_tensor_tensor(out=Sm[h][:], in0=Sm[h][:], scalar=gc[:, 6:7], in1=Q(2), op0=ALU.mult, op1=ALU.add),
                          r=QK(2) + [K("gc6"), "Sm%d" % h], w=["Sm%d" % h])
                    P.add("act", lambda e: e.copy(out=Sb[h][:], in_=Sm[h][:]), r=["Sm%d" % h], w=["Sb%d" % h])
                    yield

                for u in range(2):
                    gens = [chain(u, h) for h in range(4)]
                    while gens:
                        for g_ in list(gens):
                            try:
                                next(g_)
                            except StopIteration:
                                gens.remove(g_)
            for s_ in a_list:
                do_st(s_)
            fin = []
            if dbg:
                for nm, tsr in (("HT", HT), ("QKV", QKVL[0]), ("RAW", RAW), ("ZS", ZSL[0]), ("GST", GSTL[0]), ("BET", BETL[0]), ("ABT", ABTL[0]), ("CV", CV),
                                ("A1C", A1C), ("SH1C", SH1C), ("Sm0", Sm[0]), ("decS", TS[DS]["decS"]), ("A", TS[DS]["A"]), ("RT", TS[DS]["RT"]),
                                ("vnew", TS[DS]["vnew"]), ("gc", TS[DS]["gc"]), ("kbg", TS[DS]["kbg"]), ("kdec", TS[DS]["kdec"]), ("nwT", TS[DS]["nwT"]),
                                ("decT", TS[DS]["decT"]), ("attnT", TS[DS]["attnT"]), ("qdT", TS[DS]["qdT"]), ("o1", TS[DS]["o1"])):
                    dd = nc.dram_tensor("dump_" + nm, list(tsr.shape), F32, kind="ExternalOutput").ap()
                    P.add("pool", (lambda dd, tsr: lambda e: e.dma_start(out=dd, in_=tsr[:]))(dd, tsr), w=["dump_" + nm], dma=True, after_all=True)
                    fin.append("dump_" + nm)
                P.add("pool", lambda e: e.dma_start(out=dbg_o["ota"], in_=OTA[:]), w=["dbg_ota"], dma=True, after_all=True)
                fin.append("dbg_ota")
            P.emit(st, final_wait_keys=fin)
            print("phase A ops:", len(P.ops))

        with ExitStack() as st:
            S = mk(st)
            P = Prog(nc)
            WB = S("WB", [128, 8, 1536], BF16); WO = S("WO", [128, 8, D], BF16)
            BM5 = S("BM5", [128, 8, 640]); KR = S("KR", [128, 4, 1024], BF16); VR = S("VR", [128, 8, 512], BF16)
            WRF = S("WRF", [128, 8, NE]); BR = S("BR", [128, NE]); KM = S("KM", [128, 1]); G1B = S("G1B", [128, D])
            XT = [S("XTb%d" % i, [128, D]) for i in range(2)]
            JNK = S("JNKb", [128, D], BF16)
            XS = [S("XSb%d" % i, [128, D], BF16) for i in range(2)]
            HT = S("HTb", [128, 8, ST], BF16)
            QB = S("QB", [128, 4, ST], BF16)
            STS = S("STS", [128, 640]); PT = S("PT", [128, 640], BF16); RDEN = S("RDEN", [64, 128])
            OTB = S("OTB", [128, 4, ST], BF16)
            T1 = S("T1", [128, D]); H2 = S("H2", [128, D]); H2F = S("H2F", [128, 8, 128]); H2B = S("H2B", [128, 8, 128], BF16)
            SSC = S("SSCb", [128, 8]); RSC = S("RSCb", [128, 8])
            w_in_r = w_in.rearrange("(k p) c -> p k c", p=128)
            P.add("pool", lambda e: e.dma_start(out=WB[:], in_=w_in_r[:, :, 2056:3592]), w=["WB"], dma=True)
            P.add("pool", lambda e: e.dma_start(out=WO[:], in_=w_out.rearrange("(k p) c -> p k c", p=128)), w=["WO"], dma=True)
            P.add("sp", lambda e: e.dma_start(out=BM5[:], in_=bm5_d), w=["BM5"], dma=True)
            P.add("sp", lambda e: e.dma_start(out=WRF[:], in_=w_router.rearrange("(k p) c -> p k c", p=128)), w=["WRF"], dma=True)
            P.add("sp", lambda e: e.dma_start(out=BR[:], in_=br_d), w=["BR"], dma=True)
            P.add("sp", lambda e: e.dma_start(out=KM[:], in_=kmask_d), w=["KM"], dma=True)
            for half in range(2):
                P.add("pe", (lambda half: lambda e: e.matmul(PB[1][:, :], lhsT=ONF[0:1, :], rhs=G1R[0:1, half * 512:(half + 1) * 512], start=True, stop=True))(half),
                      r=[], w=bank(1))
                P.add("dve", (lambda half: lambda e: e.tensor_copy(out=G1B[:, half * 512:(half + 1) * 512], in_=PB[1][:, :]))(half), r=bank(1), w=["G1B"])
            xw_t = xw.rearrange("(n p) d -> n p d", p=128)
            x1s_t = x1s.rearrange("(n p) d -> n p d", p=128)
            PBb0 = PB[0][:].bitcast(BF16)
            for s in (range(HALO0, NST) if do_b else []):
                own = s >= OWN0
                for u in range(2):
                    ti = 2 * s + u
                    xt = XT[u]; xs_ = XS[u]
                    P.add("sp", (lambda xt, ti: lambda e: e.dma_start(out=xt[:], in_=xw_t[ti]))(xt, ti), w=["XT%d" % u], dma=True)
                    P.add("act", (lambda xt, u: lambda e: e.activation(out=JNK[:], in_=xt[:], func=AF.Square, accum_out=SSC[:, u:u + 1]))(xt, u),
                          r=["XT%d" % u], w=["JNK", "SSC%d" % u])
                    P.add("act", (lambda u: lambda e: e.activation(out=RSC[:, u:u + 1], in_=SSC[:, u:u + 1], func=AF.Ln, bias=EPSC[:], scale=1.0 / D))(u),
                          r=["SSC%d" % u], w=["RSC%d" % u])
                    P.add("act", (lambda u: lambda e: e.activation(out=RSC[:, u:u + 1], in_=RSC[:, u:u + 1], func=AF.Exp, scale=-0.5))(u),
                          r=["RSC%d" % u], w=["RSC%d" % u])
                    P.add("dve", (lambda xt, xs_, u: lambda e: e.tensor_scalar(out=xs_[:], in0=xt[:], scalar1=RSC[:, u:u + 1], scalar2=None, op0=ALU.mult))(xt, xs_, u),
                          r=["XT%d" % u, "RSC%d" % u], w=["XS%d" % u])
                    P.add("pe", (lambda xs_: _grp([(lambda k: lambda e: e.transpose(PBb0[:, k * 128:(k + 1) * 128], xs_[:, k * 128:(k + 1) * 128], IDB[:]))(k)
                                                   for k in range(8)]))(xs_), r=["XS%d" % u], w=bank(0))
                    for k in range(8):
                        eng = "act"
                        if eng == "act":
                            f = (lambda k, u: lambda e: e.activation(out=HT[:, k, u * 128:(u + 1) * 128], in_=PBb0[:, k * 128:(k + 1) * 128],
                                                                     func=AF.Identity, bias=SH1C[:, k:k + 1], scale=A1C[:, k:k + 1]))(k, u)
                        else:
                            f = (lambda k, u: lambda e: e.tensor_scalar(out=HT[:, k, u * 128:(u + 1) * 128], in0=PBb0[:, k * 128:(k + 1) * 128],
                                                                        scalar1=A1C[:, k:k + 1], scalar2=SH1C[:, k:k + 1], op0=ALU.mult, op1=ALU.add))(k, u)
                        P.add(eng, f, r=bank(0), w=["HT%d_%d" % (k, u)])
                HTK = ["HT%d_%d" % (k, u) for k in range(8) for u in range(2)]
                slot0 = (2 * s) % 8
                for p in range(4):
                    P.add("pe", (lambda p: _grp([(lambda k: lambda e: e.matmul(PB[1][:, 0:ST], lhsT=WB[:, k, 512 + p * 128:512 + (p + 1) * 128], rhs=HT[:, k, :],
                                                                            start=(k == 0), stop=(k == 7)))(k) for k in range(8)]))(p),
                          r=HTK + ["WB"], w=qs(1, 0, 2))
                    P.add("act", (lambda p, slot0: lambda e: e.copy(out=KR[:, p, slot0 * 128:slot0 * 128 + ST], in_=PB[1][:, 0:ST]))(p, slot0),
                          r=qs(1, 0, 2), w=["KR%d_%d" % (p, slot0), "KR%d_%d" % (p, slot0 + 1)])
                    if own:
                        P.add("pe", (lambda p: _grp([(lambda k: lambda e: e.matmul(PB[1][:, ST:2 * ST], lhsT=WB[:, k, p * 128:(p + 1) * 128], rhs=HT[:, k, :],
                                                                                start=(k == 0), stop=(k == 7)))(k) for k in range(8)]))(p),
                              r=HTK + ["WB"], w=qs(1, 2, 2))
                        P.add("act", (lambda p: lambda e: e.activation(out=QB[:, p, :], in_=PB[1][:, ST:2 * ST], func=AF.Copy, scale=0.125))(p),
                              r=qs(1, 2, 2), w=["QB%d" % p])
                for u in range(2):
                    P.add("pe", (lambda u: _grp([(lambda k: lambda e: e.matmul(PB[2][:, :], lhsT=HT[:, k, u * 128:(u + 1) * 128], rhs=WB[:, k, 1024:1536],
                                                                            start=(k == 0), stop=(k == 7)))(k) for k in range(8)]))(u),
                          r=HTK + ["WB"], w=bank(2))
                    P.add("dve", (lambda u, slot0: lambda e: e.tensor_copy(out=VR[:, slot0 + u, :], in_=PB[2][:, :]))(u, slot0), r=bank(2), w=["VR%d" % (slot0 + u)])
                if not own:
                    continue
                for u in range(2):
                    qt_ = (s - OWN0) * 2 + u
                    W = 48 + qt_
                    slots = [(W - 4 + t) % 8 for t in range(5)]
                    ucs = slice(u * 128, (u + 1) * 128)
                    nh = max(0, 4 - qt_)
                    for hb in range(8):
                        p, r0 = hb // 2, (hb % 2) * 64
                        fns = []
                        for t in range(5):
                            o_ = PB[3][:, t * 128:(t + 1) * 128] if t < 4 else PB[4][:, 0:128]
                            fns.append((lambda o_, p, r0, sl, ucs: lambda e: e.matmul(o_, lhsT=KR[r0:r0 + 64, p, sl * 128:(sl + 1) * 128], rhs=QB[r0:r0 + 64, p, ucs],
                                                                                      start=True, stop=True))(o_, p, r0, slots[t], ucs))
                        P.add("pe", _grp(fns), r=["QB%d" % p] + ["KR%d_%d" % (p, sl) for sl in slots], w=bank(3) + qs(4, 0))
                        P.add("dve", (lambda hb: lambda e: e.tensor_tensor(out=STS[:, 0:512], in0=PB[3][:, :], in1=BM5[:, hb, 0:512], op=ALU.add))(hb),
                              r=bank(3) + ["BM5"], w=["STSa"])
                        P.add("dve", (lambda hb: lambda e: e.tensor_tensor(out=STS[:, 512:640], in0=PB[4][:, 0:128], in1=BM5[:, hb, 512:640], op=ALU.add))(hb),
                              r=qs(4, 0) + ["BM5"], w=["STSb"])
                        if nh > 0:
                            P.add("act", (lambda nh: lambda e: e.activation(out=PT[:, 0:nh * 128], in_=STS[:, 0:nh * 128], func=AF.Exp, bias=KM[:, 0:1]))(nh),
                                  r=["STSa", "KM"], w=["PTa"])
                        P.add("act", (lambda nh: lambda e: e.activation(out=PT[:, nh * 128:640], in_=STS[:, nh * 128:640], func=AF.Exp))(nh),
                              r=["STSa", "STSb"], w=["PTb"])
                        fns = [(lambda t, sl, hb: lambda e: e.matmul(PB[4][0:64, 128:256], lhsT=VR[:, sl, hb * 64:(hb + 1) * 64], rhs=PT[:, t * 128:(t + 1) * 128],
                                                                     start=(t == 0), stop=(t == 4)))(t, slots[t], hb) for t in range(5)]
                        P.add("pe", _grp(fns), r=["PTa", "PTb"] + ["VR%d" % sl for sl in slots], w=qs(4, 1))
                        fns = [(lambda t: lambda e: e.matmul(PB[4][0:64, 256:384], lhsT=ONB[:, 0:64], rhs=PT[:, t * 128:(t + 1) * 128],
                                                             start=(t == 0), stop=(t == 4)))(t) for t in range(5)]
                        P.add("pe", _grp(fns), r=["PTa", "PTb"], w=qs(4, 2))
                        P.add("dve", lambda e: e.reciprocal(out=RDEN[:], in_=PB[4][0:64, 256:384]), r=qs(4, 2), w=["RDEN"])
                        P.add("dve", (lambda p, r0, ucs: lambda e: e.tensor_tensor(out=OTB[r0:r0 + 64, p, ucs], in0=PB[4][0:64, 128:256], in1=RDEN[:], op=ALU.mult))(p, r0, ucs),
                              r=qs(4, 1) + ["RDEN"], w=["OTB%d_%d_%d" % (p, u, hb % 2)])
                    ocs = slice(qt_ * 128, (qt_ + 1) * 128)
                    for half in range(2):
                        fns = []
                        for c in range(8):
                            l_ = OTA[:, c, ocs] if c < 4 else OTB[:, c - 4, ucs]
                            fns.append((lambda c, l_, half: lambda e: e.matmul(PB[5 + half][:, :], lhsT=l_, rhs=WO[:, c, half * 512:(half + 1) * 512],
                                                                               start=(c == 0), stop=(c == 7)))(c, l_, half))
                        P.add("pe", _grp(fns), r=["WO"] + ["OTA%d_%d" % (h, qt_) for h in range(4)] + ["OTB%d_%d_%d" % (p, u, z) for p in range(4) for z in range(2)],
                              w=bank(5 + half))
                        P.add("act", (lambda half: lambda e: e.activation(out=JNK[:, half * 512:(half + 1) * 512], in_=PB[5 + half][:, :], func=AF.Square,
                                                                          accum_out=SSC[:, 2 + half:3 + half]))(half), r=bank(5 + half), w=["JNK", "SSC%d" % (2 + half)])
                    P.add("dve", lambda e: e.tensor_tensor(out=SSC[:, 4:5], in0=SSC[:, 2:3], in1=SSC[:, 3:4], op=ALU.add), r=["SSC2", "SSC3"], w=["SSC4"])
                    P.add("act", lambda e: e.activation(out=RSC[:, 4:5], in_=SSC[:, 4:5], func=AF.Ln, bias=EPSC[:], scale=1.0 / D), r=["SSC4"], w=["RSC4"])
                    P.add("act", lambda e: e.activation(out=RSC[:, 4:5], in_=RSC[:, 4:5], func=AF.Exp, scale=-0.5), r=["RSC4"], w=["RSC4"])
                    for half in range(2):
                        hs = slice(half * 512, (half + 1) * 512)
                        P.add("dve", (lambda half, hs: lambda e: e.scalar_tensor_tensor(out=T1[:, hs], in0=PB[5 + half][:, :], scalar=RSC[:, 4:5], in1=G1B[:, hs],
                                                                                        op0=ALU.mult, op1=ALU.mult))(half, hs),
                              r=bank(5 + half) + ["RSC4", "G1B"], w=["T1_%d" % half])
                    P.add("pool", (lambda u: lambda e: e.tensor_tensor(out=T1[:], in0=T1[:], in1=XT[u][:], op=ALU.add))(u), r=["T1_0", "T1_1", "XT%d" % u], w=["T1_0", "T1_1"])
                    P.add("sp", (lambda qt_: lambda e: e.dma_start(out=x1s_t[qt_], in_=T1[:]))(qt_), r=["T1_0", "T1_1"], w=["x1s"], dma=True)
                    if dbg:
                        P.add("sp", (lambda qt_: lambda e: e.dma_start(out=dbg_o["x1"].rearrange("(n p) d -> n p d", p=128)[qt_], in_=T1[:]))(qt_), r=["T1_0", "T1_1"], w=["dbg_x1"], dma=True)
                    P.add("act", lambda e: e.activation(out=JNK[:], in_=T1[:], func=AF.Square, accum_out=SSC[:, 5:6]), r=["T1_0", "T1_1"], w=["JNK", "SSC5"])
                    P.add("act", lambda e: e.activation(out=RSC[:, 5:6], in_=SSC[:, 5:6], func=AF.Ln, bias=EPSC[:], scale=1.0 / D), r=["SSC5"], w=["RSC5"])
                    P.add("act", lambda e: e.activation(out=RSC[:, 5:6], in_=RSC[:, 5:6], func=AF.Exp, scale=-0.5), r=["RSC5"], w=["RSC5"])
                    P.add("dve", lambda e: e.tensor_scalar(out=H2[:], in0=T1[:], scalar1=RSC[:, 5:6], scalar2=None, op0=ALU.mult), r=["T1_0", "T1_1", "RSC5"], w=["H2"])
                    for half in range(2):
                        P.add("pe", (lambda half: _grp([(lambda k: lambda e: e.transpose(PB[5 + half][:, (k % 4) * 128:(k % 4 + 1) * 128], H2[:, k * 128:(k + 1) * 128], IDF[:]))(k)
                                                        for k in range(half * 4, half * 4 + 4)]))(half), r=["H2"], w=bank(5 + half))
                        for k in range(half * 4, half * 4 + 4):
                            eng = "act"
                            src = PB[5 + half][:, (k % 4) * 128:(k % 4 + 1) * 128]
                            if eng == "act":
                                f = (lambda k, src: lambda e: e.activation(out=H2F[:, k, :], in_=src, func=AF.Identity, bias=SH2C[:, k:k + 1], scale=A2C[:, k:k + 1]))(k, src)
                            else:
                                f = (lambda k, src: lambda e: e.tensor_scalar(out=H2F[:, k, :], in0=src, scalar1=A2C[:, k:k + 1], scalar2=SH2C[:, k:k + 1], op0=ALU.mult, op1=ALU.add))(k, src)
                            P.add(eng, f, r=bank(5 + half), w=["H2F%d" % k])
                    H2FK = ["H2F%d" % k for k in range(8)]
                    P.add("pool", lambda e: e.tensor_copy(out=H2B[:], in_=H2F[:]), r=H2FK, w=["H2B"])
                    P.add("sp", (lambda ocs: lambda e: e.dma_start(out=h2s[:, :, ocs], in_=H2B[:]))(ocs), r=["H2B"], w=["h2s"], dma=True)
                    P.add("pe", _grp([(lambda k: lambda e: e.matmul(PB[7][:, 0:NE], lhsT=H2F[:, k, :], rhs=WRF[:, k, :], start=(k == 0), stop=(k == 7)))(k) for k in range(8)]),
                          r=H2FK + ["WRF"], w=qs(7, 0))
                    P.add("dve", (lambda qt_: lambda e: e.tensor_tensor(out=LG[:, qt_, :], in0=PB[7][:, 0:NE], in1=BR[:], op=ALU.add))(qt_), r=qs(7, 0) + ["BR"], w=["LG"])
            fin = ["x1s", "h2s"]
            if dbg and do_b:
                P.add("sp", lambda e: e.dma_start(out=dbg_o["lg"], in_=LG[:]), r=["LG"], w=["dbg_lg"], dma=True)
                fin += ["dbg_lg", "dbg_x1"]
            P.emit(st, final_wait_keys=fin)
            print("phase B ops:", len(P.ops))

        stAB.close()
        with ExitStack() as st:
            S = mk(st)
            P = Prog(nc)
            H2T = S("H2T", [128, 8, NTOK], BF16); Y = S("Y", [128, 16, D]); G = S("G", [128, 16, NE])
            WU = S("WU", [128, 8, 2 * D], BF16); WD = S("WD", [128, 8, D], BF16); ACTT = S("ACTT", [128, 8, 1024], BF16)
            BU = S("BU", [128, NE, 16]); BDN = S("BDN", [NE, D]); GT = S("GT", [NE, 128])
            TG = [S("TG%d" % i, [128, 512]) for i in range(2)]
            TSg = [S("TSg%d" % i, [128, 512], BF16) for i in range(2)]
            TL = [S("TL%d" % i, [128, 512]) for i in range(2)]
            G2B = S("G2B", [128, D]); X1T = S("X1T", [128, D]); JNK = S("JNKc", [128, D], BF16)
            T8 = S("T8", [128, 8]); MSK = S("MSK", [128, NE]); EX = S("EX", [128, NE]); CL = S("CL", [128, 8])
            P.add("sp", lambda e: e.dma_start(out=BU[:], in_=bu_d), w=["BU"], dma=True)
            P.add("dve", lambda e: e.tensor_scalar(out=BU[:, :, 8:16], in0=BU[:, :, 8:16], scalar1=1.0, scalar2=None, op0=ALU.add), r=["BU"], w=["BU"])
            P.add("sp", lambda e: e.dma_start(out=BDN[0:n_exp, :], in_=b_down), w=["BDN"], dma=True)
            for hh in range(2):
                P.add("sp", (lambda hh: lambda e: e.dma_start(out=H2T[:, :, hh * 1024:(hh + 1) * 1024], in_=h2s[:, :, hh * 1024:(hh + 1) * 1024]))(hh), w=["H2T"], dma=True)
            P.add("pool", lambda e: e.dma_start(out=WU[:], in_=w_up[0].rearrange("(k p) f -> p k f", p=128)), w=["WU"], dma=True)
            P.add("pool", lambda e: e.dma_start(out=WD[:], in_=w_down[0].rearrange("(k p) f -> p k f", p=128)), w=["WD"], dma=True)
            P.add("pool", lambda e: e.memset(Y[:], 0.0), w=["Y%d" % i for i in range(16)])
            for half in range(2):
                P.add("pe", (lambda half: lambda e: e.matmul(PB[1][:, :], lhsT=ONF[0:1, :], rhs=G2R[0:1, half * 512:(half + 1) * 512], start=True, stop=True))(half),
                      r=[], w=bank(1))
                P.add("dve", (lambda half: lambda e: e.tensor_copy(out=G2B[:, half * 512:(half + 1) * 512], in_=PB[1][:, :]))(half), r=bank(1), w=["G2B"])
            for tt in range(16):
                L_ = LG[:, tt, :]
                P.add("dve", (lambda L_: lambda e: e.max(out=T8[:], in_=L_))(L_), r=[], w=["T8"])
                P.add("dve", (lambda L_: lambda e: e.tensor_scalar(out=MSK[:], in0=L_, scalar1=T8[:, 3:4], scalar2=None, op0=ALU.is_ge))(L_), r=["T8"], w=["MSK"])
                P.add("dve", lambda e: e.tensor_scalar(out=CL[:, 0:1], in0=T8[:, 0:1], scalar1=-1.0, scalar2=None, op0=ALU.mult), r=["T8"], w=["CL0"])
                P.add("act", (lambda L_: lambda e: e.activation(out=EX[:], in_=L_, func=AF.Exp, bias=CL[:, 0:1]))(L_), r=["CL0"], w=["EX"])
                P.add("dve", lambda e: e.tensor_tensor(out=EX[:], in0=EX[:], in1=MSK[:], op=ALU.mult), r=["EX", "MSK"], w=["EX"])
                P.add("dve", lambda e: e.reduce_sum(out=CL[:, 1:2], in_=EX[:], axis=AX.X), r=["EX"], w=["CL1"])
                P.add("dve", lambda e: e.reciprocal(out=CL[:, 2:3], in_=CL[:, 1:2]), r=["CL1"], w=["CL2"])
                P.add("dve", (lambda tt: lambda e: e.tensor_scalar(out=G[:, tt, :], in0=EX[:], scalar1=CL[:, 2:3], scalar2=None, op0=ALU.mult))(tt), r=["EX", "CL2"], w=["G"])
            x1s_t = x1s.rearrange("(n p) d -> n p d", p=128)
            out_t = out.rearrange("(n p) d -> n p d", p=128)
            for ex in (range(n_exp) if do_c else []):
                for tg in range(4):
                    slot = tg % 2
                    tcs = slice(tg * 512, (tg + 1) * 512)
                    for fc in range(8):
                        pb = (fc % 2) * 2; ss = fc % 2
                        for z, fcc in ((0, fc), (1, fc + 8)):
                            P.add("pe", (lambda fcc, pbz, tcs: _grp([(lambda k: lambda e: e.matmul(PB[pbz][:, :], lhsT=WU[:, k, fcc * 128:(fcc + 1) * 128], rhs=H2T[:, k, tcs],
                                                                                               start=(k == 0), stop=(k == 7)))(k) for k in range(8)]))(fcc, pb + z, tcs),
                                  r=["WU", "H2T"], w=bank(pb + z))
                        P.add("dve", (lambda ex, fc, pb, ss: lambda e: e.tensor_scalar(out=TG[ss][:], in0=PB[pb][:, :], scalar1=BU[:, ex, fc:fc + 1], scalar2=7.0, op0=ALU.add, op1=ALU.min))(ex, fc, pb, ss),
                              r=bank(pb) + ["BU"], w=["TG%d" % ss])
                        P.add("act", (lambda ss: lambda e: e.activation(out=TSg[ss][:], in_=TG[ss][:], func=AF.Sigmoid, scale=1.702))(ss), r=["TG%d" % ss], w=["TSg%d" % ss])
                        P.add("act", (lambda ex, fc, pb, ss: lambda e: e.activation(out=TL[ss][:], in_=PB[pb + 1][:, :], func=AF.Identity, bias=BU[:, ex, 8 + fc:9 + fc]))(ex, fc, pb, ss),
                              r=bank(pb + 1) + ["BU"], w=["TL%d" % ss])
                        P.add("pool", (lambda ss: lambda e: e.tensor_scalar(out=TL[ss][:], in0=TL[ss][:], scalar1=8.0, scalar2=-6.0, op0=ALU.min, op1=ALU.max))(ss), r=["TL%d" % ss], w=["TL%d" % ss])
                        P.add("dve", (lambda ss: lambda e: e.tensor_tensor(out=TG[ss][:], in0=TG[ss][:], in1=TSg[ss][:], op=ALU.mult))(ss), r=["TG%d" % ss, "TSg%d" % ss], w=["TG%d" % ss])
                        P.add("pool", (lambda fc, ss, slot: lambda e: e.tensor_tensor(out=ACTT[:, fc, slot * 512:(slot + 1) * 512], in0=TG[ss][:], in1=TL[ss][:], op=ALU.mult))(fc, ss, slot),
                              r=["TG%d" % ss, "TL%d" % ss], w=["ACTT%d_%d" % (fc, slot)])
                    if tg == 3 and ex + 1 < n_exp:
                        P.add("pool", (lambda ex: lambda e: e.dma_start(out=WU[:], in_=w_up[ex + 1].rearrange("(k p) f -> p k f", p=128)))(ex), w=["WU"], dma=True)
                    for t4 in range(4):
                        tt = tg * 4 + t4
                        acs = slice(slot * 512 + t4 * 128, slot * 512 + (t4 + 1) * 128)
                        for half in range(2):
                            pb = 4 + (tt % 2) * 2 + half
                            fns = [(lambda fc, pb, acs, half: lambda e: e.matmul(PB[pb][:, :], lhsT=ACTT[:, fc, acs], rhs=WD[:, fc, half * 512:(half + 1) * 512],
                                                                                 start=(fc == 0), stop=(fc == 7)))(fc, pb, acs, half) for fc in range(8)]
                            P.add("pe", _grp(fns), r=["WD"] + ["ACTT%d_%d" % (fc, slot) for fc in range(8)], w=bank(pb))
                            P.add("dve", (lambda tt, half, pb, ex: lambda e: e.scalar_tensor_tensor(out=Y[:, tt, half * 512:(half + 1) * 512], in0=PB[pb][:, :],
                                                                                                  scalar=G[:, tt, ex:ex + 1], in1=Y[:, tt, half * 512:(half + 1) * 512],
                                                                                                  op0=ALU.mult, op1=ALU.add))(tt, half, pb, ex),
                                  r=bank(pb) + ["G", "Y%d" % tt], w=["Y%d" % tt])
                if ex + 1 < n_exp:
                    P.add("pool", (lambda ex: lambda e: e.dma_start(out=WD[:], in_=w_down[ex + 1].rearrange("(k p) f -> p k f", p=128)))(ex), w=["WD"], dma=True)
            for tt in (range(16) if do_c else []):
                P.add("pe", (lambda tt: lambda e: e.transpose(PB[2][0:NE, 0:128], G[:, tt, :], IDF[:]))(tt), r=["G", "IDF"], w=bank(2))
                P.add("dve", lambda e: e.tensor_copy(out=GT[:], in_=PB[2][0:NE, 0:128]), r=bank(2), w=["GT"])
                for half in range(2):
                    P.add("pe", (lambda half: lambda e: e.matmul(PB[half][:, :], lhsT=GT[0:n_exp, :], rhs=BDN[0:n_exp, half * 512:(half + 1) * 512], start=True, stop=True))(half),
                          r=["GT", "BDN"], w=bank(half))
                    P.add("dve", (lambda tt, half: lambda e: e.tensor_tensor(out=Y[:, tt, half * 512:(half + 1) * 512], in0=Y[:, tt, half * 512:(half + 1) * 512],
                                                                            in1=PB[half][:, :], op=ALU.add))(tt, half), r=bank(half) + ["Y%d" % tt], w=["Y%d" % tt])
                P.add("sp", (lambda tt: lambda e: e.dma_start(out=X1T[:], in_=x1s_t[tt]))(tt), w=["X1T"], dma=True)
                P.add("act", (lambda tt: lambda e: e.activation(out=JNK[:], in_=Y[:, tt, :], func=AF.Square, accum_out=CL[:, 4:5]))(tt), r=["Y%d" % tt], w=["JNK", "CL4"])
                P.add("act", lambda e: e.activation(out=CL[:, 5:6], in_=CL[:, 4:5], func=AF.Ln, bias=EPSC[:], scale=1.0 / D), r=["CL4"], w=["CL5"])
                P.add("act", lambda e: e.activation(out=CL[:, 5:6], in_=CL[:, 5:6], func=AF.Exp, scale=-0.5), r=["CL5"], w=["CL5"])
                P.add("dve", (lambda tt: lambda e: e.scalar_tensor_tensor(out=Y[:, tt, :], in0=Y[:, tt, :], scalar=CL[:, 5:6], in1=G2B[:], op0=ALU.mult, op1=ALU.mult))(tt),
                      r=["Y%d" % tt, "CL5", "G2B"], w=["Y%d" % tt])
                P.add("dve", (lambda tt: lambda e: e.tensor_tensor(out=X1T[:], in0=X1T[:], in1=Y[:, tt, :], op=ALU.add))(tt), r=["Y%d" % tt, "X1T"], w=["X1T"])
                P.add("sp", (lambda tt: lambda e: e.dma_start(out=out_t[tt], in_=X1T[:]))(tt), r=["X1T"], w=["out"], dma=True)
            P.emit(st, final_wait_keys=["out"] if do_c else [])
            print("phase C ops:", len(P.ops))
    return nc


def _host_inputs(inputs):
    x = np.ascontiguousarray(inputs["x"], dtype=np.float32)
    c = inputs["c"]
    rel = inputs["rel_bias"][0]
    kk = np.arange(128)[:, None, None]; t = np.arange(5)[None, :, None]; qq = np.arange(128)[None, None, :]
    diff = 128 * (4 - t) + qq - kk
    idx = np.clip(diff, -128, 128) + 128
    cd = 8 - 2 * t + qq // 64 - kk // 64
    valid = (cd >= 0) & (cd <= 8)
    bm5 = np.empty((128, 8, 640), np.float32)
    for hb in range(8):
        bm5[:, hb, :] = np.where(valid, rel[hb][idx], np.float32(NEG)).reshape(128, 640)
    ii = np.arange(128)[:, None]; jj = np.arange(128)[None, :]
    imask = np.zeros((128, 5, 128), np.float32)
    imask[:, 0, :] = (ii // 8 == jj // 8)
    for lv, sz in enumerate((8, 16, 32, 64), start=1):
        mk = (ii // (2 * sz) == jj // (2 * sz)) & ((ii % (2 * sz)) >= sz) & ((jj % (2 * sz)) < sz)
        imask[:, lv, :] = mk.T
    cw = np.ascontiguousarray(inputs["conv_w"][0].reshape(4, 12, 128).transpose(2, 1, 0))
    bu = np.ascontiguousarray(inputs["b_up"][0].reshape(NE, 16, 128).transpose(2, 0, 1))
    shared = {
        "w_ada": inputs["w_ada"][0], "b_ada": inputs["b_ada"][0].reshape(1, -1), "norm_w": inputs["norm_w"][0].reshape(1, -1),
        "w_in": inputs["w_in"][0], "cw": cw, "alog": np.ascontiguousarray(np.broadcast_to(inputs["a_log"][0], (128, 4))),
        "dtb": np.ascontiguousarray(np.broadcast_to(inputs["dt_bias"][0], (128, 4))), "anw": inputs["a_norm_w"][0].reshape(128, 1),
        "imask": imask, "bm5": bm5, "w_out": inputs["w_out"][0], "w_router": inputs["w_router"][0],
        "br": np.ascontiguousarray(np.broadcast_to(inputs["b_router"][0], (128, NE))), "w_up": inputs["w_up"][0], "bu": bu,
        "w_down": inputs["w_down"][0], "b_down": inputs["b_down"][0],
    }
    shared = {k: np.ascontiguousarray(v, dtype=np.float32) for k, v in shared.items()}
    maps = []
    for core in range(8):
        b, j = core // 4, core % 4
        xw = np.zeros((WIN, D), np.float32)
        n_real = NTOK * (j + 1)
        xw[WIN - n_real:] = x[b, :n_real]
        qv = np.zeros((128, 3), np.float32)
        for q in range(3):
            qv[:, q] = 1.0 if (j - 3 + q) >= 0 else 0.0
        km = np.full((128, 1), 0.0 if j >= 1 else NEG, np.float32)
        m = dict(shared)
        m.update({"xw": xw, "qvalid": qv, "kmask": km, "ccol": np.ascontiguousarray(c[b].reshape(8, 128).T, dtype=np.float32)})
        maps.append(m)
    return maps


_NC_CACHE = {}


def kernel(**inputs):
    maps = _host_inputs(inputs)
    if "nc" not in _NC_CACHE:
        _NC_CACHE["nc"] = build()
    nc = _NC_CACHE["nc"]
    res = run_bass_kernel_spmd(nc, maps, core_ids=list(range(8)))
    full = np.empty((2, 8192, D), np.float32)
    for core in range(8):
        b, j = core // 4, core % 4
        full[b, j * NTOK:(j + 1) * NTOK] = res.results[core]["out"]
    return full
```

```python
import numpy as np
from contextlib import ExitStack
import concourse.bass as bass
import concourse.mybir as mybir
from concourse.bass_utils import run_bass_kernel_spmd

F32 = mybir.dt.float32
BF16 = mybir.dt.bfloat16
AF = mybir.ActivationFunctionType
ALU = mybir.AluOpType
AX = mybir.AxisListType

ENGS = ("pe", "act", "dve", "pool", "sp")
DS = 1
NEG = -30000.0
EPS = 1e-6


class Prog:
    def __init__(self, nc, n_dma_sems=6):
        self.nc = nc
        self.ops = []
        self.last_w = {}
        self.readers = {}
        self.n_dma_sems = n_dma_sems

    def add(self, eng, fn, r=(), w=(), dma=False, after_all=False):
        banks = set()
        for k in list(r) + list(w):
            if k[0] == "q" and "_" in k and k[1:k.index("_")].isdigit():
                banks.add("BK" + k[1:k.index("_")])
        deps = set()
        for k in r:
            if k in self.last_w:
                deps.add(self.last_w[k])
        for k in w:
            if k in self.last_w:
                deps.add(self.last_w[k])
            deps.update(self.readers.get(k, ()))
        tdeps = set()
        for k in banks:
            if k in self.last_w and self.last_w[k] not in deps:
                tdeps.add(self.last_w[k])
        deps |= tdeps
        w = list(w) + sorted(banks)
        idx = len(self.ops)
        if after_all:
            deps = set(range(idx))
        self.ops.append(dict(eng=eng, fn=fn, deps=deps, dma=dma, sig=False, tdeps=tdeps))
        for k in r:
            self.readers.setdefault(k, []).append(idx)
        for k in w:
            self.last_w[k] = idx
            self.readers[k] = []
        return idx

    def emit(self, stack, final_wait_keys=()):
        nc = self.nc
        ops = self.ops
        final_deps = set()
        for k in final_wait_keys:
            if k in self.last_w:
                final_deps.add(self.last_w[k])
        for o in ops:
            for d in o["deps"]:
                ops[d]["sig"] = True
        for d in final_deps:
            ops[d]["sig"] = True
        esem = {e: stack.enter_context(nc.semaphore("s_" + e)) for e in ENGS}
        dsem = {e: [stack.enter_context(nc.semaphore("d_%s%d" % (e, i))) for i in range(self.n_dma_sems)]
                for e in ENGS if e != "pe"}
        ecount = {e: 0 for e in ENGS}
        dcount = {e: [0] * self.n_dma_sems for e in dsem}
        drr = {e: 0 for e in dsem}
        for o in ops:
            e = o["eng"]
            if o["dma"]:
                j = drr[e]
                drr[e] = (j + 1) % self.n_dma_sems
                o["prev_on_sem"] = (dsem[e][j], dcount[e][j]) if dcount[e][j] else None
                dcount[e][j] += 16
                o["sem"] = dsem[e][j]
                o["val"] = dcount[e][j]
            elif o["sig"]:
                ecount[e] += 1
                o["sem"] = esem[e]
                o["val"] = ecount[e]
        per_eng = {e: [] for e in ENGS}
        for i, o in enumerate(ops):
            per_eng[o["eng"]].append(i)
        block = stack.enter_context(nc.Block())

        def run(e, eng):
            waited = {}

            def wait(sem, val):
                key = id(sem)
                if waited.get(key, 0) >= val:
                    return
                waited[key] = val
                eng.wait_ge(sem, val)

            for i in per_eng[e]:
                o = ops[i]
                for d in sorted(o["deps"]):
                    p = ops[d]
                    if p["eng"] == "pe" and e == "pe" and not p["dma"] and not o["dma"]:
                        continue
                    if d in o["tdeps"] and p["eng"] == e and not p["dma"] and not o["dma"]:
                        continue
                    wait(p["sem"], p["val"])
                if o["dma"] and o["prev_on_sem"] is not None:
                    wait(*o["prev_on_sem"])
                ins = o["fn"](eng)
                if o["dma"]:
                    ins.then_inc(o["sem"], 16)
                elif o["sig"]:
                    ins.then_inc(o["sem"], 1)
            if e == "sp":
                for d in sorted(final_deps):
                    wait(ops[d]["sem"], ops[d]["val"])

        @block.tensor
        def _(eng):
            run("pe", eng)

        @block.scalar
        def _(eng):
            run("act", eng)

        @block.vector
        def _(eng):
            run("dve", eng)

        @block.gpsimd
        def _(eng):
            run("pool", eng)

        @block.sync
        def _(eng):
            run("sp", eng)


D = 1024
NTOK = 2048
WIN = 8192
ST = 256
NST = WIN // ST
OWN0 = (WIN - NTOK) // ST
HALO0 = OWN0 - 2
C_Q, C_K, C_V, C_Z, C_AB, C_QB, C_KB, C_VB = 0, 512, 1024, 1536, 2048, 2056, 2568, 3080
NE = 32


def _grp(fns):
    def f(e):
        ins = None
        for g in fns:
            ins = g(e)
        return ins
    return f


def build(n_exp=NE, dbg=False, a_list=None, do_b=True, do_c=True, stop=99):
    a_list = list(range(NST)) if a_list is None else a_list
    nc = bass.Bass("TRN2", target_bir_lowering=False)
    DI = lambda name, shape, dt=F32: nc.dram_tensor(name, shape, dt, kind="ExternalInput").ap()
    xw = DI("xw", [WIN, D]); qvalid_d = DI("qvalid", [128, 3]); kmask_d = DI("kmask", [128, 1])
    ccol_d = DI("ccol", [128, 8]); w_ada = DI("w_ada", [D, 6 * D]); b_ada = DI("b_ada", [1, 6 * D])
    norm_w = DI("norm_w", [1, 4 * D]); w_in = DI("w_in", [D, 3592]); cw_d = DI("cw", [128, 12, 4])
    imk_d = DI("imask", [128, 5, 128]); alog_d = DI("alog", [128, 4]); dtb_d = DI("dtb", [128, 4]); anw_d = DI("anw", [128, 1])
    bm5_d = DI("bm5", [128, 8, 640]); w_out = DI("w_out", [D, D]); w_router = DI("w_router", [D, NE])
    br_d = DI("br", [128, NE]); w_up = DI("w_up", [n_exp, D, 2 * D]); bu_d = DI("bu", [128, NE, 16])
    w_down = DI("w_down", [n_exp, D, D]); b_down = DI("b_down", [n_exp, D])
    out = nc.dram_tensor("out", [NTOK, D], F32, kind="ExternalOutput").ap()
    x1s = nc.dram_tensor("x1s", [NTOK, D], F32).ap()
    h2s = nc.dram_tensor("h2s", [128, 8, NTOK], BF16).ap()
    dbg_o = {}
    if dbg:
        dbg_o["ota"] = nc.dram_tensor("dbg_ota", [128, 4, NTOK], F32, kind="ExternalOutput").ap()
        dbg_o["otb"] = nc.dram_tensor("dbg_otb", [128, 4, NTOK], F32, kind="ExternalOutput").ap()
        dbg_o["x1"] = nc.dram_tensor("dbg_x1", [NTOK, D], F32, kind="ExternalOutput").ap()
        dbg_o["lg"] = nc.dram_tensor("dbg_lg", [128, 16, NE], F32, kind="ExternalOutput").ap()

    with ExitStack() as st0:
        def mk(st):
            return lambda name, shape, dt=F32: st.enter_context(nc.sbuf_tensor(name, shape, dt))
        S0 = mk(st0)
        IDF = S0("IDF", [128, 128]); IDB = S0("IDB", [128, 128], BF16)
        ONF = S0("ONF", [128, 128]); ONB = S0("ONB", [128, 128], BF16)
        LG = S0("LG", [128, 16, NE])
        G2R = S0("G2R", [1, D])
        A1C = S0("A1C", [128, 8]); SH1C = S0("SH1C", [128, 8]); A2C = S0("A2C", [128, 8]); SH2C = S0("SH2C", [128, 8])
        EPSC = S0("EPSC", [128, 1])
        PB = [st0.enter_context(nc.psum_tensor("pb%d" % i, [128, 512], F32)) for i in range(8)]
        stAB = ExitStack()
        SAB = mk(stAB)
        OTA = SAB("OTA", [128, 4, NTOK], BF16)
        G1R = SAB("G1R", [1, D])

        def bank(b):
            return ["q%d_%d" % (b, i) for i in range(4)]

        def qs(b, q0, n=1):
            return ["q%d_%d" % (b, i) for i in range(q0, q0 + n)]

        with ExitStack() as st:
            S = mk(st)
            P = Prog(nc)
            UF = S("UF", [128, 128]); NM2 = S("NM2", [128, 128]); NMT = S("NMT", [128, 128]); IMK = S("IMK", [128, 5, 128])
            WA = S("WA", [128, 8, 2056], BF16)
            QV = S("QV", [128, 3]); CW = S("CW", [128, 12, 4]); NEGA = S("NEGA", [128, 4]); DTB = S("DTB", [128, 4])
            ANW = S("ANW", [128, 1]); CC = S("CC", [128, 8]); SC = S("SC", [128, 8])
            WST = S("WST", [128, 8, 512])
            BST = S("BST", [1, 512]); NWT = S("NWT", [1, 512]); ROWT = S("ROWT", [1, 512])
            P.add("pool", lambda e: e.memset(IDF[:], 0.0), w=["IDF"])
            P.add("pool", lambda e: e.affine_select(out=IDF[:], in_=IDF[:], pattern=[[-1, 128]], compare_op=ALU.not_equal,
                                                     fill=1.0, base=0, channel_multiplier=1), r=["IDF"], w=["IDF"])
            P.add("pool", lambda e: e.tensor_copy(out=IDB[:], in_=IDF[:]), r=["IDF"], w=["IDB"])
            P.add("pool", lambda e: e.memset(ONF[:], 1.0), w=["ONF"])
            P.add("pool", lambda e: e.memset(ONB[:], 1.0), w=["ONB"])
            P.add("pool", lambda e: e.memset(EPSC[:], EPS), w=["EPSC"])
            P.add("pool", lambda e: e.memset(UF[:], 1.0), w=["UF"])
            P.add("pool", lambda e: e.affine_select(out=UF[:], in_=UF[:], pattern=[[1, 128]], compare_op=ALU.is_ge,
                                                     fill=0.0, base=0, channel_multiplier=-1), r=["UF"], w=["UF"])
            P.add("pool", lambda e: e.memset(NM2[:], -NEG), w=["NM2"])
            P.add("pool", lambda e: e.affine_select(out=NM2[:], in_=NM2[:], pattern=[[1, 128]], compare_op=ALU.is_ge,
                                                     fill=0.0, base=0, channel_multiplier=-1), r=["NM2"], w=["NM2"])
            P.add("pool", lambda e: e.memset(NMT[:], NEG), w=["NMT"])
            P.add("pool", lambda e: e.affine_select(out=NMT[:], in_=NMT[:], pattern=[[-1, 128]], compare_op=ALU.is_gt,
                                                     fill=0.0, base=0, channel_multiplier=1), r=["NMT"], w=["NMT"])
            for dst, src, key in ((IMK, imk_d, "IMK"), (QV, qvalid_d, "QV"), (CW, cw_d, "CW"), (NEGA, alog_d, "NEGA"), (DTB, dtb_d, "DTB"),
                                  (ANW, anw_d, "ANW"), (CC, ccol_d, "CC")):
                P.add("sp", (lambda dst, src: lambda e: e.dma_start(out=dst[:], in_=src))(dst, src), w=[key], dma=True)
            w_in_r = w_in.rearrange("(k p) c -> p k c", p=128)
            P.add("pool", lambda e: e.dma_start(out=WA[:, :, 0:2048], in_=w_in_r[:, :, 0:2048]), w=["WA0"], dma=True)
            P.add("pool", lambda e: e.dma_start(out=WA[:, :, 2048:2056], in_=w_in_r[:, :, 2048:2056]), w=["WA1"], dma=True)
            P.add("act", lambda e: e.activation(out=NEGA[:], in_=NEGA[:], func=AF.Exp), r=["NEGA"], w=["NEGA"])
            P.add("dve", lambda e: e.tensor_scalar(out=NEGA[:], in0=NEGA[:], scalar1=-1.0, scalar2=None, op0=ALU.mult), r=["NEGA"], w=["NEGA"])
            P.add("act", lambda e: e.activation(out=SC[:], in_=CC[:], func=AF.Silu), r=["CC"], w=["SC"])
            w_ada_r = w_ada.rearrange("(k p) n -> p k n", p=128)
            for nb in range(12):
                kind, half = nb // 2, nb % 2
                P.add("sp", (lambda nb: lambda e: e.dma_start(out=WST[:], in_=w_ada_r[:, :, nb * 512:(nb + 1) * 512]))(nb), w=["WST"], dma=True)
                P.add("sp", (lambda nb: lambda e: e.dma_start(out=BST[:], in_=b_ada[0:1, nb * 512:(nb + 1) * 512]))(nb), w=["BST"], dma=True)
                if kind in (1, 2, 4, 5):
                    nwi = {1: 0, 2: 1, 4: 2, 5: 3}[kind]
                    P.add("sp", (lambda o_: lambda e: e.dma_start(out=NWT[:], in_=norm_w[0:1, o_:o_ + 512]))(nwi * D + half * 512), w=["NWT"], dma=True)
                P.add("pe", _grp([(lambda k: lambda e: e.matmul(PB[0][0:1, :], lhsT=SC[:, k:k + 1], rhs=WST[:, k, :], start=(k == 0), stop=(k == 7)))(k) for k in range(8)]),
                      r=["SC", "WST"], w=bank(0))
                P.add("dve", lambda e: e.tensor_tensor(out=ROWT[:], in0=PB[0][0:1, :], in1=BST[:], op=ALU.add), r=bank(0) + ["BST"], w=["ROWT"])
                if kind in (1, 4):
                    P.add("dve", lambda e: e.scalar_tensor_tensor(out=ROWT[:], in0=ROWT[:], scalar=1.0, in1=NWT[:], op0=ALU.add, op1=ALU.mult), r=["ROWT", "NWT"], w=["ROWT"])
                elif kind in (2, 5):
                    P.add("dve", lambda e: e.tensor_tensor(out=ROWT[:], in0=ROWT[:], in1=NWT[:], op=ALU.mult), r=["ROWT", "NWT"], w=["ROWT"])
                if kind in (0, 1, 3, 4):
                    dst, key = {0: (SH1C, "SH1C"), 1: (A1C, "A1C"), 3: (SH2C, "SH2C"), 4: (A2C, "A2C")}[kind]
                    P.add("pe", _grp([(lambda k: lambda e: e.matmul(PB[1][:, k:k + 1], lhsT=ROWT[0:1, k * 128:(k + 1) * 128], rhs=ONF[0:1, 0:1], start=True, stop=True))(k)
                                      for k in range(4)]), r=["ROWT", "ONF"], w=bank(1))
                    P.add("dve", (lambda dst, half: lambda e: e.tensor_copy(out=dst[:, half * 4:half * 4 + 4], in_=PB[1][:, 0:4]))(dst, half), r=bank(1), w=[key])
                else:
                    dst, key = (G1R, "G1R") if kind == 2 else (G2R, "G2R")
                    P.add("dve", (lambda dst, half: lambda e: e.tensor_copy(out=dst[0:1, half * 512:(half + 1) * 512], in_=ROWT[:]))(dst, half), r=["ROWT"], w=[key])

            XT = [S("XT%d" % i, [128, D]) for i in range(2)]
            JNK = S("JNK", [128, D], BF16)
            XS = [S("XS%d" % i, [128, D], BF16) for i in range(2)]
            HT = S("HT", [128, 8, ST], BF16)
            RAW = S("RAW", [128, 12, ST + 3], BF16)
            CV = S("CV", [128, 12, ST])
            SIL = S("SIL", [128, 12, ST], BF16)
            SQ = S("SQ", [128, 8, ST], BF16)
            LNT = S("LNT", [128, 8, ST])
            QKVL = [S("QKV%d" % i, [128, 12, ST], BF16) for i in range(2)]
            ZSL = [S("ZS%d" % i, [128, 4, ST], BF16) for i in range(2)]
            ABTL = [S("ABT%d" % i, [128, 2, 8]) for i in range(2)]
            GSTL = [S("GST%d" % i, [128, 2, 4]) for i in range(2)]; BETL = [S("BET%d" % i, [128, 2, 4]) for i in range(2)]
            NBETL = [S("NBET%d" % i, [128, 2, 4]) for i in range(2)]
            SSC = S("SSC", [128, 4]); RSC = S("RSC", [128, 4])
            Sm = [S("Sm%d" % h, [128, 128]) for h in range(4)]
            Sb = [S("Sb%d" % h, [128, 128], BF16) for h in range(4)]
            NSET = 4
            def tset(i):
                t = {}
                for nm in ("gsb", "decS", "decT", "egb", "o1"):
                    t[nm] = S("%s_%d" % (nm, i), [128, 128])
                for nm in ("A", "AT", "P0", "PT0", "P1", "PT1", "RT", "TM", "BKm", "Ym", "kbg", "kdec", "vb", "nwT", "vnew", "attnT", "qdT", "sq"):
                    t[nm] = S("%s_%d" % (nm, i), [128, 128], BF16)
                t["gc"] = S("gc_%d" % i, [128, 8])
                return t
            TS = [tset(i) for i in range(NSET)]
            P.add("pool", lambda e: e.memset(RAW[:], 0.0), w=["RAW%d" % c for c in range(12)])
            for h in range(4):
                P.add("pool", (lambda h: lambda e: e.memset(Sm[h][:], 0.0))(h), w=["Sm%d" % h])
                P.add("pool", (lambda h: lambda e: e.memset(Sb[h][:], 0.0))(h), w=["Sb%d" % h])

            xw_t = xw.rearrange("(n p) d -> n p d", p=128)
            PBb0 = PB[0][:].bitcast(BF16)
            PBbH = [PB[4 + h][:].bitcast(BF16) for h in range(4)]

            def do_st(s):
                pending = []
                FA = lambda *a_, **k_: pending.append((a_, k_))
                own = s >= OWN0
                q = s // 8
                par = s % 2; kp = "p%d" % par
                QKVc, ZSc, ABTc, GSTc, BETc, NBETc = QKVL[par], ZSL[par], ABTL[par], GSTL[par], BETL[par], NBETL[par]
                for u in range(2):
                    ti = 2 * s + u
                    xt = XT[u]; xs_ = XS[u]
                    FA("sp", (lambda xt, ti: lambda e: e.dma_start(out=xt[:], in_=xw_t[ti]))(xt, ti), w=["XT%d" % u], dma=True)
                    if stop <= 0.25:
                        continue
                    FA("act", (lambda xt, u: lambda e: e.activation(out=JNK[:], in_=xt[:], func=AF.Square, accum_out=SSC[:, u:u + 1]))(xt, u),
                          r=["XT%d" % u], w=["JNK", "SSC%d" % u])
                    FA("act", (lambda u: lambda e: e.activation(out=RSC[:, u:u + 1], in_=SSC[:, u:u + 1], func=AF.Ln, bias=EPSC[:], scale=1.0 / D))(u),
                          r=["SSC%d" % u, "EPSC"], w=["RSC%d" % u])
                    FA("act", (lambda u: lambda e: e.activation(out=RSC[:, u:u + 1], in_=RSC[:, u:u + 1], func=AF.Exp, scale=-0.5))(u),
                          r=["RSC%d" % u], w=["RSC%d" % u])
                    FA("dve", (lambda xt, xs_, u: lambda e: e.tensor_scalar(out=xs_[:], in0=xt[:], scalar1=RSC[:, u:u + 1], scalar2=None, op0=ALU.mult))(xt, xs_, u),
                          r=["XT%d" % u, "RSC%d" % u], w=["XS%d" % u])
                    if stop <= 0.5:
                        continue
                    FA("pe", (lambda xs_: _grp([(lambda k: lambda e: e.transpose(PBb0[:, k * 128:(k + 1) * 128], xs_[:, k * 128:(k + 1) * 128], IDB[:]))(k)
                                                   for k in range(8)]))(xs_), r=["XS%d" % u, "IDB"], w=bank(0))
                    for k in range(8):
                        eng = "act"
                        if eng == "act":
                            f = (lambda k, u: lambda e: e.activation(out=HT[:, k, u * 128:(u + 1) * 128], in_=PBb0[:, k * 128:(k + 1) * 128],
                                                                     func=AF.Identity, bias=SH1C[:, k:k + 1], scale=A1C[:, k:k + 1]))(k, u)
                        else:
                            f = (lambda k, u: lambda e: e.tensor_scalar(out=HT[:, k, u * 128:(u + 1) * 128], in0=PBb0[:, k * 128:(k + 1) * 128],
                                                                        scalar1=A1C[:, k:k + 1], scalar2=SH1C[:, k:k + 1], op0=ALU.mult, op1=ALU.add))(k, u)
                        FA(eng, f, r=bank(0) + ["A1C", "SH1C"], w=["HT%d_%d" % (k, u)])
                HTK = ["HT%d_%d" % (k, u) for k in range(8) for u in range(2)]
                if stop <= 1:
                    return pending, None
                chunks = list(range(4, 12)) + (list(range(0, 4)) + list(range(12, 16)) if own else ([0, 1, 2, 3] if s == OWN0 - 1 else []))
                for ci, c in enumerate(chunks):
                    slot_b, slot_q = 1 + (ci % 2), 0
                    pso = PB[slot_b][:, 0:ST]
                    FA("pe", (lambda c, pso: _grp([(lambda k: lambda e: e.matmul(pso, lhsT=WA[:, k, c * 128:(c + 1) * 128], rhs=HT[:, k, :],
                                                                                start=(k == 0), stop=(k == 7)))(k) for k in range(8)]))(c, pso),
                          r=HTK + ["WA0"], w=qs(slot_b, 0, 2))
                    if c < 12:
                        dst = RAW[:, c, 3:3 + ST]
                        if own:
                            FA("act", (lambda dst, pso: lambda e: e.copy(out=dst, in_=pso))(dst, pso), r=qs(slot_b, 0, 2), w=["RAW%d" % c])
                        else:
                            FA("dve", (lambda dst, pso, q: lambda e: e.tensor_scalar(out=dst, in0=pso, scalar1=QV[:, q:q + 1], scalar2=None, op0=ALU.mult))(dst, pso, q),
                                  r=qs(slot_b, 0, 2) + ["QV"], w=["RAW%d" % c])
                    else:
                        FA("act", (lambda c, pso: lambda e: e.activation(out=ZSc[:, c - 12, :], in_=pso, func=AF.Silu))(c, pso),
                              r=qs(slot_b, 0, 2), w=["ZS%d" % (c - 12) + kp])
                if stop <= 2:
                    return pending, None
                for u in range(2):
                    FA("pe", (lambda u: _grp([(lambda k: lambda e: e.matmul(PB[3][:, 0:8], lhsT=HT[:, k, u * 128:(u + 1) * 128], rhs=WA[:, k, 2048:2056],
                                                                            start=(k == 0), stop=(k == 7)))(k) for k in range(8)]))(u),
                          r=HTK + ["WA1"], w=qs(3, 0))
                    if own:
                        FA("dve", (lambda u: lambda e: e.tensor_copy(out=ABTc[:, u, :], in_=PB[3][:, 0:8]))(u), r=qs(3, 0), w=["ABT%d" % u + kp])
                    else:
                        FA("dve", (lambda u, q: lambda e: e.tensor_scalar(out=ABTc[:, u, :], in0=PB[3][:, 0:8], scalar1=QV[:, q:q + 1], scalar2=None, op0=ALU.mult))(u, q),
                              r=qs(3, 0) + ["QV"], w=["ABT%d" % u + kp])
                    FA("dve", (lambda u: lambda e: e.tensor_tensor(out=GSTc[:, u, :], in0=ABTc[:, u, 0:4], in1=DTB[:], op=ALU.add))(u), r=["ABT%d" % u + kp, "DTB"], w=["GST%d" % u + kp])
                    FA("act", (lambda u: lambda e: e.activation(out=GSTc[:, u, :], in_=GSTc[:, u, :], func=AF.Exp))(u), r=["GST%d" % u + kp], w=["GST%d" % u + kp])
                    FA("act", (lambda u: lambda e: e.activation(out=GSTc[:, u, :], in_=GSTc[:, u, :], func=AF.Ln, bias=ONF[:, 0:1]))(u), r=["GST%d" % u + kp, "ONF"], w=["GST%d" % u + kp])
                    FA("dve", (lambda u: lambda e: e.tensor_tensor(out=GSTc[:, u, :], in0=GSTc[:, u, :], in1=NEGA[:], op=ALU.mult))(u), r=["GST%d" % u + kp, "NEGA"], w=["GST%d" % u + kp])
                    FA("act", (lambda u: lambda e: e.activation(out=BETc[:, u, :], in_=ABTc[:, u, 4:8], func=AF.Sigmoid))(u), r=["ABT%d" % u + kp], w=["BET%d" % u + kp])
                    FA("dve", (lambda u: lambda e: e.tensor_scalar(out=NBETc[:, u, :], in0=BETc[:, u, :], scalar1=-1.0, scalar2=None, op0=ALU.mult))(u), r=["BET%d" % u + kp], w=["NBET%d" % u + kp])
                if stop <= 3:
                    return pending, None
                for c in chunks:
                    if c >= 12:
                        continue
                    rk = "RAW%d" % c
                    FA("pool", (lambda c: lambda e: e.tensor_scalar(out=CV[:, c, :], in0=RAW[:, c, 0:ST], scalar1=CW[:, c, 0:1], scalar2=None, op0=ALU.mult))(c),
                          r=[rk, "CW"], w=["CV%d" % c])
                    for tp in range(1, 4):
                        FA("dve", (lambda c, tp: lambda e: e.scalar_tensor_tensor(out=CV[:, c, :], in0=RAW[:, c, tp:tp + ST], scalar=CW[:, c, tp:tp + 1],
                                                                                      in1=CV[:, c, :], op0=ALU.mult, op1=ALU.add))(c, tp),
                              r=[rk, "CW", "CV%d" % c], w=["CV%d" % c])
                    FA("pool", (lambda c: lambda e: e.tensor_copy(out=RAW[:, c, 0:3], in_=RAW[:, c, ST:ST + 3]))(c), r=[rk], w=[rk])
                    if c >= 8:
                        FA("act", (lambda c: lambda e: e.activation(out=QKVc[:, c, :], in_=CV[:, c, :], func=AF.Silu))(c), r=["CV%d" % c], w=["QKV%d" % c + kp])
                    else:
                        FA("act", (lambda c: lambda e: e.activation(out=SIL[:, c, :], in_=CV[:, c, :], func=AF.Silu))(c), r=["CV%d" % c], w=["SIL%d" % c])
                        FA("act", (lambda c: lambda e: e.activation(out=SQ[:, c, :], in_=SIL[:, c, :], func=AF.Square))(c), r=["SIL%d" % c], w=["SQ%d" % c])
                        FA("pe", (lambda c: lambda e: e.matmul(PB[3][:, ST:2 * ST], lhsT=ONB[:], rhs=SQ[:, c, :], start=True, stop=True))(c),
                              r=["SQ%d" % c, "ONB"], w=qs(3, 2, 2))
                        FA("act", (lambda c: lambda e: e.activation(out=LNT[:, c, :], in_=PB[3][:, ST:2 * ST], func=AF.Ln, bias=EPSC[:]))(c),
                              r=qs(3, 2, 2) + ["EPSC"], w=["LNT%d" % c])
                        FA("act", (lambda c: lambda e: e.activation(out=LNT[:, c, :], in_=LNT[:, c, :], func=AF.Exp, scale=-0.5))(c), r=["LNT%d" % c], w=["LNT%d" % c])
                        sc_ = (128.0 ** -0.5) if c < 4 else 1.0
                        FA("dve", (lambda c, sc_: lambda e: e.scalar_tensor_tensor(out=QKVc[:, c, :], in0=SIL[:, c, :], scalar=sc_, in1=LNT[:, c, :],
                                                                                      op0=ALU.mult, op1=ALU.mult))(c, sc_),
                              r=["SIL%d" % c, "LNT%d" % c], w=["QKV%d" % c + kp])
                if stop <= 4:
                    return pending, None
                def chain(u, h):
                    cs = slice(u * 128, (u + 1) * 128)
                    t = TS[h]; tk = "_%d" % h
                    K = lambda n: n + tk
                    hb = 4 + h; Hf = PB[hb]; H16 = PBbH[h]
                    Q = lambda q_: Hf[:, q_ * 128:(q_ + 1) * 128]
                    QK = lambda q_: qs(hb, q_)
                    kT = QKVc[:, 4 + h, cs]; vT = QKVc[:, 8 + h, cs]; qT = QKVc[:, h, cs]
                    kk, vk, qk = "QKV%d" % (4 + h) + kp, "QKV%d" % (8 + h) + kp, "QKV%d" % h + kp
                    gcol = GSTc[:, u, h:h + 1]
                    gk, bk_, nbk = "GST%d" % u + kp, "BET%d" % u + kp, "NBET%d" % u + kp
                    gc = t["gc"]
                    BETl, NBETl, ZSl = BETc, NBETc, ZSc
                    P.add("dve", lambda e: e.tensor_scalar(out=t["gsb"][:], in0=ONF[:], scalar1=gcol, scalar2=None, op0=ALU.mult), r=["ONF", gk], w=[K("gsb")])
                    yield
                    P.add("pe", _grp([lambda e: e.matmul(Q(0), lhsT=t["gsb"][:], rhs=UF[:], start=True, stop=True),
                                      lambda e: e.matmul(Hf[:, 128:129], lhsT=UF[:], rhs=gcol, start=True, stop=True),
                                      lambda e: e.matmul(Hf[:, 129:130], lhsT=ONF[:], rhs=gcol, start=True, stop=True)]),
                          r=[K("gsb"), "UF", "ONF", gk], w=QK(0) + QK(1))
                    P.add("dve", lambda e: e.tensor_copy(out=gc[:, 0:2], in_=Hf[:, 128:130]), r=QK(1), w=[K("gc")])
                    if own:
                        P.add("act", lambda e: e.activation(out=t["egb"][:], in_=Q(0), func=AF.Exp), r=QK(0), w=[K("egb")])
                    yield
                    P.add("dve", lambda e: e.tensor_scalar(out=gc[:, 2:3], in0=gc[:, 0:1], scalar1=-1.0, scalar2=None, op0=ALU.mult), r=[K("gc")], w=[K("gc2")])
                    P.add("dve", lambda e: e.tensor_tensor(out=gc[:, 3:4], in0=gc[:, 1:2], in1=gc[:, 0:1], op=ALU.subtract), r=[K("gc")], w=[K("gc3")])
                    P.add("act", lambda e: e.activation(out=gc[:, 4:5], in_=gc[:, 0:1], func=AF.Exp), r=[K("gc")], w=[K("gc4")])
                    P.add("act", lambda e: e.activation(out=gc[:, 6:7], in_=gc[:, 1:2], func=AF.Exp), r=[K("gc")], w=[K("gc6")])
                    yield
                    P.add("dve", lambda e: e.tensor_tensor(out=gc[:, 4:5], in0=gc[:, 4:5], in1=BETl[:, u, h:h + 1], op=ALU.mult), r=[K("gc4"), bk_], w=[K("gc4")])
                    P.add("act", lambda e: e.activation(out=gc[:, 5:6], in_=gc[:, 3:4], func=AF.Exp), r=[K("gc3")], w=[K("gc5")])
                    P.add("pe", _grp([lambda e: e.matmul(Q(2), lhsT=t["gsb"][:], rhs=UF[:], start=True, stop=False),
                                      lambda e: e.matmul(Q(2), lhsT=IDF[:], rhs=NM2[:], start=False, stop=True)]),
                          r=[K("gsb"), "UF", "IDF", "NM2"], w=QK(2))
                    P.add("act", lambda e: e.activation(out=t["decS"][:], in_=Q(2), func=AF.Exp, bias=gc[:, 0:1], scale=-1.0), r=QK(2) + [K("gc")], w=[K("decS")])
                    yield
                    P.add("pe", _grp([lambda e: e.transpose(H16[:, 768:896], kT, IDB[:]), lambda e: e.transpose(H16[:, 896:1024], vT, IDB[:])]),
                          r=[kk, vk, "IDB"], w=QK(3))
                    P.add("dve", lambda e: e.tensor_scalar(out=t["kbg"][:], in0=H16[:, 768:896], scalar1=gc[:, 4:5], scalar2=None, op0=ALU.mult), r=QK(3) + [K("gc4")], w=[K("kbg")])
                    P.add("dve", lambda e: e.tensor_scalar(out=t["kdec"][:], in0=H16[:, 768:896], scalar1=gc[:, 5:6], scalar2=None, op0=ALU.mult), r=QK(3) + [K("gc5")], w=[K("kdec")])
                    P.add("dve", lambda e: e.tensor_scalar(out=t["vb"][:], in0=H16[:, 896:1024], scalar1=BETl[:, u, h:h + 1], scalar2=None, op0=ALU.mult), r=QK(3) + [bk_], w=[K("vb")])
                    yield
                    P.add("pe", lambda e: e.matmul(Q(2), lhsT=kT, rhs=kT, start=True, stop=True), r=[kk], w=QK(2))
                    P.add("dve", lambda e: e.scalar_tensor_tensor(out=t["A"][:], in0=Q(2), scalar=NBETl[:, u, h:h + 1], in1=t["decS"][:], op0=ALU.mult, op1=ALU.mult),
                          r=QK(2) + [nbk, K("decS")], w=[K("A")])
                    yield
                    P.add("pe", lambda e: e.transpose(H16[:, 768:896], t["A"][:], IDB[:]), r=[K("A"), "IDB"], w=QK(3))
                    P.add("dve", lambda e: e.tensor_copy(out=t["AT"][:], in_=H16[:, 768:896]), r=QK(3), w=[K("AT")])
                    P.add("pool", lambda e: e.tensor_tensor(out=t["P0"][:], in0=t["A"][:], in1=IMK[:, 0, :], op=ALU.mult), r=[K("A"), "IMK"], w=[K("P0")])
                    P.add("pool", lambda e: e.tensor_tensor(out=t["TM"][:], in0=t["P0"][:], in1=IDB[:], op=ALU.add), r=[K("P0"), "IDB"], w=[K("TM")])
                    yield
                    P.add("pool", lambda e: e.tensor_tensor(out=t["PT0"][:], in0=t["AT"][:], in1=IMK[:, 0, :], op=ALU.mult), r=[K("AT"), "IMK"], w=[K("PT0")])
                    P.add("pool", lambda e: e.tensor_tensor(out=t["RT"][:], in0=t["PT0"][:], in1=IDB[:], op=ALU.add), r=[K("PT0"), "IDB"], w=[K("RT")])
                    yield

                    def upd(ln, with_T):
                        P.add("pe", lambda e: e.matmul(Q(2), lhsT=t[ln][:], rhs=t["RT"][:], start=True, stop=True), r=[K(ln), K("RT")], w=QK(2))
                        if with_T:
                            P.add("pe", lambda e: e.matmul(Q(3), lhsT=t["RT"][:], rhs=t[ln][:], start=True, stop=True), r=[K(ln), K("RT")], w=QK(3))
                        P.add("dve", lambda e: e.tensor_tensor(out=t["RT"][:], in0=t["RT"][:], in1=Q(2), op=ALU.add), r=QK(2) + [K("RT")], w=[K("RT")])
                        if with_T:
                            P.add("dve", lambda e: e.tensor_tensor(out=t["TM"][:], in0=t["TM"][:], in1=Q(3), op=ALU.add), r=QK(3) + [K("TM")], w=[K("TM")])
                    P.add("pe", lambda e: e.matmul(Q(0), lhsT=t["PT0"][:], rhs=t["P0"][:], start=True, stop=True), r=[K("P0"), K("PT0")], w=QK(0))
                    P.add("pe", lambda e: e.matmul(Q(1), lhsT=t["P0"][:], rhs=t["PT0"][:], start=True, stop=True), r=[K("P0"), K("PT0")], w=QK(1))
                    P.add("act", lambda e: e.copy(out=t["P1"][:], in_=Q(0)), r=QK(0), w=[K("P1")])
                    P.add("act", lambda e: e.copy(out=t["PT1"][:], in_=Q(1)), r=QK(1), w=[K("PT1")])
                    yield
                    upd("P1", True)
                    yield
                    P.add("pe", lambda e: e.matmul(Q(0), lhsT=t["PT1"][:], rhs=t["P1"][:], start=True, stop=True), r=[K("P1"), K("PT1")], w=QK(0))
                    P.add("act", lambda e: e.copy(out=t["P0"][:], in_=Q(0)), r=QK(0), w=[K("P0")])
                    yield
                    upd("P0", True)
                    yield
                    for lv in range(1, 5):
                        P.add("pool", (lambda lv: lambda e: e.tensor_tensor(out=t["BKm"][:], in0=t["AT"][:], in1=IMK[:, lv, :], op=ALU.mult))(lv), r=[K("AT"), "IMK"], w=[K("BKm")])
                        P.add("pe", lambda e: e.matmul(Q(0), lhsT=t["BKm"][:], rhs=t["TM"][:], start=True, stop=True), r=[K("BKm"), K("TM")], w=QK(0))
                        P.add("act", lambda e: e.copy(out=t["Ym"][:], in_=Q(0)), r=QK(0), w=[K("Ym")])
                        yield
                        upd("Ym", lv < 4)
                        yield
                    P.add("pe", lambda e: e.matmul(Q(0), lhsT=t["kbg"][:], rhs=t["RT"][:], start=True, stop=True), r=[K("kbg"), K("RT")], w=QK(0))
                    P.add("act", lambda e: e.activation(out=t["nwT"][:], in_=Q(0), func=AF.Copy, scale=-1.0), r=QK(0), w=[K("nwT")])
                    yield
                    P.add("pe", _grp([lambda e: e.matmul(Q(1), lhsT=t["RT"][:], rhs=t["vb"][:], start=True, stop=False),
                                      lambda e: e.matmul(Q(1), lhsT=t["nwT"][:], rhs=Sb[h][:], start=False, stop=True)]),
                          r=[K("RT"), K("vb"), K("nwT"), "Sb%d" % h], w=QK(1))
                    P.add("act", lambda e: e.copy(out=t["vnew"][:], in_=Q(1)), r=QK(1), w=[K("vnew")])
                    yield
                    if own:
                        qt_ = (s - OWN0) * 2 + u
                        ocs = slice(qt_ * 128, (qt_ + 1) * 128)
                        P.add("pe", _grp([lambda e: e.matmul(Q(2), lhsT=t["gsb"][:], rhs=UF[:], start=True, stop=False),
                                          lambda e: e.matmul(Q(2), lhsT=IDF[:], rhs=NMT[:], start=False, stop=True)]),
                              r=[K("gsb"), "UF", "IDF", "NMT"], w=QK(2))
                        P.add("pe", lambda e: e.matmul(Q(3), lhsT=kT, rhs=qT, start=True, stop=True), r=[kk, qk], w=QK(3))
                        P.add("act", lambda e: e.activation(out=t["decT"][:], in_=Q(2), func=AF.Exp, bias=gc[:, 2:3]), r=QK(2) + [K("gc2")], w=[K("decT")])
                        P.add("pool", lambda e: e.tensor_tensor(out=t["qdT"][:], in0=qT, in1=t["egb"][:], op=ALU.mult), r=[qk, K("egb")], w=[K("qdT")])
                        yield
                        P.add("dve", lambda e: e.tensor_tensor(out=t["attnT"][:], in0=Q(3), in1=t["decT"][:], op=ALU.mult), r=QK(3) + [K("decT")], w=[K("attnT")])
                        yield
                        P.add("pe", _grp([lambda e: e.matmul(Q(0), lhsT=Sb[h][:], rhs=t["qdT"][:], start=True, stop=False),
                                          lambda e: e.matmul(Q(0), lhsT=t["vnew"][:], rhs=t["attnT"][:], start=False, stop=True)]),
                              r=["Sb%d" % h, K("qdT"), K("vnew"), K("attnT")], w=QK(0))
                        P.add("act", lambda e: e.activation(out=t["sq"][:], in_=Q(0), func=AF.Square), r=QK(0), w=[K("sq")])
                        P.add("act", lambda e: e.copy(out=t["o1"][:], in_=Q(0)), r=QK(0), w=[K("o1")])
                        yield
                        P.add("pe", lambda e: e.matmul(Q(1), lhsT=ONB[:], rhs=t["sq"][:], start=True, stop=True), r=[K("sq"), "ONB"], w=QK(1))
                        P.add("act", lambda e: e.activation(out=t["egb"][:], in_=Q(1), func=AF.Ln, bias=EPSC[:], scale=1.0 / 128), r=QK(1) + ["EPSC", K("qdT")], w=[K("egb")])
                        P.add("act", lambda e: e.activation(out=t["egb"][:], in_=t["egb"][:], func=AF.Exp, scale=-0.5), r=[K("egb")], w=[K("egb")])
                        yield
                        P.add("dve", lambda e: e.tensor_tensor(out=t["o1"][:], in0=t["o1"][:], in1=t["egb"][:], op=ALU.mult), r=[K("o1"), K("egb")], w=[K("o1")])
                        P.add("dve", lambda e: e.scalar_tensor_tensor(out=OTA[:, h, ocs], in0=t["o1"][:], scalar=ANW[:, 0:1], in1=ZSl[:, h, cs], op0=ALU.mult, op1=ALU.mult),
                              r=[K("o1"), "ANW", "ZS%d" % h + kp], w=["OTA%d_%d" % (h, qt_)])
                        yield
                    P.add("pe", lambda e: e.matmul(Q(2), lhsT=t["kdec"][:], rhs=t["vnew"][:], start=True, stop=True), r=[K("kdec"), K("vnew")], w=QK(2))
                    P.add("dve", lambda e: e.scalar_tensor_tensor(out=Sm[h][:], in0=Sm[h][:], scalar=gc[:, 6:7], in1=Q(2), op0=ALU.mult, op1=ALU.add),
                          r=QK(2) + [K("gc6"), "Sm%d" % h], w=["Sm%d" % h])
                    P.add("act", lambda e: e.copy(out=Sb[h][:], in_=Sm[h][:]), r=["Sm%d" % h], w=["Sb%d" % h])
                    yield

                return pending, chain

            def run_round(chain_fn, pend):
                pi = 0
                if chain_fn is not None:
                    per = max(1, -(-len(pend) // 60))
                    for u in range(2):
                        gens = [chain_fn(u, h) for h in range(4)]
                        while gens:
                            for g_ in list(gens):
                                try:
                                    next(g_)
                                except StopIteration:
                                    gens.remove(g_)
                            for _ in range(per):
                                if pi < len(pend):
                                    P.add(*pend[pi][0], **pend[pi][1]); pi += 1
                while pi < len(pend):
                    P.add(*pend[pi][0], **pend[pi][1]); pi += 1

            prev_chain = None
            for s_ in list(a_list) + [None]:
                pend, ch = do_st(s_) if s_ is not None else ([], None)
                run_round(prev_chain, pend)
                prev_chain = ch
            fin = []
            if dbg:
                for nm, tsr in (("HT", HT), ("QKV", QKVL[0]), ("RAW", RAW), ("ZS", ZSL[0]), ("GST", GSTL[0]), ("BET", BETL[0]), ("ABT", ABTL[0]), ("CV", CV),
                                ("A1C", A1C), ("SH1C", SH1C), ("Sm0", Sm[0]), ("decS", TS[DS]["decS"]), ("A", TS[DS]["A"]), ("RT", TS[DS]["RT"]),
                                ("vnew", TS[DS]["vnew"]), ("gc", TS[DS]["gc"]), ("kbg", TS[DS]["kbg"]), ("kdec", TS[DS]["kdec"]), ("nwT", TS[DS]["nwT"]),
                                ("decT", TS[DS]["decT"]), ("attnT", TS[DS]["attnT"]), ("qdT", TS[DS]["qdT"]), ("o1", TS[DS]["o1"])):
                    dd = nc.dram_tensor("dump_" + nm, list(tsr.shape), F32, kind="ExternalOutput").ap()
                    P.add("pool", (lambda dd, tsr: lambda e: e.dma_start(out=dd, in_=tsr[:]))(dd, tsr), w=["dump_" + nm], dma=True, after_all=True)
                    fin.append("dump_" + nm)
                P.add("pool", lambda e: e.dma_start(out=dbg_o["ota"], in_=OTA[:]), w=["dbg_ota"], dma=True, after_all=True)
                fin.append("dbg_ota")
            P.emit(st, final_wait_keys=fin)
            print("phase A ops:", len(P.ops))

        with ExitStack() as st:
            S = mk(st)
            P = Prog(nc)
            WB = S("WB", [128, 8, 1536], BF16); WO = S("WO", [128, 8, D], BF16)
            BM5 = S("BM5", [128, 8, 640]); KR = S("KR", [128, 4, 1024], BF16); VR = S("VR", [128, 8, 512], BF16)
            WRF = S("WRF", [128, 8, NE]); BR = S("BR", [128, NE]); KM = S("KM", [128, 1]); G1B = S("G1B", [128, D])
            XT = [S("XTb%d" % i, [128, D]) for i in range(2)]
            JNK = S("JNKb", [128, D], BF16)
            XS = [S("XSb%d" % i, [128, D], BF16) for i in range(2)]
            HT = S("HTb", [128, 8, ST], BF16)
            QB = S("QB", [128, 4, ST], BF16)
            STS = S("STS", [128, 640]); PT = S("PT", [128, 640], BF16); RDEN = S("RDEN", [64, 128])
            OTB = S("OTB", [128, 4, ST], BF16)
            T1 = S("T1", [128, D]); H2 = S("H2", [128, D]); H2F = S("H2F", [128, 8, 128]); H2B = S("H2B", [128, 8, 128], BF16)
            SSC = S("SSCb", [128, 8]); RSC = S("RSCb", [128, 8])
            w_in_r = w_in.rearrange("(k p) c -> p k c", p=128)
            P.add("pool", lambda e: e.dma_start(out=WB[:], in_=w_in_r[:, :, 2056:3592]), w=["WB"], dma=True)
            P.add("pool", lambda e: e.dma_start(out=WO[:], in_=w_out.rearrange("(k p) c -> p k c", p=128)), w=["WO"], dma=True)
            P.add("sp", lambda e: e.dma_start(out=BM5[:], in_=bm5_d), w=["BM5"], dma=True)
            P.add("sp", lambda e: e.dma_start(out=WRF[:], in_=w_router.rearrange("(k p) c -> p k c", p=128)), w=["WRF"], dma=True)
            P.add("sp", lambda e: e.dma_start(out=BR[:], in_=br_d), w=["BR"], dma=True)
            P.add("sp", lambda e: e.dma_start(out=KM[:], in_=kmask_d), w=["KM"], dma=True)
            for half in range(2):
                P.add("pe", (lambda half: lambda e: e.matmul(PB[1][:, :], lhsT=ONF[0:1, :], rhs=G1R[0:1, half * 512:(half + 1) * 512], start=True, stop=True))(half),
                      r=[], w=bank(1))
                P.add("dve", (lambda half: lambda e: e.tensor_copy(out=G1B[:, half * 512:(half + 1) * 512], in_=PB[1][:, :]))(half), r=bank(1), w=["G1B"])
            xw_t = xw.rearrange("(n p) d -> n p d", p=128)
            x1s_t = x1s.rearrange("(n p) d -> n p d", p=128)
            PBb0 = PB[0][:].bitcast(BF16)
            for s in (range(HALO0, NST) if do_b else []):
                own = s >= OWN0
                for u in range(2):
                    ti = 2 * s + u
                    xt = XT[u]; xs_ = XS[u]
                    P.add("sp", (lambda xt, ti: lambda e: e.dma_start(out=xt[:], in_=xw_t[ti]))(xt, ti), w=["XT%d" % u], dma=True)
                    P.add("act", (lambda xt, u: lambda e: e.activation(out=JNK[:], in_=xt[:], func=AF.Square, accum_out=SSC[:, u:u + 1]))(xt, u),
                          r=["XT%d" % u], w=["JNK", "SSC%d" % u])
                    P.add("act", (lambda u: lambda e: e.activation(out=RSC[:, u:u + 1], in_=SSC[:, u:u + 1], func=AF.Ln, bias=EPSC[:], scale=1.0 / D))(u),
                          r=["SSC%d" % u], w=["RSC%d" % u])
                    P.add("act", (lambda u: lambda e: e.activation(out=RSC[:, u:u + 1], in_=RSC[:, u:u + 1], func=AF.Exp, scale=-0.5))(u),
                          r=["RSC%d" % u], w=["RSC%d" % u])
                    P.add("dve", (lambda xt, xs_, u: lambda e: e.tensor_scalar(out=xs_[:], in0=xt[:], scalar1=RSC[:, u:u + 1], scalar2=None, op0=ALU.mult))(xt, xs_, u),
                          r=["XT%d" % u, "RSC%d" % u], w=["XS%d" % u])
                    P.add("pe", (lambda xs_: _grp([(lambda k: lambda e: e.transpose(PBb0[:, k * 128:(k + 1) * 128], xs_[:, k * 128:(k + 1) * 128], IDB[:]))(k)
                                                   for k in range(8)]))(xs_), r=["XS%d" % u], w=bank(0))
                    for k in range(8):
                        eng = "act"
                        if eng == "act":
                            f = (lambda k, u: lambda e: e.activation(out=HT[:, k, u * 128:(u + 1) * 128], in_=PBb0[:, k * 128:(k + 1) * 128],
                                                                     func=AF.Identity, bias=SH1C[:, k:k + 1], scale=A1C[:, k:k + 1]))(k, u)
                        else:
                            f = (lambda k, u: lambda e: e.tensor_scalar(out=HT[:, k, u * 128:(u + 1) * 128], in0=PBb0[:, k * 128:(k + 1) * 128],
                                                                        scalar1=A1C[:, k:k + 1], scalar2=SH1C[:, k:k + 1], op0=ALU.mult, op1=ALU.add))(k, u)
                        P.add(eng, f, r=bank(0), w=["HT%d_%d" % (k, u)])
                HTK = ["HT%d_%d" % (k, u) for k in range(8) for u in range(2)]
                slot0 = (2 * s) % 8
                for p in range(4):
                    P.add("pe", (lambda p: _grp([(lambda k: lambda e: e.matmul(PB[1][:, 0:ST], lhsT=WB[:, k, 512 + p * 128:512 + (p + 1) * 128], rhs=HT[:, k, :],
                                                                            start=(k == 0), stop=(k == 7)))(k) for k in range(8)]))(p),
                          r=HTK + ["WB"], w=qs(1, 0, 2))
                    P.add("act", (lambda p, slot0: lambda e: e.copy(out=KR[:, p, slot0 * 128:slot0 * 128 + ST], in_=PB[1][:, 0:ST]))(p, slot0),
                          r=qs(1, 0, 2), w=["KR%d_%d" % (p, slot0), "KR%d_%d" % (p, slot0 + 1)])
                    if own:
                        P.add("pe", (lambda p: _grp([(lambda k: lambda e: e.matmul(PB[1][:, ST:2 * ST], lhsT=WB[:, k, p * 128:(p + 1) * 128], rhs=HT[:, k, :],
                                                                                start=(k == 0), stop=(k == 7)))(k) for k in range(8)]))(p),
                              r=HTK + ["WB"], w=qs(1, 2, 2))
                        P.add("act", (lambda p: lambda e: e.activation(out=QB[:, p, :], in_=PB[1][:, ST:2 * ST], func=AF.Copy, scale=0.125))(p),
                              r=qs(1, 2, 2), w=["QB%d" % p])
                for u in range(2):
                    P.add("pe", (lambda u: _grp([(lambda k: lambda e: e.matmul(PB[2][:, :], lhsT=HT[:, k, u * 128:(u + 1) * 128], rhs=WB[:, k, 1024:1536],
                                                                            start=(k == 0), stop=(k == 7)))(k) for k in range(8)]))(u),
                          r=HTK + ["WB"], w=bank(2))
                    P.add("dve", (lambda u, slot0: lambda e: e.tensor_copy(out=VR[:, slot0 + u, :], in_=PB[2][:, :]))(u, slot0), r=bank(2), w=["VR%d" % (slot0 + u)])
                if not own:
                    continue
                for u in range(2):
                    qt_ = (s - OWN0) * 2 + u
                    W = 48 + qt_
                    slots = [(W - 4 + t) % 8 for t in range(5)]
                    ucs = slice(u * 128, (u + 1) * 128)
                    nh = max(0, 4 - qt_)
                    for hb in range(8):
                        p, r0 = hb // 2, (hb % 2) * 64
                        fns = []
                        for t in range(5):
                            o_ = PB[3][:, t * 128:(t + 1) * 128] if t < 4 else PB[4][:, 0:128]
                            fns.append((lambda o_, p, r0, sl, ucs: lambda e: e.matmul(o_, lhsT=KR[r0:r0 + 64, p, sl * 128:(sl + 1) * 128], rhs=QB[r0:r0 + 64, p, ucs],
                                                                                      start=True, stop=True))(o_, p, r0, slots[t], ucs))
                        P.add("pe", _grp(fns), r=["QB%d" % p] + ["KR%d_%d" % (p, sl) for sl in slots], w=bank(3) + qs(4, 0))
                        P.add("dve", (lambda hb: lambda e: e.tensor_tensor(out=STS[:, 0:512], in0=PB[3][:, :], in1=BM5[:, hb, 0:512], op=ALU.add))(hb),
                              r=bank(3) + ["BM5"], w=["STSa"])
                        P.add("dve", (lambda hb: lambda e: e.tensor_tensor(out=STS[:, 512:640], in0=PB[4][:, 0:128], in1=BM5[:, hb, 512:640], op=ALU.add))(hb),
                              r=qs(4, 0) + ["BM5"], w=["STSb"])
                        if nh > 0:
                            P.add("act", (lambda nh: lambda e: e.activation(out=PT[:, 0:nh * 128], in_=STS[:, 0:nh * 128], func=AF.Exp, bias=KM[:, 0:1]))(nh),
                                  r=["STSa", "KM"], w=["PTa"])
                        P.add("act", (lambda nh: lambda e: e.activation(out=PT[:, nh * 128:640], in_=STS[:, nh * 128:640], func=AF.Exp))(nh),
                              r=["STSa", "STSb"], w=["PTb"])
                        fns = [(lambda t, sl, hb: lambda e: e.matmul(PB[4][0:64, 128:256], lhsT=VR[:, sl, hb * 64:(hb + 1) * 64], rhs=PT[:, t * 128:(t + 1) * 128],
                                                                     start=(t == 0), stop=(t == 4)))(t, slots[t], hb) for t in range(5)]
                        P.add("pe", _grp(fns), r=["PTa", "PTb"] + ["VR%d" % sl for sl in slots], w=qs(4, 1))
                        fns = [(lambda t: lambda e: e.matmul(PB[4][0:64, 256:384], lhsT=ONB[:, 0:64], rhs=PT[:, t * 128:(t + 1) * 128],
                                                             start=(t == 0), stop=(t == 4)))(t) for t in range(5)]
                        P.add("pe", _grp(fns), r=["PTa", "PTb"], w=qs(4, 2))
                        P.add("dve", lambda e: e.reciprocal(out=RDEN[:], in_=PB[4][0:64, 256:384]), r=qs(4, 2), w=["RDEN"])
                        P.add("dve", (lambda p, r0, ucs: lambda e: e.tensor_tensor(out=OTB[r0:r0 + 64, p, ucs], in0=PB[4][0:64, 128:256], in1=RDEN[:], op=ALU.mult))(p, r0, ucs),
                              r=qs(4, 1) + ["RDEN"], w=["OTB%d_%d_%d" % (p, u, hb % 2)])
                    ocs = slice(qt_ * 128, (qt_ + 1) * 128)
                    for half in range(2):
                        fns = []
                        for c in range(8):
                            l_ = OTA[:, c, ocs] if c < 4 else OTB[:, c - 4, ucs]
                            fns.append((lambda c, l_, half: lambda e: e.matmul(PB[5 + half][:, :], lhsT=l_, rhs=WO[:, c, half * 512:(half + 1) * 512],
                                                                               start=(c == 0), stop=(c == 7)))(c, l_, half))
                        P.add("pe", _grp(fns), r=["WO"] + ["OTA%d_%d" % (h, qt_) for h in range(4)] + ["OTB%d_%d_%d" % (p, u, z) for p in range(4) for z in range(2)],
                              w=bank(5 + half))
                        P.add("act", (lambda half: lambda e: e.activation(out=JNK[:, half * 512:(half + 1) * 512], in_=PB[5 + half][:, :], func=AF.Square,
                                                                          accum_out=SSC[:, 2 + half:3 + half]))(half), r=bank(5 + half), w=["JNK", "SSC%d" % (2 + half)])
                    P.add("dve", lambda e: e.tensor_tensor(out=SSC[:, 4:5], in0=SSC[:, 2:3], in1=SSC[:, 3:4], op=ALU.add), r=["SSC2", "SSC3"], w=["SSC4"])
                    P.add("act", lambda e: e.activation(out=RSC[:, 4:5], in_=SSC[:, 4:5], func=AF.Ln, bias=EPSC[:], scale=1.0 / D), r=["SSC4"], w=["RSC4"])
                    P.add("act", lambda e: e.activation(out=RSC[:, 4:5], in_=RSC[:, 4:5], func=AF.Exp, scale=-0.5), r=["RSC4"], w=["RSC4"])
                    for half in range(2):
                        hs = slice(half * 512, (half + 1) * 512)
                        P.add("dve", (lambda half, hs: lambda e: e.scalar_tensor_tensor(out=T1[:, hs], in0=PB[5 + half][:, :], scalar=RSC[:, 4:5], in1=G1B[:, hs],
                                                                                        op0=ALU.mult, op1=ALU.mult))(half, hs),
                              r=bank(5 + half) + ["RSC4", "G1B"], w=["T1_%d" % half])
                    P.add("pool", (lambda u: lambda e: e.tensor_tensor(out=T1[:], in0=T1[:], in1=XT[u][:], op=ALU.add))(u), r=["T1_0", "T1_1", "XT%d" % u], w=["T1_0", "T1_1"])
                    P.add("sp", (lambda qt_: lambda e: e.dma_start(out=x1s_t[qt_], in_=T1[:]))(qt_), r=["T1_0", "T1_1"], w=["x1s"], dma=True)
                    if dbg:
                        P.add("sp", (lambda qt_: lambda e: e.dma_start(out=dbg_o["x1"].rearrange("(n p) d -> n p d", p=128)[qt_], in_=T1[:]))(qt_), r=["T1_0", "T1_1"], w=["dbg_x1"], dma=True)
                    P.add("act", lambda e: e.activation(out=JNK[:], in_=T1[:], func=AF.Square, accum_out=SSC[:, 5:6]), r=["T1_0", "T1_1"], w=["JNK", "SSC5"])
                    P.add("act", lambda e: e.activation(out=RSC[:, 5:6], in_=SSC[:, 5:6], func=AF.Ln, bias=EPSC[:], scale=1.0 / D), r=["SSC5"], w=["RSC5"])
                    P.add("act", lambda e: e.activation(out=RSC[:, 5:6], in_=RSC[:, 5:6], func=AF.Exp, scale=-0.5), r=["RSC5"], w=["RSC5"])
                    P.add("dve", lambda e: e.tensor_scalar(out=H2[:], in0=T1[:], scalar1=RSC[:, 5:6], scalar2=None, op0=ALU.mult), r=["T1_0", "T1_1", "RSC5"], w=["H2"])
                    for half in range(2):
                        P.add("pe", (lambda half: _grp([(lambda k: lambda e: e.transpose(PB[5 + half][:, (k % 4) * 128:(k % 4 + 1) * 128], H2[:, k * 128:(k + 1) * 128], IDF[:]))(k)
                                                        for k in range(half * 4, half * 4 + 4)]))(half), r=["H2"], w=bank(5 + half))
                        for k in range(half * 4, half * 4 + 4):
                            eng = "act"
                            src = PB[5 + half][:, (k % 4) * 128:(k % 4 + 1) * 128]
                            if eng == "act":
                                f = (lambda k, src: lambda e: e.activation(out=H2F[:, k, :], in_=src, func=AF.Identity, bias=SH2C[:, k:k + 1], scale=A2C[:, k:k + 1]))(k, src)
                            else:
                                f = (lambda k, src: lambda e: e.tensor_scalar(out=H2F[:, k, :], in0=src, scalar1=A2C[:, k:k + 1], scalar2=SH2C[:, k:k + 1], op0=ALU.mult, op1=ALU.add))(k, src)
                            P.add(eng, f, r=bank(5 + half), w=["H2F%d" % k])
                    H2FK = ["H2F%d" % k for k in range(8)]
                    P.add("pool", lambda e: e.tensor_copy(out=H2B[:], in_=H2F[:]), r=H2FK, w=["H2B"])
                    P.add("sp", (lambda ocs: lambda e: e.dma_start(out=h2s[:, :, ocs], in_=H2B[:]))(ocs), r=["H2B"], w=["h2s"], dma=True)
                    P.add("pe", _grp([(lambda k: lambda e: e.matmul(PB[7][:, 0:NE], lhsT=H2F[:, k, :], rhs=WRF[:, k, :], start=(k == 0), stop=(k == 7)))(k) for k in range(8)]),
                          r=H2FK + ["WRF"], w=qs(7, 0))
                    P.add("dve", (lambda qt_: lambda e: e.tensor_tensor(out=LG[:, qt_, :], in0=PB[7][:, 0:NE], in1=BR[:], op=ALU.add))(qt_), r=qs(7, 0) + ["BR"], w=["LG"])
            fin = ["x1s", "h2s"]
            if dbg and do_b:
                P.add("sp", lambda e: e.dma_start(out=dbg_o["lg"], in_=LG[:]), r=["LG"], w=["dbg_lg"], dma=True)
                fin += ["dbg_lg", "dbg_x1"]
            P.emit(st, final_wait_keys=fin)
            print("phase B ops:", len(P.ops))

        stAB.close()
        with ExitStack() as st:
            S = mk(st)
            P = Prog(nc)
            H2T = S("H2T", [128, 8, NTOK], BF16); Y = S("Y", [128, 16, D]); G = S("G", [128, 16, NE])
            WU = S("WU", [128, 8, 2 * D], BF16); WD = S("WD", [128, 8, D], BF16); ACTT = S("ACTT", [128, 8, 1024], BF16)
            BU = S("BU", [128, NE, 16]); BDN = S("BDN", [NE, D]); GT = S("GT", [NE, 128])
            TG = [S("TG%d" % i, [128, 512]) for i in range(2)]
            TSg = [S("TSg%d" % i, [128, 512], BF16) for i in range(2)]
            TL = [S("TL%d" % i, [128, 512]) for i in range(2)]
            G2B = S("G2B", [128, D]); X1T = S("X1T", [128, D]); JNK = S("JNKc", [128, D], BF16)
            T8 = S("T8", [128, 8]); MSK = S("MSK", [128, NE]); EX = S("EX", [128, NE]); CL = S("CL", [128, 8])
            P.add("sp", lambda e: e.dma_start(out=BU[:], in_=bu_d), w=["BU"], dma=True)
            P.add("dve", lambda e: e.tensor_scalar(out=BU[:, :, 8:16], in0=BU[:, :, 8:16], scalar1=1.0, scalar2=None, op0=ALU.add), r=["BU"], w=["BU"])
            P.add("sp", lambda e: e.dma_start(out=BDN[0:n_exp, :], in_=b_down), w=["BDN"], dma=True)
            for hh in range(2):
                P.add("sp", (lambda hh: lambda e: e.dma_start(out=H2T[:, :, hh * 1024:(hh + 1) * 1024], in_=h2s[:, :, hh * 1024:(hh + 1) * 1024]))(hh), w=["H2T"], dma=True)
            P.add("pool", lambda e: e.dma_start(out=WU[:], in_=w_up[0].rearrange("(k p) f -> p k f", p=128)), w=["WU"], dma=True)
            P.add("pool", lambda e: e.dma_start(out=WD[:], in_=w_down[0].rearrange("(k p) f -> p k f", p=128)), w=["WD"], dma=True)
            P.add("pool", lambda e: e.memset(Y[:], 0.0), w=["Y%d" % i for i in range(16)])
            for half in range(2):
                P.add("pe", (lambda half: lambda e: e.matmul(PB[1][:, :], lhsT=ONF[0:1, :], rhs=G2R[0:1, half * 512:(half + 1) * 512], start=True, stop=True))(half),
                      r=[], w=bank(1))
                P.add("dve", (lambda half: lambda e: e.tensor_copy(out=G2B[:, half * 512:(half + 1) * 512], in_=PB[1][:, :]))(half), r=bank(1), w=["G2B"])
            for tt in range(16):
                L_ = LG[:, tt, :]
                P.add("dve", (lambda L_: lambda e: e.max(out=T8[:], in_=L_))(L_), r=[], w=["T8"])
                P.add("dve", (lambda L_: lambda e: e.tensor_scalar(out=MSK[:], in0=L_, scalar1=T8[:, 3:4], scalar2=None, op0=ALU.is_ge))(L_), r=["T8"], w=["MSK"])
                P.add("dve", lambda e: e.tensor_scalar(out=CL[:, 0:1], in0=T8[:, 0:1], scalar1=-1.0, scalar2=None, op0=ALU.mult), r=["T8"], w=["CL0"])
                P.add("act", (lambda L_: lambda e: e.activation(out=EX[:], in_=L_, func=AF.Exp, bias=CL[:, 0:1]))(L_), r=["CL0"], w=["EX"])
                P.add("dve", lambda e: e.tensor_tensor(out=EX[:], in0=EX[:], in1=MSK[:], op=ALU.mult), r=["EX", "MSK"], w=["EX"])
                P.add("dve", lambda e: e.reduce_sum(out=CL[:, 1:2], in_=EX[:], axis=AX.X), r=["EX"], w=["CL1"])
                P.add("dve", lambda e: e.reciprocal(out=CL[:, 2:3], in_=CL[:, 1:2]), r=["CL1"], w=["CL2"])
                P.add("dve", (lambda tt: lambda e: e.tensor_scalar(out=G[:, tt, :], in0=EX[:], scalar1=CL[:, 2:3], scalar2=None, op0=ALU.mult))(tt), r=["EX", "CL2"], w=["G"])
            x1s_t = x1s.rearrange("(n p) d -> n p d", p=128)
            out_t = out.rearrange("(n p) d -> n p d", p=128)
            for ex in (range(n_exp) if do_c else []):
                for tg in range(4):
                    slot = tg % 2
                    tcs = slice(tg * 512, (tg + 1) * 512)
                    for fc in range(8):
                        pb = (fc % 2) * 2; ss = fc % 2
                        for z, fcc in ((0, fc), (1, fc + 8)):
                            P.add("pe", (lambda fcc, pbz, tcs: _grp([(lambda k: lambda e: e.matmul(PB[pbz][:, :], lhsT=WU[:, k, fcc * 128:(fcc + 1) * 128], rhs=H2T[:, k, tcs],
                                                                                               start=(k == 0), stop=(k == 7)))(k) for k in range(8)]))(fcc, pb + z, tcs),
                                  r=["WU", "H2T"], w=bank(pb + z))
                        P.add("dve", (lambda ex, fc, pb, ss: lambda e: e.tensor_scalar(out=TG[ss][:], in0=PB[pb][:, :], scalar1=BU[:, ex, fc:fc + 1], scalar2=7.0, op0=ALU.add, op1=ALU.min))(ex, fc, pb, ss),
                              r=bank(pb) + ["BU"], w=["TG%d" % ss])
                        P.add("act", (lambda ss: lambda e: e.activation(out=TSg[ss][:], in_=TG[ss][:], func=AF.Sigmoid, scale=1.702))(ss), r=["TG%d" % ss], w=["TSg%d" % ss])
                        P.add("act", (lambda ex, fc, pb, ss: lambda e: e.activation(out=TL[ss][:], in_=PB[pb + 1][:, :], func=AF.Identity, bias=BU[:, ex, 8 + fc:9 + fc]))(ex, fc, pb, ss),
                              r=bank(pb + 1) + ["BU"], w=["TL%d" % ss])
                        P.add("pool", (lambda ss: lambda e: e.tensor_scalar(out=TL[ss][:], in0=TL[ss][:], scalar1=8.0, scalar2=-6.0, op0=ALU.min, op1=ALU.max))(ss), r=["TL%d" % ss], w=["TL%d" % ss])
                        P.add("dve", (lambda ss: lambda e: e.tensor_tensor(out=TG[ss][:], in0=TG[ss][:], in1=TSg[ss][:], op=ALU.mult))(ss), r=["TG%d" % ss, "TSg%d" % ss], w=["TG%d" % ss])
                        P.add("pool", (lambda fc, ss, slot: lambda e: e.tensor_tensor(out=ACTT[:, fc, slot * 512:(slot + 1) * 512], in0=TG[ss][:], in1=TL[ss][:], op=ALU.mult))(fc, ss, slot),
                              r=["TG%d" % ss, "TL%d" % ss], w=["ACTT%d_%d" % (fc, slot)])
                    if tg == 3 and ex + 1 < n_exp:
                        P.add("pool", (lambda ex: lambda e: e.dma_start(out=WU[:], in_=w_up[ex + 1].rearrange("(k p) f -> p k f", p=128)))(ex), w=["WU"], dma=True)
                    for t4 in range(4):
                        tt = tg * 4 + t4
                        acs = slice(slot * 512 + t4 * 128, slot * 512 + (t4 + 1) * 128)
                        for half in range(2):
                            pb = 4 + (tt % 2) * 2 + half
                            fns = [(lambda fc, pb, acs, half: lambda e: e.matmul(PB[pb][:, :], lhsT=ACTT[:, fc, acs], rhs=WD[:, fc, half * 512:(half + 1) * 512],
                                                                                 start=(fc == 0), stop=(fc == 7)))(fc, pb, acs, half) for fc in range(8)]
                            P.add("pe", _grp(fns), r=["WD"] + ["ACTT%d_%d" % (fc, slot) for fc in range(8)], w=bank(pb))
                            P.add("dve", (lambda tt, half, pb, ex: lambda e: e.scalar_tensor_tensor(out=Y[:, tt, half * 512:(half + 1) * 512], in0=PB[pb][:, :],
                                                                                                  scalar=G[:, tt, ex:ex + 1], in1=Y[:, tt, half * 512:(half + 1) * 512],
                                                                                                  op0=ALU.mult, op1=ALU.add))(tt, half, pb, ex),
                                  r=bank(pb) + ["G", "Y%d" % tt], w=["Y%d" % tt])
                if ex + 1 < n_exp:
                    P.add("pool", (lambda ex: lambda e: e.dma_start(out=WD[:], in_=w_down[ex + 1].rearrange("(k p) f -> p k f", p=128)))(ex), w=["WD"], dma=True)
            for tt in (range(16) if do_c else []):
                P.add("pe", (lambda tt: lambda e: e.transpose(PB[2][0:NE, 0:128], G[:, tt, :], IDF[:]))(tt), r=["G", "IDF"], w=bank(2))
                P.add("dve", lambda e: e.tensor_copy(out=GT[:], in_=PB[2][0:NE, 0:128]), r=bank(2), w=["GT"])
                for half in range(2):
                    P.add("pe", (lambda half: lambda e: e.matmul(PB[half][:, :], lhsT=GT[0:n_exp, :], rhs=BDN[0:n_exp, half * 512:(half + 1) * 512], start=True, stop=True))(half),
                          r=["GT", "BDN"], w=bank(half))
                    P.add("dve", (lambda tt, half: lambda e: e.tensor_tensor(out=Y[:, tt, half * 512:(half + 1) * 512], in0=Y[:, tt, half * 512:(half + 1) * 512],
                                                                            in1=PB[half][:, :], op=ALU.add))(tt, half), r=bank(half) + ["Y%d" % tt], w=["Y%d" % tt])
                P.add("sp", (lambda tt: lambda e: e.dma_start(out=X1T[:], in_=x1s_t[tt]))(tt), w=["X1T"], dma=True)
                P.add("act", (lambda tt: lambda e: e.activation(out=JNK[:], in_=Y[:, tt, :], func=AF.Square, accum_out=CL[:, 4:5]))(tt), r=["Y%d" % tt], w=["JNK", "CL4"])
                P.add("act", lambda e: e.activation(out=CL[:, 5:6], in_=CL[:, 4:5], func=AF.Ln, bias=EPSC[:], scale=1.0 / D), r=["CL4"], w=["CL5"])
                P.add("act", lambda e: e.activation(out=CL[:, 5:6], in_=CL[:, 5:6], func=AF.Exp, scale=-0.5), r=["CL5"], w=["CL5"])
                P.add("dve", (lambda tt: lambda e: e.scalar_tensor_tensor(out=Y[:, tt, :], in0=Y[:, tt, :], scalar=CL[:, 5:6], in1=G2B[:], op0=ALU.mult, op1=ALU.mult))(tt),
                      r=["Y%d" % tt, "CL5", "G2B"], w=["Y%d" % tt])
                P.add("dve", (lambda tt: lambda e: e.tensor_tensor(out=X1T[:], in0=X1T[:], in1=Y[:, tt, :], op=ALU.add))(tt), r=["Y%d" % tt, "X1T"], w=["X1T"])
                P.add("sp", (lambda tt: lambda e: e.dma_start(out=out_t[tt], in_=X1T[:]))(tt), r=["X1T"], w=["out"], dma=True)
            P.emit(st, final_wait_keys=["out"] if do_c else [])
            print("phase C ops:", len(P.ops))
    return nc


def _host_inputs(inputs):
    x = np.ascontiguousarray(inputs["x"], dtype=np.float32)
    c = inputs["c"]
    rel = inputs["rel_bias"][0]
    kk = np.arange(128)[:, None, None]; t = np.arange(5)[None, :, None]; qq = np.arange(128)[None, None, :]
    diff = 128 * (4 - t) + qq - kk
    idx = np.clip(diff, -128, 128) + 128
    cd = 8 - 2 * t + qq // 64 - kk // 64
    valid = (cd >= 0) & (cd <= 8)
    bm5 = np.empty((128, 8, 640), np.float32)
    for hb in range(8):
        bm5[:, hb, :] = np.where(valid, rel[hb][idx], np.float32(NEG)).reshape(128, 640)
    ii = np.arange(128)[:, None]; jj = np.arange(128)[None, :]
    imask = np.zeros((128, 5, 128), np.float32)
    imask[:, 0, :] = (ii // 8 == jj // 8)
    for lv, sz in enumerate((8, 16, 32, 64), start=1):
        mk = (ii // (2 * sz) == jj // (2 * sz)) & ((ii % (2 * sz)) >= sz) & ((jj % (2 * sz)) < sz)
        imask[:, lv, :] = mk.T
    cw = np.ascontiguousarray(inputs["conv_w"][0].reshape(4, 12, 128).transpose(2, 1, 0))
    bu = np.ascontiguousarray(inputs["b_up"][0].reshape(NE, 16, 128).transpose(2, 0, 1))
    shared = {
        "w_ada": inputs["w_ada"][0], "b_ada": inputs["b_ada"][0].reshape(1, -1), "norm_w": inputs["norm_w"][0].reshape(1, -1),
        "w_in": inputs["w_in"][0], "cw": cw, "alog": np.ascontiguousarray(np.broadcast_to(inputs["a_log"][0], (128, 4))),
        "dtb": np.ascontiguousarray(np.broadcast_to(inputs["dt_bias"][0], (128, 4))), "anw": inputs["a_norm_w"][0].reshape(128, 1),
        "imask": imask, "bm5": bm5, "w_out": inputs["w_out"][0], "w_router": inputs["w_router"][0],
        "br": np.ascontiguousarray(np.broadcast_to(inputs["b_router"][0], (128, NE))), "w_up": inputs["w_up"][0], "bu": bu,
        "w_down": inputs["w_down"][0], "b_down": inputs["b_down"][0],
    }
    shared = {k: np.ascontiguousarray(v, dtype=np.float32) for k, v in shared.items()}
    maps = []
    for core in range(8):
        b, j = core // 4, core % 4
        xw = np.zeros((WIN, D), np.float32)
        n_real = NTOK * (j + 1)
        xw[WIN - n_real:] = x[b, :n_real]
        qv = np.zeros((128, 3), np.float32)
        for q in range(3):
            qv[:, q] = 1.0 if (j - 3 + q) >= 0 else 0.0
        km = np.full((128, 1), 0.0 if j >= 1 else NEG, np.float32)
        m = dict(shared)
        m.update({"xw": xw, "qvalid": qv, "kmask": km, "ccol": np.ascontiguousarray(c[b].reshape(8, 128).T, dtype=np.float32)})
        maps.append(m)
    return maps


_NC_CACHE = {}


def kernel(**inputs):
    maps = _host_inputs(inputs)
    if "nc" not in _NC_CACHE:
        _NC_CACHE["nc"] = build()
    nc = _NC_CACHE["nc"]
    res = run_bass_kernel_spmd(nc, maps, core_ids=list(range(8)))
    full = np.empty((2, 8192, D), np.float32)
    for core in range(8):
        b, j = core // 4, core % 4
        full[b, j * NTOK:(j + 1) * NTOK] = res.results[core]["out"]
    return full
```

```python
import numpy as np
from contextlib import ExitStack
import concourse.bass as bass
import concourse.mybir as mybir
from concourse.bass_utils import run_bass_kernel_spmd

F32 = mybir.dt.float32
BF16 = mybir.dt.bfloat16
AF = mybir.ActivationFunctionType
ALU = mybir.AluOpType
AX = mybir.AxisListType

ENGS = ("pe", "act", "dve", "pool", "sp")
DS = 1
NEG = -30000.0
EPS = 1e-6


class Prog:
    def __init__(self, nc, n_dma_sems=6):
        self.nc = nc
        self.ops = []
        self.last_w = {}
        self.readers = {}
        self.n_dma_sems = n_dma_sems

    def add(self, eng, fn, r=(), w=(), dma=False, after_all=False):
        banks = set()
        for k in list(r) + list(w):
            if k[0] == "q" and "_" in k and k[1:k.index("_")].isdigit():
                banks.add("BK" + k[1:k.index("_")])
        deps = set()
        for k in r:
            if k in self.last_w:
                deps.add(self.last_w[k])
        for k in w:
            if k in self.last_w:
                deps.add(self.last_w[k])
            deps.update(self.readers.get(k, ()))
        tdeps = set()
        for k in banks:
            if k in self.last_w and self.last_w[k] not in deps:
                tdeps.add(self.last_w[k])
        deps |= tdeps
        w = list(w) + sorted(banks)
        idx = len(self.ops)
        if after_all:
            deps = set(range(idx))
        self.ops.append(dict(eng=eng, fn=fn, deps=deps, dma=dma, sig=False, tdeps=tdeps))
        for k in r:
            self.readers.setdefault(k, []).append(idx)
        for k in w:
            self.last_w[k] = idx
            self.readers[k] = []
        return idx

    def emit(self, stack, final_wait_keys=()):
        nc = self.nc
        ops = self.ops
        final_deps = set()
        for k in final_wait_keys:
            if k in self.last_w:
                final_deps.add(self.last_w[k])
        for o in ops:
            for d in o["deps"]:
                ops[d]["sig"] = True
        for d in final_deps:
            ops[d]["sig"] = True
        esem = {e: stack.enter_context(nc.semaphore("s_" + e)) for e in ENGS}
        dsem = {e: [stack.enter_context(nc.semaphore("d_%s%d" % (e, i))) for i in range(self.n_dma_sems)]
                for e in ENGS if e != "pe"}
        ecount = {e: 0 for e in ENGS}
        dcount = {e: [0] * self.n_dma_sems for e in dsem}
        drr = {e: 0 for e in dsem}
        for o in ops:
            e = o["eng"]
            if o["dma"]:
                j = drr[e]
                drr[e] = (j + 1) % self.n_dma_sems
                o["prev_on_sem"] = (dsem[e][j], dcount[e][j]) if dcount[e][j] else None
                dcount[e][j] += 16
                o["sem"] = dsem[e][j]
                o["val"] = dcount[e][j]
            elif o["sig"]:
                ecount[e] += 1
                o["sem"] = esem[e]
                o["val"] = ecount[e]
        per_eng = {e: [] for e in ENGS}
        for i, o in enumerate(ops):
            per_eng[o["eng"]].append(i)
        block = stack.enter_context(nc.Block())

        def run(e, eng):
            waited = {}

            def wait(sem, val):
                key = id(sem)
                if waited.get(key, 0) >= val:
                    return
                waited[key] = val
                eng.wait_ge(sem, val)

            for i in per_eng[e]:
                o = ops[i]
                for d in sorted(o["deps"]):
                    p = ops[d]
                    if p["eng"] == "pe" and e == "pe" and not p["dma"] and not o["dma"]:
                        continue
                    if d in o["tdeps"] and p["eng"] == e and not p["dma"] and not o["dma"]:
                        continue
                    wait(p["sem"], p["val"])
                if o["dma"] and o["prev_on_sem"] is not None:
                    wait(*o["prev_on_sem"])
                ins = o["fn"](eng)
                if o["dma"]:
                    ins.then_inc(o["sem"], 16)
                elif o["sig"]:
                    ins.then_inc(o["sem"], 1)
            if e == "sp":
                for d in sorted(final_deps):
                    wait(ops[d]["sem"], ops[d]["val"])

        @block.tensor
        def _(eng):
            run("pe", eng)

        @block.scalar
        def _(eng):
            run("act", eng)

        @block.vector
        def _(eng):
            run("dve", eng)

        @block.gpsimd
        def _(eng):
            run("pool", eng)

        @block.sync
        def _(eng):
            run("sp", eng)


D = 1024
NTOK = 2048
WIN = 8192
ST = 256
NST = WIN // ST
OWN0 = (WIN - NTOK) // ST
HALO0 = OWN0 - 2
C_Q, C_K, C_V, C_Z, C_AB, C_QB, C_KB, C_VB = 0, 512, 1024, 1536, 2048, 2056, 2568, 3080
NE = 32


def _grp(fns):
    def f(e):
        ins = None
        for g in fns:
            ins = g(e)
        return ins
    return f


def build(n_exp=NE, dbg=False, a_list=None, do_b=True, do_c=True, stop=99):
    a_list = list(range(NST)) if a_list is None else a_list
    nc = bass.Bass("TRN2", target_bir_lowering=False)
    DI = lambda name, shape, dt=F32: nc.dram_tensor(name, shape, dt, kind="ExternalInput").ap()
    xw = DI("xw", [WIN, D]); qvalid_d = DI("qvalid", [128, 3]); kmask_d = DI("kmask", [128, 1])
    ccol_d = DI("ccol", [128, 8]); w_ada = DI("w_ada", [D, 6 * D]); b_ada = DI("b_ada", [1, 6 * D])
    norm_w = DI("norm_w", [1, 4 * D]); w_in = DI("w_in", [D, 3592]); cw_d = DI("cw", [128, 12, 4])
    imk_d = DI("imask", [128, 5, 128]); alog_d = DI("alog", [128, 4]); dtb_d = DI("dtb", [128, 4]); anw_d = DI("anw", [128, 1])
    bm5_d = DI("bm5", [128, 8, 640]); w_out = DI("w_out", [D, D]); w_router = DI("w_router", [D, NE])
    br_d = DI("br", [128, NE]); w_up = DI("w_up", [n_exp, D, 2 * D]); bu_d = DI("bu", [128, NE, 16])
    w_down = DI("w_down", [n_exp, D, D]); b_down = DI("b_down", [n_exp, D])
    out = nc.dram_tensor("out", [NTOK, D], F32, kind="ExternalOutput").ap()
    x1s = nc.dram_tensor("x1s", [NTOK, D], F32).ap()
    h2s = nc.dram_tensor("h2s", [128, 8, NTOK], BF16).ap()
    dbg_o = {}
    if dbg:
        dbg_o["ota"] = nc.dram_tensor("dbg_ota", [128, 4, NTOK], F32, kind="ExternalOutput").ap()
        dbg_o["otb"] = nc.dram_tensor("dbg_otb", [128, 4, NTOK], F32, kind="ExternalOutput").ap()
        dbg_o["x1"] = nc.dram_tensor("dbg_x1", [NTOK, D], F32, kind="ExternalOutput").ap()
        dbg_o["lg"] = nc.dram_tensor("dbg_lg", [128, 16, NE], F32, kind="ExternalOutput").ap()

    with ExitStack() as st0:
        def mk(st):
            return lambda name, shape, dt=F32: st.enter_context(nc.sbuf_tensor(name, shape, dt))
        S0 = mk(st0)
        IDF = S0("IDF", [128, 128]); IDB = S0("IDB", [128, 128], BF16)
        ONF = S0("ONF", [128, 128]); ONB = S0("ONB", [128, 128], BF16)
        LG = S0("LG", [128, 16, NE])
        G2R = S0("G2R", [1, D])
        A1C = S0("A1C", [128, 8]); SH1C = S0("SH1C", [128, 8]); A2C = S0("A2C", [128, 8]); SH2C = S0("SH2C", [128, 8])
        EPSC = S0("EPSC", [128, 1])
        PB = [st0.enter_context(nc.psum_tensor("pb%d" % i, [128, 512], F32)) for i in range(8)]
        stAB = ExitStack()
        SAB = mk(stAB)
        OTA = SAB("OTA", [128, 4, NTOK], BF16)
        G1R = SAB("G1R", [1, D])

        def bank(b):
            return ["q%d_%d" % (b, i) for i in range(4)]

        def qs(b, q0, n=1):
            return ["q%d_%d" % (b, i) for i in range(q0, q0 + n)]

        with ExitStack() as st:
            S = mk(st)
            P = Prog(nc)
            UF = S("UF", [128, 128]); NM2 = S("NM2", [128, 128]); NMT = S("NMT", [128, 128]); IMK = S("IMK", [128, 5, 128])
            WA = S("WA", [128, 8, 2056], BF16)
            QV = S("QV", [128, 3]); CW = S("CW", [128, 12, 4]); NEGA = S("NEGA", [128, 4]); DTB = S("DTB", [128, 4])
            ANW = S("ANW", [128, 1]); CC = S("CC", [128, 8]); SC = S("SC", [128, 8])
            WST = S("WST", [128, 8, 512])
            BST = S("BST", [1, 512]); NWT = S("NWT", [1, 512]); ROWT = S("ROWT", [1, 512])
            P.add("pool", lambda e: e.memset(IDF[:], 0.0), w=["IDF"])
            P.add("pool", lambda e: e.affine_select(out=IDF[:], in_=IDF[:], pattern=[[-1, 128]], compare_op=ALU.not_equal,
                                                     fill=1.0, base=0, channel_multiplier=1), r=["IDF"], w=["IDF"])
            P.add("pool", lambda e: e.tensor_copy(out=IDB[:], in_=IDF[:]), r=["IDF"], w=["IDB"])
            P.add("pool", lambda e: e.memset(ONF[:], 1.0), w=["ONF"])
            P.add("pool", lambda e: e.memset(ONB[:], 1.0), w=["ONB"])
            P.add("pool", lambda e: e.memset(EPSC[:], EPS), w=["EPSC"])
            P.add("pool", lambda e: e.memset(UF[:], 1.0), w=["UF"])
            P.add("pool", lambda e: e.affine_select(out=UF[:], in_=UF[:], pattern=[[1, 128]], compare_op=ALU.is_ge,
                                                     fill=0.0, base=0, channel_multiplier=-1), r=["UF"], w=["UF"])
            P.add("pool", lambda e: e.memset(NM2[:], -NEG), w=["NM2"])
            P.add("pool", lambda e: e.affine_select(out=NM2[:], in_=NM2[:], pattern=[[1, 128]], compare_op=ALU.is_ge,
                                                     fill=0.0, base=0, channel_multiplier=-1), r=["NM2"], w=["NM2"])
            P.add("pool", lambda e: e.memset(NMT[:], NEG), w=["NMT"])
            P.add("pool", lambda e: e.affine_select(out=NMT[:], in_=NMT[:], pattern=[[-1, 128]], compare_op=ALU.is_gt,
                                                     fill=0.0, base=0, channel_multiplier=1), r=["NMT"], w=["NMT"])
            for dst, src, key in ((IMK, imk_d, "IMK"), (QV, qvalid_d, "QV"), (CW, cw_d, "CW"), (NEGA, alog_d, "NEGA"), (DTB, dtb_d, "DTB"),
                                  (ANW, anw_d, "ANW"), (CC, ccol_d, "CC")):
                P.add("sp", (lambda dst, src: lambda e: e.dma_start(out=dst[:], in_=src))(dst, src), w=[key], dma=True)
            w_in_r = w_in.rearrange("(k p) c -> p k c", p=128)
            P.add("pool", lambda e: e.dma_start(out=WA[:, :, 0:2048], in_=w_in_r[:, :, 0:2048]), w=["WA0"], dma=True)
            P.add("pool", lambda e: e.dma_start(out=WA[:, :, 2048:2056], in_=w_in_r[:, :, 2048:2056]), w=["WA1"], dma=True)
            P.add("act", lambda e: e.activation(out=NEGA[:], in_=NEGA[:], func=AF.Exp), r=["NEGA"], w=["NEGA"])
            P.add("dve", lambda e: e.tensor_scalar(out=NEGA[:], in0=NEGA[:], scalar1=-1.0, scalar2=None, op0=ALU.mult), r=["NEGA"], w=["NEGA"])
            P.add("act", lambda e: e.activation(out=SC[:], in_=CC[:], func=AF.Silu), r=["CC"], w=["SC"])
            w_ada_r = w_ada.rearrange("(k p) n -> p k n", p=128)
            for nb in range(12):
                kind, half = nb // 2, nb % 2
                P.add("sp", (lambda nb: lambda e: e.dma_start(out=WST[:], in_=w_ada_r[:, :, nb * 512:(nb + 1) * 512]))(nb), w=["WST"], dma=True)
                P.add("sp", (lambda nb: lambda e: e.dma_start(out=BST[:], in_=b_ada[0:1, nb * 512:(nb + 1) * 512]))(nb), w=["BST"], dma=True)
                if kind in (1, 2, 4, 5):
                    nwi = {1: 0, 2: 1, 4: 2, 5: 3}[kind]
                    P.add("sp", (lambda o_: lambda e: e.dma_start(out=NWT[:], in_=norm_w[0:1, o_:o_ + 512]))(nwi * D + half * 512), w=["NWT"], dma=True)
                P.add("pe", _grp([(lambda k: lambda e: e.matmul(PB[0][0:1, :], lhsT=SC[:, k:k + 1], rhs=WST[:, k, :], start=(k == 0), stop=(k == 7)))(k) for k in range(8)]),
                      r=["SC", "WST"], w=bank(0))
                P.add("dve", lambda e: e.tensor_tensor(out=ROWT[:], in0=PB[0][0:1, :], in1=BST[:], op=ALU.add), r=bank(0) + ["BST"], w=["ROWT"])
                if kind in (1, 4):
                    P.add("dve", lambda e: e.scalar_tensor_tensor(out=ROWT[:], in0=ROWT[:], scalar=1.0, in1=NWT[:], op0=ALU.add, op1=ALU.mult), r=["ROWT", "NWT"], w=["ROWT"])
                elif kind in (2, 5):
                    P.add("dve", lambda e: e.tensor_tensor(out=ROWT[:], in0=ROWT[:], in1=NWT[:], op=ALU.mult), r=["ROWT", "NWT"], w=["ROWT"])
                if kind in (0, 1, 3, 4):
                    dst, key = {0: (SH1C, "SH1C"), 1: (A1C, "A1C"), 3: (SH2C, "SH2C"), 4: (A2C, "A2C")}[kind]
                    P.add("pe", _grp([(lambda k: lambda e: e.matmul(PB[1][:, k:k + 1], lhsT=ROWT[0:1, k * 128:(k + 1) * 128], rhs=ONF[0:1, 0:1], start=True, stop=True))(k)
                                      for k in range(4)]), r=["ROWT", "ONF"], w=bank(1))
                    P.add("dve", (lambda dst, half: lambda e: e.tensor_copy(out=dst[:, half * 4:half * 4 + 4], in_=PB[1][:, 0:4]))(dst, half), r=bank(1), w=[key])
                else:
                    dst, key = (G1R, "G1R") if kind == 2 else (G2R, "G2R")
                    P.add("dve", (lambda dst, half: lambda e: e.tensor_copy(out=dst[0:1, half * 512:(half + 1) * 512], in_=ROWT[:]))(dst, half), r=["ROWT"], w=[key])

            XT = [S("XT%d" % i, [128, D]) for i in range(2)]
            JNK = S("JNK", [128, D], BF16)
            XS = [S("XS%d" % i, [128, D], BF16) for i in range(2)]
            HT = S("HT", [128, 8, ST], BF16)
            RAW = S("RAW", [128, 12, ST + 3], BF16)
            CV = S("CV", [128, 12, ST])
            SIL = S("SIL", [128, 12, ST], BF16)
            SQ = S("SQ", [128, 8, ST], BF16)
            LNT = S("LNT", [128, 8, ST])
            QKVL = [S("QKV%d" % i, [128, 12, ST], BF16) for i in range(2)]
            ZSL = [S("ZS%d" % i, [128, 4, ST], BF16) for i in range(2)]
            ABTL = [S("ABT%d" % i, [128, 2, 8]) for i in range(2)]
            GSTL = [S("GST%d" % i, [128, 2, 4]) for i in range(2)]; BETL = [S("BET%d" % i, [128, 2, 4]) for i in range(2)]
            NBETL = [S("NBET%d" % i, [128, 2, 4]) for i in range(2)]
            SSC = S("SSC", [128, 4]); RSC = S("RSC", [128, 4])
            Sm = [S("Sm%d" % h, [128, 128]) for h in range(4)]
            Sb = [S("Sb%d" % h, [128, 128], BF16) for h in range(4)]
            NSET = 4
            def tset(i):
                t = {}
                for nm in ("gsb", "decS", "decT", "egb", "o1"):
                    t[nm] = S("%s_%d" % (nm, i), [128, 128])
                for nm in ("A", "AT", "P0", "PT0", "P1", "PT1", "RT", "TM", "BKm", "Ym", "kbg", "kdec", "vb", "nwT", "vnew", "attnT", "qdT", "sq"):
                    t[nm] = S("%s_%d" % (nm, i), [128, 128], BF16)
                t["gc"] = S("gc_%d" % i, [128, 8])
                t["BKall"] = S("BKall_%d" % i, [128, 4, 128], BF16)
                return t
            TS = [tset(i) for i in range(NSET)]
            P.add("pool", lambda e: e.memset(RAW[:], 0.0), w=["RAW%d" % c for c in range(12)])
            for h in range(4):
                P.add("pool", (lambda h: lambda e: e.memset(Sm[h][:], 0.0))(h), w=["Sm%d" % h])
                P.add("pool", (lambda h: lambda e: e.memset(Sb[h][:], 0.0))(h), w=["Sb%d" % h])

            xw_t = xw.rearrange("(n p) d -> n p d", p=128)
            PBb0 = PB[0][:].bitcast(BF16)
            PBbH = [PB[4 + h][:].bitcast(BF16) for h in range(4)]

            def do_st(s):
                pending = []
                FA = lambda *a_, **k_: pending.append((a_, k_))
                own = s >= OWN0
                q = s // 8
                par = s % 2; kp = "p%d" % par
                QKVc, ZSc, ABTc, GSTc, BETc, NBETc = QKVL[par], ZSL[par], ABTL[par], GSTL[par], BETL[par], NBETL[par]
                for u in range(2):
                    ti = 2 * s + u
                    xt = XT[u]; xs_ = XS[u]
                    FA("sp", (lambda xt, ti: lambda e: e.dma_start(out=xt[:], in_=xw_t[ti]))(xt, ti), w=["XT%d" % u], dma=True)
                    if stop <= 0.25:
                        continue
                    FA("act", (lambda xt, u: lambda e: e.activation(out=JNK[:], in_=xt[:], func=AF.Square, accum_out=SSC[:, u:u + 1]))(xt, u),
                          r=["XT%d" % u], w=["JNK", "SSC%d" % u])
                    FA("act", (lambda u: lambda e: e.activation(out=RSC[:, u:u + 1], in_=SSC[:, u:u + 1], func=AF.Ln, bias=EPSC[:], scale=1.0 / D))(u),
                          r=["SSC%d" % u, "EPSC"], w=["RSC%d" % u])
                    FA("act", (lambda u: lambda e: e.activation(out=RSC[:, u:u + 1], in_=RSC[:, u:u + 1], func=AF.Exp, scale=-0.5))(u),
                          r=["RSC%d" % u], w=["RSC%d" % u])
                    FA("dve", (lambda xt, xs_, u: lambda e: e.tensor_scalar(out=xs_[:], in0=xt[:], scalar1=RSC[:, u:u + 1], scalar2=None, op0=ALU.mult))(xt, xs_, u),
                          r=["XT%d" % u, "RSC%d" % u], w=["XS%d" % u])
                    if stop <= 0.5:
                        continue
                    FA("pe", (lambda xs_: _grp([(lambda k: lambda e: e.transpose(PBb0[:, k * 128:(k + 1) * 128], xs_[:, k * 128:(k + 1) * 128], IDB[:]))(k)
                                                   for k in range(8)]))(xs_), r=["XS%d" % u, "IDB"], w=bank(0))
                    for k in range(8):
                        eng = "act"
                        if eng == "act":
                            f = (lambda k, u: lambda e: e.activation(out=HT[:, k, u * 128:(u + 1) * 128], in_=PBb0[:, k * 128:(k + 1) * 128],
                                                                     func=AF.Identity, bias=SH1C[:, k:k + 1], scale=A1C[:, k:k + 1]))(k, u)
                        else:
                            f = (lambda k, u: lambda e: e.tensor_scalar(out=HT[:, k, u * 128:(u + 1) * 128], in0=PBb0[:, k * 128:(k + 1) * 128],
                                                                        scalar1=A1C[:, k:k + 1], scalar2=SH1C[:, k:k + 1], op0=ALU.mult, op1=ALU.add))(k, u)
                        FA(eng, f, r=bank(0) + ["A1C", "SH1C"], w=["HT%d_%d" % (k, u)])
                HTK = ["HT%d_%d" % (k, u) for k in range(8) for u in range(2)]
                if stop <= 1:
                    return pending, None
                chunks = list(range(4, 12)) + (list(range(0, 4)) + list(range(12, 16)) if own else ([0, 1, 2, 3] if s == OWN0 - 1 else []))
                for ci, c in enumerate(chunks):
                    slot_b, slot_q = 1 + (ci % 2), 0
                    pso = PB[slot_b][:, 0:ST]
                    FA("pe", (lambda c, pso: _grp([(lambda k: lambda e: e.matmul(pso, lhsT=WA[:, k, c * 128:(c + 1) * 128], rhs=HT[:, k, :],
                                                                                start=(k == 0), stop=(k == 7)))(k) for k in range(8)]))(c, pso),
                          r=HTK + ["WA0"], w=qs(slot_b, 0, 2))
                    if c < 12:
                        dst = RAW[:, c, 3:3 + ST]
                        if own:
                            FA("act", (lambda dst, pso: lambda e: e.copy(out=dst, in_=pso))(dst, pso), r=qs(slot_b, 0, 2), w=["RAW%d" % c])
                        else:
                            FA("dve", (lambda dst, pso, q: lambda e: e.tensor_scalar(out=dst, in0=pso, scalar1=QV[:, q:q + 1], scalar2=None, op0=ALU.mult))(dst, pso, q),
                                  r=qs(slot_b, 0, 2) + ["QV"], w=["RAW%d" % c])
                    else:
                        FA("act", (lambda c, pso: lambda e: e.activation(out=ZSc[:, c - 12, :], in_=pso, func=AF.Silu))(c, pso),
                              r=qs(slot_b, 0, 2), w=["ZS%d" % (c - 12) + kp])
                if stop <= 2:
                    return pending, None
                for u in range(2):
                    FA("pe", (lambda u: _grp([(lambda k: lambda e: e.matmul(PB[3][:, 0:8], lhsT=HT[:, k, u * 128:(u + 1) * 128], rhs=WA[:, k, 2048:2056],
                                                                            start=(k == 0), stop=(k == 7)))(k) for k in range(8)]))(u),
                          r=HTK + ["WA1"], w=qs(3, 0))
                    if own:
                        FA("dve", (lambda u: lambda e: e.tensor_copy(out=ABTc[:, u, :], in_=PB[3][:, 0:8]))(u), r=qs(3, 0), w=["ABT%d" % u + kp])
                    else:
                        FA("dve", (lambda u, q: lambda e: e.tensor_scalar(out=ABTc[:, u, :], in0=PB[3][:, 0:8], scalar1=QV[:, q:q + 1], scalar2=None, op0=ALU.mult))(u, q),
                              r=qs(3, 0) + ["QV"], w=["ABT%d" % u + kp])
                    FA("dve", (lambda u: lambda e: e.tensor_tensor(out=GSTc[:, u, :], in0=ABTc[:, u, 0:4], in1=DTB[:], op=ALU.add))(u), r=["ABT%d" % u + kp, "DTB"], w=["GST%d" % u + kp])
                    FA("act", (lambda u: lambda e: e.activation(out=GSTc[:, u, :], in_=GSTc[:, u, :], func=AF.Exp))(u), r=["GST%d" % u + kp], w=["GST%d" % u + kp])
                    FA("act", (lambda u: lambda e: e.activation(out=GSTc[:, u, :], in_=GSTc[:, u, :], func=AF.Ln, bias=ONF[:, 0:1]))(u), r=["GST%d" % u + kp, "ONF"], w=["GST%d" % u + kp])
                    FA("dve", (lambda u: lambda e: e.tensor_tensor(out=GSTc[:, u, :], in0=GSTc[:, u, :], in1=NEGA[:], op=ALU.mult))(u), r=["GST%d" % u + kp, "NEGA"], w=["GST%d" % u + kp])
                    FA("act", (lambda u: lambda e: e.activation(out=BETc[:, u, :], in_=ABTc[:, u, 4:8], func=AF.Sigmoid))(u), r=["ABT%d" % u + kp], w=["BET%d" % u + kp])
                    FA("dve", (lambda u: lambda e: e.tensor_scalar(out=NBETc[:, u, :], in0=BETc[:, u, :], scalar1=-1.0, scalar2=None, op0=ALU.mult))(u), r=["BET%d" % u + kp], w=["NBET%d" % u + kp])
                if stop <= 3:
                    return pending, None
                for c in chunks:
                    if c >= 12:
                        continue
                    rk = "RAW%d" % c
                    FA("pool", (lambda c: lambda e: e.tensor_scalar(out=CV[:, c, :], in0=RAW[:, c, 0:ST], scalar1=CW[:, c, 0:1], scalar2=None, op0=ALU.mult))(c),
                          r=[rk, "CW"], w=["CV%d" % c])
                    for tp in range(1, 4):
                        FA("dve", (lambda c, tp: lambda e: e.scalar_tensor_tensor(out=CV[:, c, :], in0=RAW[:, c, tp:tp + ST], scalar=CW[:, c, tp:tp + 1],
                                                                                      in1=CV[:, c, :], op0=ALU.mult, op1=ALU.add))(c, tp),
                              r=[rk, "CW", "CV%d" % c], w=["CV%d" % c])
                    FA("pool", (lambda c: lambda e: e.tensor_copy(out=RAW[:, c, 0:3], in_=RAW[:, c, ST:ST + 3]))(c), r=[rk], w=[rk])
                    if c >= 8:
                        FA("act", (lambda c: lambda e: e.activation(out=QKVc[:, c, :], in_=CV[:, c, :], func=AF.Silu))(c), r=["CV%d" % c], w=["QKV%d" % c + kp])
                    else:
                        FA("act", (lambda c: lambda e: e.activation(out=SIL[:, c, :], in_=CV[:, c, :], func=AF.Silu))(c), r=["CV%d" % c], w=["SIL%d" % c])
                        FA("act", (lambda c: lambda e: e.activation(out=SQ[:, c, :], in_=SIL[:, c, :], func=AF.Square))(c), r=["SIL%d" % c], w=["SQ%d" % c])
                        FA("pe", (lambda c: lambda e: e.matmul(PB[3][:, ST:2 * ST], lhsT=ONB[:], rhs=SQ[:, c, :], start=True, stop=True))(c),
                              r=["SQ%d" % c, "ONB"], w=qs(3, 2, 2))
                        FA("act", (lambda c: lambda e: e.activation(out=LNT[:, c, :], in_=PB[3][:, ST:2 * ST], func=AF.Ln, bias=EPSC[:]))(c),
                              r=qs(3, 2, 2) + ["EPSC"], w=["LNT%d" % c])
                        FA("act", (lambda c: lambda e: e.activation(out=LNT[:, c, :], in_=LNT[:, c, :], func=AF.Exp, scale=-0.5))(c), r=["LNT%d" % c], w=["LNT%d" % c])
                        sc_ = (128.0 ** -0.5) if c < 4 else 1.0
                        FA("dve", (lambda c, sc_: lambda e: e.scalar_tensor_tensor(out=QKVc[:, c, :], in0=SIL[:, c, :], scalar=sc_, in1=LNT[:, c, :],
                                                                                      op0=ALU.mult, op1=ALU.mult))(c, sc_),
                              r=["SIL%d" % c, "LNT%d" % c], w=["QKV%d" % c + kp])
                if stop <= 4:
                    return pending, None
                def chain(u, h):
                    cs = slice(u * 128, (u + 1) * 128)
                    t = TS[h]; tk = "_%d" % h
                    K = lambda n: n + tk
                    hb = 4 + h; Hf = PB[hb]; H16 = PBbH[h]
                    Q = lambda q_: Hf[:, q_ * 128:(q_ + 1) * 128]
                    QK = lambda q_: qs(hb, q_)
                    kT = QKVc[:, 4 + h, cs]; vT = QKVc[:, 8 + h, cs]; qT = QKVc[:, h, cs]
                    kk, vk, qk = "QKV%d" % (4 + h) + kp, "QKV%d" % (8 + h) + kp, "QKV%d" % h + kp
                    gcol = GSTc[:, u, h:h + 1]
                    gk, bk_, nbk = "GST%d" % u + kp, "BET%d" % u + kp, "NBET%d" % u + kp
                    gc = t["gc"]
                    BETl, NBETl, ZSl = BETc, NBETc, ZSc
                    P.add("dve", lambda e: e.tensor_scalar(out=t["gsb"][:], in0=ONF[:], scalar1=gcol, scalar2=None, op0=ALU.mult), r=["ONF", gk], w=[K("gsb")])
                    yield
                    P.add("pe", _grp([lambda e: e.matmul(Q(0), lhsT=t["gsb"][:], rhs=UF[:], start=True, stop=True),
                                      lambda e: e.matmul(Hf[:, 128:129], lhsT=UF[:], rhs=gcol, start=True, stop=True),
                                      lambda e: e.matmul(Hf[:, 129:130], lhsT=ONF[:], rhs=gcol, start=True, stop=True)]),
                          r=[K("gsb"), "UF", "ONF", gk], w=QK(0) + QK(1))
                    P.add("dve", lambda e: e.tensor_copy(out=gc[:, 0:2], in_=Hf[:, 128:130]), r=QK(1), w=[K("gc")])
                    if own:
                        P.add("act", lambda e: e.activation(out=t["egb"][:], in_=Q(0), func=AF.Exp), r=QK(0), w=[K("egb")])
                    yield
                    P.add("dve", lambda e: e.tensor_scalar(out=gc[:, 2:3], in0=gc[:, 0:1], scalar1=-1.0, scalar2=None, op0=ALU.mult), r=[K("gc")], w=[K("gc2")])
                    P.add("dve", lambda e: e.tensor_tensor(out=gc[:, 3:4], in0=gc[:, 1:2], in1=gc[:, 0:1], op=ALU.subtract), r=[K("gc")], w=[K("gc3")])
                    P.add("act", lambda e: e.activation(out=gc[:, 4:5], in_=gc[:, 0:1], func=AF.Exp), r=[K("gc")], w=[K("gc4")])
                    P.add("act", lambda e: e.activation(out=gc[:, 6:7], in_=gc[:, 1:2], func=AF.Exp), r=[K("gc")], w=[K("gc6")])
                    yield
                    P.add("dve", lambda e: e.tensor_tensor(out=gc[:, 4:5], in0=gc[:, 4:5], in1=BETl[:, u, h:h + 1], op=ALU.mult), r=[K("gc4"), bk_], w=[K("gc4")])
                    P.add("act", lambda e: e.activation(out=gc[:, 5:6], in_=gc[:, 3:4], func=AF.Exp), r=[K("gc3")], w=[K("gc5")])
                    P.add("pe", _grp([lambda e: e.matmul(Q(2), lhsT=t["gsb"][:], rhs=UF[:], start=True, stop=False),
                                      lambda e: e.matmul(Q(2), lhsT=IDF[:], rhs=NM2[:], start=False, stop=True)]),
                          r=[K("gsb"), "UF", "IDF", "NM2"], w=QK(2))
                    P.add("act", lambda e: e.activation(out=t["decS"][:], in_=Q(2), func=AF.Exp, bias=gc[:, 0:1], scale=-1.0), r=QK(2) + [K("gc")], w=[K("decS")])
                    yield
                    P.add("pe", _grp([lambda e: e.transpose(H16[:, 768:896], kT, IDB[:]), lambda e: e.transpose(H16[:, 896:1024], vT, IDB[:])]),
                          r=[kk, vk, "IDB"], w=QK(3))
                    P.add("dve", lambda e: e.tensor_scalar(out=t["kbg"][:], in0=H16[:, 768:896], scalar1=gc[:, 4:5], scalar2=None, op0=ALU.mult), r=QK(3) + [K("gc4")], w=[K("kbg")])
                    P.add("dve", lambda e: e.tensor_scalar(out=t["kdec"][:], in0=H16[:, 768:896], scalar1=gc[:, 5:6], scalar2=None, op0=ALU.mult), r=QK(3) + [K("gc5")], w=[K("kdec")])
                    P.add("dve", lambda e: e.tensor_scalar(out=t["vb"][:], in0=H16[:, 896:1024], scalar1=BETl[:, u, h:h + 1], scalar2=None, op0=ALU.mult), r=QK(3) + [bk_], w=[K("vb")])
                    yield
                    P.add("pe", lambda e: e.matmul(Q(2), lhsT=kT, rhs=kT, start=True, stop=True), r=[kk], w=QK(2))
                    P.add("dve", lambda e: e.scalar_tensor_tensor(out=t["A"][:], in0=Q(2), scalar=NBETl[:, u, h:h + 1], in1=t["decS"][:], op0=ALU.mult, op1=ALU.mult),
                          r=QK(2) + [nbk, K("decS")], w=[K("A")])
                    yield
                    P.add("pe", lambda e: e.transpose(H16[:, 768:896], t["A"][:], IDB[:]), r=[K("A"), "IDB"], w=QK(3))
                    P.add("dve", lambda e: e.tensor_copy(out=t["AT"][:], in_=H16[:, 768:896]), r=QK(3), w=[K("AT")])
                    P.add("dve", lambda e: e.tensor_tensor(out=t["P0"][:], in0=t["A"][:], in1=IMK[:, 0, :], op=ALU.mult), r=[K("A"), "IMK"], w=[K("P0")])
                    P.add("pool", lambda e: e.tensor_tensor(out=t["TM"][:], in0=t["P0"][:], in1=IDB[:], op=ALU.add), r=[K("P0"), "IDB"], w=[K("TM")])
                    yield
                    P.add("dve", lambda e: e.tensor_tensor(out=t["PT0"][:], in0=t["AT"][:], in1=IMK[:, 0, :], op=ALU.mult), r=[K("AT"), "IMK"], w=[K("PT0")])
                    P.add("pool", lambda e: e.tensor_tensor(out=t["RT"][:], in0=t["PT0"][:], in1=IDB[:], op=ALU.add), r=[K("PT0"), "IDB"], w=[K("RT")])
                    P.add("dve", lambda e: e.tensor_tensor(out=t["BKall"][:], in0=t["AT"][:].unsqueeze(1).to_broadcast([128, 4, 128]), in1=IMK[:, 1:5, :], op=ALU.mult),
                          r=[K("AT"), "IMK"], w=[K("BKall")])
                    yield

                    def upd(ln, with_T):
                        P.add("pe", lambda e: e.matmul(Q(2), lhsT=t[ln][:], rhs=t["RT"][:], start=True, stop=True), r=[K(ln), K("RT")], w=QK(2))
                        if with_T:
                            P.add("pe", lambda e: e.matmul(Q(3), lhsT=t["RT"][:], rhs=t[ln][:], start=True, stop=True), r=[K(ln), K("RT")], w=QK(3))
                        P.add("dve", lambda e: e.tensor_tensor(out=t["RT"][:], in0=t["RT"][:], in1=Q(2), op=ALU.add), r=QK(2) + [K("RT")], w=[K("RT")])
                        if with_T:
                            P.add("dve", lambda e: e.tensor_tensor(out=t["TM"][:], in0=t["TM"][:], in1=Q(3), op=ALU.add), r=QK(3) + [K("TM")], w=[K("TM")])
                    P.add("pe", lambda e: e.matmul(Q(0), lhsT=t["PT0"][:], rhs=t["P0"][:], start=True, stop=True), r=[K("P0"), K("PT0")], w=QK(0))
                    P.add("pe", lambda e: e.matmul(Q(1), lhsT=t["P0"][:], rhs=t["PT0"][:], start=True, stop=True), r=[K("P0"), K("PT0")], w=QK(1))
                    P.add("act", lambda e: e.copy(out=t["P1"][:], in_=Q(0)), r=QK(0), w=[K("P1")])
                    P.add("act", lambda e: e.copy(out=t["PT1"][:], in_=Q(1)), r=QK(1), w=[K("PT1")])
                    yield
                    upd("P1", True)
                    yield
                    P.add("pe", lambda e: e.matmul(Q(0), lhsT=t["PT1"][:], rhs=t["P1"][:], start=True, stop=True), r=[K("P1"), K("PT1")], w=QK(0))
                    P.add("act", lambda e: e.copy(out=t["P0"][:], in_=Q(0)), r=QK(0), w=[K("P0")])
                    yield
                    upd("P0", True)
                    yield
                    for lv in range(1, 5):
                        P.add("pe", (lambda lv: lambda e: e.matmul(Q(0), lhsT=t["BKall"][:, lv - 1, :], rhs=t["TM"][:], start=True, stop=True))(lv), r=[K("BKall"), K("TM")], w=QK(0))
                        P.add("act", lambda e: e.copy(out=t["Ym"][:], in_=Q(0)), r=QK(0), w=[K("Ym")])
                        yield
                        upd("Ym", lv < 4)
                        yield
                    P.add("pe", lambda e: e.matmul(Q(0), lhsT=t["kbg"][:], rhs=t["RT"][:], start=True, stop=True), r=[K("kbg"), K("RT")], w=QK(0))
                    P.add("act", lambda e: e.activation(out=t["nwT"][:], in_=Q(0), func=AF.Copy, scale=-1.0), r=QK(0), w=[K("nwT")])
                    yield
                    P.add("pe", _grp([lambda e: e.matmul(Q(1), lhsT=t["RT"][:], rhs=t["vb"][:], start=True, stop=False),
                                      lambda e: e.matmul(Q(1), lhsT=t["nwT"][:], rhs=Sb[h][:], start=False, stop=True)]),
                          r=[K("RT"), K("vb"), K("nwT"), "Sb%d" % h], w=QK(1))
                    P.add("act", lambda e: e.copy(out=t["vnew"][:], in_=Q(1)), r=QK(1), w=[K("vnew")])
                    yield
                    if own:
                        qt_ = (s - OWN0) * 2 + u
                        ocs = slice(qt_ * 128, (qt_ + 1) * 128)
                        P.add("pe", _grp([lambda e: e.matmul(Q(2), lhsT=t["gsb"][:], rhs=UF[:], start=True, stop=False),
                                          lambda e: e.matmul(Q(2), lhsT=IDF[:], rhs=NMT[:], start=False, stop=True)]),
                              r=[K("gsb"), "UF", "IDF", "NMT"], w=QK(2))
                        P.add("pe", lambda e: e.matmul(Q(3), lhsT=kT, rhs=qT, start=True, stop=True), r=[kk, qk], w=QK(3))
                        P.add("act", lambda e: e.activation(out=t["decT"][:], in_=Q(2), func=AF.Exp, bias=gc[:, 2:3]), r=QK(2) + [K("gc2")], w=[K("decT")])
                        P.add("pool", lambda e: e.tensor_tensor(out=t["qdT"][:], in0=qT, in1=t["egb"][:], op=ALU.mult), r=[qk, K("egb")], w=[K("qdT")])
                        yield
                        P.add("dve", lambda e: e.tensor_tensor(out=t["attnT"][:], in0=Q(3), in1=t["decT"][:], op=ALU.mult), r=QK(3) + [K("decT")], w=[K("attnT")])
                        yield
                        P.add("pe", _grp([lambda e: e.matmul(Q(0), lhsT=Sb[h][:], rhs=t["qdT"][:], start=True, stop=False),
                                          lambda e: e.matmul(Q(0), lhsT=t["vnew"][:], rhs=t["attnT"][:], start=False, stop=True)]),
                              r=["Sb%d" % h, K("qdT"), K("vnew"), K("attnT")], w=QK(0))
                        P.add("act", lambda e: e.activation(out=t["sq"][:], in_=Q(0), func=AF.Square), r=QK(0), w=[K("sq")])
                        P.add("act", lambda e: e.copy(out=t["o1"][:], in_=Q(0)), r=QK(0), w=[K("o1")])
                        yield
                        P.add("pe", lambda e: e.matmul(Q(1), lhsT=ONB[:], rhs=t["sq"][:], start=True, stop=True), r=[K("sq"), "ONB"], w=QK(1))
                        P.add("act", lambda e: e.activation(out=t["egb"][:], in_=Q(1), func=AF.Ln, bias=EPSC[:], scale=1.0 / 128), r=QK(1) + ["EPSC", K("qdT")], w=[K("egb")])
                        P.add("act", lambda e: e.activation(out=t["egb"][:], in_=t["egb"][:], func=AF.Exp, scale=-0.5), r=[K("egb")], w=[K("egb")])
                        yield
                        P.add("dve", lambda e: e.tensor_tensor(out=t["o1"][:], in0=t["o1"][:], in1=t["egb"][:], op=ALU.mult), r=[K("o1"), K("egb")], w=[K("o1")])
                        P.add("dve", lambda e: e.scalar_tensor_tensor(out=OTA[:, h, ocs], in0=t["o1"][:], scalar=ANW[:, 0:1], in1=ZSl[:, h, cs], op0=ALU.mult, op1=ALU.mult),
                              r=[K("o1"), "ANW", "ZS%d" % h + kp], w=["OTA%d_%d" % (h, qt_)])
                        yield
                    P.add("pe", lambda e: e.matmul(Q(2), lhsT=t["kdec"][:], rhs=t["vnew"][:], start=True, stop=True), r=[K("kdec"), K("vnew")], w=QK(2))
                    P.add("dve", lambda e: e.scalar_tensor_tensor(out=Sm[h][:], in0=Sm[h][:], scalar=gc[:, 6:7], in1=Q(2), op0=ALU.mult, op1=ALU.add),
                          r=QK(2) + [K("gc6"), "Sm%d" % h], w=["Sm%d" % h])
                    P.add("act", lambda e: e.copy(out=Sb[h][:], in_=Sm[h][:]), r=["Sm%d" % h], w=["Sb%d" % h])
                    yield

                return pending, chain

            def run_round(chain_fn, pend):
                pi = 0
                if chain_fn is not None:
                    per = max(1, -(-len(pend) // 60))
                    for u in range(2):
                        gens = [chain_fn(u, h) for h in range(4)]
                        while gens:
                            for g_ in list(gens):
                                try:
                                    next(g_)
                                except StopIteration:
                                    gens.remove(g_)
                            for _ in range(per):
                                if pi < len(pend):
                                    P.add(*pend[pi][0], **pend[pi][1]); pi += 1
                while pi < len(pend):
                    P.add(*pend[pi][0], **pend[pi][1]); pi += 1

            prev_chain = None
            for s_ in list(a_list) + [None]:
                pend, ch = do_st(s_) if s_ is not None else ([], None)
                run_round(prev_chain, pend)
                prev_chain = ch
            fin = []
            if dbg:
                for nm, tsr in (("HT", HT), ("QKV", QKVL[0]), ("RAW", RAW), ("ZS", ZSL[0]), ("GST", GSTL[0]), ("BET", BETL[0]), ("ABT", ABTL[0]), ("CV", CV),
                                ("A1C", A1C), ("SH1C", SH1C), ("Sm0", Sm[0]), ("decS", TS[DS]["decS"]), ("A", TS[DS]["A"]), ("RT", TS[DS]["RT"]),
                                ("vnew", TS[DS]["vnew"]), ("gc", TS[DS]["gc"]), ("kbg", TS[DS]["kbg"]), ("kdec", TS[DS]["kdec"]), ("nwT", TS[DS]["nwT"]),
                                ("decT", TS[DS]["decT"]), ("attnT", TS[DS]["attnT"]), ("qdT", TS[DS]["qdT"]), ("o1", TS[DS]["o1"])):
                    dd = nc.dram_tensor("dump_" + nm, list(tsr.shape), F32, kind="ExternalOutput").ap()
                    P.add("pool", (lambda dd, tsr: lambda e: e.dma_start(out=dd, in_=tsr[:]))(dd, tsr), w=["dump_" + nm], dma=True, after_all=True)
                    fin.append("dump_" + nm)
                P.add("pool", lambda e: e.dma_start(out=dbg_o["ota"], in_=OTA[:]), w=["dbg_ota"], dma=True, after_all=True)
                fin.append("dbg_ota")
            P.emit(st, final_wait_keys=fin)
            print("phase A ops:", len(P.ops))

        with ExitStack() as st:
            S = mk(st)
            P = Prog(nc)
            WB = S("WB", [128, 8, 1536], BF16); WO = S("WO", [128, 8, D], BF16)
            BM5 = S("BM5", [128, 8, 640]); KR = S("KR", [128, 4, 1024], BF16); VR = S("VR", [128, 8, 512], BF16)
            WRF = S("WRF", [128, 8, NE]); BR = S("BR", [128, NE]); KM = S("KM", [128, 1]); G1B = S("G1B", [128, D])
            XT = [S("XTb%d" % i, [128, D]) for i in range(2)]
            JNK = S("JNKb", [128, D], BF16)
            XS = [S("XSb%d" % i, [128, D], BF16) for i in range(2)]
            HT = S("HTb", [128, 8, ST], BF16)
            QB = S("QB", [128, 4, ST], BF16)
            STSL = [S("STS%d" % i, [128, 640]) for i in range(2)]; PTL = [S("PT%d" % i, [128, 640], BF16) for i in range(2)]
            RDENL = [S("RDEN%d" % i, [64, 128]) for i in range(2)]
            OTB = S("OTB", [128, 4, ST], BF16)
            T1 = S("T1", [128, D]); H2 = S("H2", [128, D]); H2F = S("H2F", [128, 8, 128]); H2B = S("H2B", [128, 8, 128], BF16)
            SSC = S("SSCb", [128, 8]); RSC = S("RSCb", [128, 8])
            w_in_r = w_in.rearrange("(k p) c -> p k c", p=128)
            P.add("pool", lambda e: e.dma_start(out=WB[:], in_=w_in_r[:, :, 2056:3592]), w=["WB"], dma=True)
            P.add("pool", lambda e: e.dma_start(out=WO[:], in_=w_out.rearrange("(k p) c -> p k c", p=128)), w=["WO"], dma=True)
            P.add("sp", lambda e: e.dma_start(out=BM5[:], in_=bm5_d), w=["BM5"], dma=True)
            P.add("sp", lambda e: e.dma_start(out=WRF[:], in_=w_router.rearrange("(k p) c -> p k c", p=128)), w=["WRF"], dma=True)
            P.add("sp", lambda e: e.dma_start(out=BR[:], in_=br_d), w=["BR"], dma=True)
            P.add("sp", lambda e: e.dma_start(out=KM[:], in_=kmask_d), w=["KM"], dma=True)
            for half in range(2):
                P.add("pe", (lambda half: lambda e: e.matmul(PB[1][:, :], lhsT=ONF[0:1, :], rhs=G1R[0:1, half * 512:(half + 1) * 512], start=True, stop=True))(half),
                      r=[], w=bank(1))
                P.add("dve", (lambda half: lambda e: e.tensor_copy(out=G1B[:, half * 512:(half + 1) * 512], in_=PB[1][:, :]))(half), r=bank(1), w=["G1B"])
            xw_t = xw.rearrange("(n p) d -> n p d", p=128)
            x1s_t = x1s.rearrange("(n p) d -> n p d", p=128)
            PBb0 = PB[0][:].bitcast(BF16)
            for s in (range(HALO0, NST) if do_b else []):
                own = s >= OWN0
                for u in range(2):
                    ti = 2 * s + u
                    xt = XT[u]; xs_ = XS[u]
                    P.add("sp", (lambda xt, ti: lambda e: e.dma_start(out=xt[:], in_=xw_t[ti]))(xt, ti), w=["XT%d" % u], dma=True)
                    P.add("act", (lambda xt, u: lambda e: e.activation(out=JNK[:], in_=xt[:], func=AF.Square, accum_out=SSC[:, u:u + 1]))(xt, u),
                          r=["XT%d" % u], w=["JNK", "SSC%d" % u])
                    P.add("act", (lambda u: lambda e: e.activation(out=RSC[:, u:u + 1], in_=SSC[:, u:u + 1], func=AF.Ln, bias=EPSC[:], scale=1.0 / D))(u),
                          r=["SSC%d" % u], w=["RSC%d" % u])
                    P.add("act", (lambda u: lambda e: e.activation(out=RSC[:, u:u + 1], in_=RSC[:, u:u + 1], func=AF.Exp, scale=-0.5))(u),
                          r=["RSC%d" % u], w=["RSC%d" % u])
                    P.add("dve", (lambda xt, xs_, u: lambda e: e.tensor_scalar(out=xs_[:], in0=xt[:], scalar1=RSC[:, u:u + 1], scalar2=None, op0=ALU.mult))(xt, xs_, u),
                          r=["XT%d" % u, "RSC%d" % u], w=["XS%d" % u])
                    P.add("pe", (lambda xs_: _grp([(lambda k: lambda e: e.transpose(PBb0[:, k * 128:(k + 1) * 128], xs_[:, k * 128:(k + 1) * 128], IDB[:]))(k)
                                                   for k in range(8)]))(xs_), r=["XS%d" % u], w=bank(0))
                    for k in range(8):
                        eng = "act"
                        if eng == "act":
                            f = (lambda k, u: lambda e: e.activation(out=HT[:, k, u * 128:(u + 1) * 128], in_=PBb0[:, k * 128:(k + 1) * 128],
                                                                     func=AF.Identity, bias=SH1C[:, k:k + 1], scale=A1C[:, k:k + 1]))(k, u)
                        else:
                            f = (lambda k, u: lambda e: e.tensor_scalar(out=HT[:, k, u * 128:(u + 1) * 128], in0=PBb0[:, k * 128:(k + 1) * 128],
                                                                        scalar1=A1C[:, k:k + 1], scalar2=SH1C[:, k:k + 1], op0=ALU.mult, op1=ALU.add))(k, u)
                        P.add(eng, f, r=bank(0), w=["HT%d_%d" % (k, u)])
                HTK = ["HT%d_%d" % (k, u) for k in range(8) for u in range(2)]
                slot0 = (2 * s) % 8
                for p in range(4):
                    P.add("pe", (lambda p: _grp([(lambda k: lambda e: e.matmul(PB[1][:, 0:ST], lhsT=WB[:, k, 512 + p * 128:512 + (p + 1) * 128], rhs=HT[:, k, :],
                                                                            start=(k == 0), stop=(k == 7)))(k) for k in range(8)]))(p),
                          r=HTK + ["WB"], w=qs(1, 0, 2))
                    P.add("act", (lambda p, slot0: lambda e: e.copy(out=KR[:, p, slot0 * 128:slot0 * 128 + ST], in_=PB[1][:, 0:ST]))(p, slot0),
                          r=qs(1, 0, 2), w=["KR%d_%d" % (p, slot0), "KR%d_%d" % (p, slot0 + 1)])
                    if own:
                        P.add("pe", (lambda p: _grp([(lambda k: lambda e: e.matmul(PB[1][:, ST:2 * ST], lhsT=WB[:, k, p * 128:(p + 1) * 128], rhs=HT[:, k, :],
                                                                                start=(k == 0), stop=(k == 7)))(k) for k in range(8)]))(p),
                              r=HTK + ["WB"], w=qs(1, 2, 2))
                        P.add("act", (lambda p: lambda e: e.activation(out=QB[:, p, :], in_=PB[1][:, ST:2 * ST], func=AF.Copy, scale=0.125))(p),
                              r=qs(1, 2, 2), w=["QB%d" % p])
                for u in range(2):
                    P.add("pe", (lambda u: _grp([(lambda k: lambda e: e.matmul(PB[2][:, :], lhsT=HT[:, k, u * 128:(u + 1) * 128], rhs=WB[:, k, 1024:1536],
                                                                            start=(k == 0), stop=(k == 7)))(k) for k in range(8)]))(u),
                          r=HTK + ["WB"], w=bank(2))
                    P.add("dve", (lambda u, slot0: lambda e: e.tensor_copy(out=VR[:, slot0 + u, :], in_=PB[2][:, :]))(u, slot0), r=bank(2), w=["VR%d" % (slot0 + u)])
                if not own:
                    continue
                for u in range(2):
                    qt_ = (s - OWN0) * 2 + u
                    W = 48 + qt_
                    slots = [(W - 4 + t) % 8 for t in range(5)]
                    ucs = slice(u * 128, (u + 1) * 128)
                    nh = max(0, 4 - qt_)
                    def attn(hb, bs, slots, ucs, nh, u):
                        p, r0 = hb // 2, (hb % 2) * 64
                        ba, bb = 3 + 2 * bs, 4 + 2 * bs
                        STSs, PTs, RDENs = STSL[bs], PTL[bs], RDENL[bs]
                        sk = "_%d" % bs
                        fns = []
                        for t in range(5):
                            o_ = PB[ba][:, t * 128:(t + 1) * 128] if t < 4 else PB[bb][:, 0:128]
                            fns.append((lambda o_, sl: lambda e: e.matmul(o_, lhsT=KR[r0:r0 + 64, p, sl * 128:(sl + 1) * 128], rhs=QB[r0:r0 + 64, p, ucs],
                                                                          start=True, stop=True))(o_, slots[t]))
                        P.add("pe", _grp(fns), r=["QB%d" % p] + ["KR%d_%d" % (p, sl) for sl in slots], w=bank(ba) + qs(bb, 0))
                        yield
                        P.add("dve", lambda e: e.tensor_tensor(out=STSs[:, 0:512], in0=PB[ba][:, :], in1=BM5[:, hb, 0:512], op=ALU.add), r=bank(ba) + ["BM5"], w=["STSa" + sk])
                        P.add("dve", lambda e: e.tensor_tensor(out=STSs[:, 512:640], in0=PB[bb][:, 0:128], in1=BM5[:, hb, 512:640], op=ALU.add), r=qs(bb, 0) + ["BM5"], w=["STSb" + sk])
                        yield
                        if nh > 0:
                            P.add("act", lambda e: e.activation(out=PTs[:, 0:nh * 128], in_=STSs[:, 0:nh * 128], func=AF.Exp, bias=KM[:, 0:1]), r=["STSa" + sk, "KM"], w=["PTa" + sk])
                        P.add("act", lambda e: e.activation(out=PTs[:, nh * 128:640], in_=STSs[:, nh * 128:640], func=AF.Exp), r=["STSa" + sk, "STSb" + sk], w=["PTb" + sk])
                        yield
                        fns = [(lambda t, sl: lambda e: e.matmul(PB[bb][0:64, 128:256], lhsT=VR[:, sl, hb * 64:(hb + 1) * 64], rhs=PTs[:, t * 128:(t + 1) * 128],
                                                                 start=(t == 0), stop=(t == 4)))(t, slots[t]) for t in range(5)]
                        P.add("pe", _grp(fns), r=["PTa" + sk, "PTb" + sk] + ["VR%d" % sl for sl in slots], w=qs(bb, 1))
                        fns = [(lambda t: lambda e: e.matmul(PB[bb][0:64, 256:384], lhsT=ONB[:, 0:64], rhs=PTs[:, t * 128:(t + 1) * 128],
                                                             start=(t == 0), stop=(t == 4)))(t) for t in range(5)]
                        P.add("pe", _grp(fns), r=["PTa" + sk, "PTb" + sk], w=qs(bb, 2))
                        yield
                        P.add("dve", lambda e: e.reciprocal(out=RDENs[:], in_=PB[bb][0:64, 256:384]), r=qs(bb, 2), w=["RDEN" + sk])
                        P.add("dve", lambda e: e.tensor_tensor(out=OTB[r0:r0 + 64, p, ucs], in0=PB[bb][0:64, 128:256], in1=RDENs[:], op=ALU.mult),
                              r=qs(bb, 1) + ["RDEN" + sk], w=["OTB%d_%d_%d" % (p, u, hb % 2)])
                        yield

                    for hp in range(4):
                        gens = [attn(2 * hp + z, z, slots, ucs, nh, u) for z in range(2)]
                        while gens:
                            for g_ in list(gens):
                                try:
                                    next(g_)
                                except StopIteration:
                                    gens.remove(g_)
                    ocs = slice(qt_ * 128, (qt_ + 1) * 128)
                    for half in range(2):
                        fns = []
                        for c in range(8):
                            l_ = OTA[:, c, ocs] if c < 4 else OTB[:, c - 4, ucs]
                            fns.append((lambda c, l_, half: lambda e: e.matmul(PB[5 + half][:, :], lhsT=l_, rhs=WO[:, c, half * 512:(half + 1) * 512],
                                                                               start=(c == 0), stop=(c == 7)))(c, l_, half))
                        P.add("pe", _grp(fns), r=["WO"] + ["OTA%d_%d" % (h, qt_) for h in range(4)] + ["OTB%d_%d_%d" % (p, u, z) for p in range(4) for z in range(2)],
                              w=bank(5 + half))
                        P.add("act", (lambda half: lambda e: e.activation(out=JNK[:, half * 512:(half + 1) * 512], in_=PB[5 + half][:, :], func=AF.Square,
                                                                          accum_out=SSC[:, 2 + half:3 + half]))(half), r=bank(5 + half), w=["JNK", "SSC%d" % (2 + half)])
                    P.add("dve", lambda e: e.tensor_tensor(out=SSC[:, 4:5], in0=SSC[:, 2:3], in1=SSC[:, 3:4], op=ALU.add), r=["SSC2", "SSC3"], w=["SSC4"])
                    P.add("act", lambda e: e.activation(out=RSC[:, 4:5], in_=SSC[:, 4:5], func=AF.Ln, bias=EPSC[:], scale=1.0 / D), r=["SSC4"], w=["RSC4"])
                    P.add("act", lambda e: e.activation(out=RSC[:, 4:5], in_=RSC[:, 4:5], func=AF.Exp, scale=-0.5), r=["RSC4"], w=["RSC4"])
                    for half in range(2):
                        hs = slice(half * 512, (half + 1) * 512)
                        P.add("dve", (lambda half, hs: lambda e: e.scalar_tensor_tensor(out=T1[:, hs], in0=PB[5 + half][:, :], scalar=RSC[:, 4:5], in1=G1B[:, hs],
                                                                                        op0=ALU.mult, op1=ALU.mult))(half, hs),
                              r=bank(5 + half) + ["RSC4", "G1B"], w=["T1_%d" % half])
                    P.add("pool", (lambda u: lambda e: e.tensor_tensor(out=T1[:], in0=T1[:], in1=XT[u][:], op=ALU.add))(u), r=["T1_0", "T1_1", "XT%d" % u], w=["T1_0", "T1_1"])
                    P.add("sp", (lambda qt_: lambda e: e.dma_start(out=x1s_t[qt_], in_=T1[:]))(qt_), r=["T1_0", "T1_1"], w=["x1s"], dma=True)
                    if dbg:
                        P.add("sp", (lambda qt_: lambda e: e.dma_start(out=dbg_o["x1"].rearrange("(n p) d -> n p d", p=128)[qt_], in_=T1[:]))(qt_), r=["T1_0", "T1_1"], w=["dbg_x1"], dma=True)
                    P.add("act", lambda e: e.activation(out=JNK[:], in_=T1[:], func=AF.Square, accum_out=SSC[:, 5:6]), r=["T1_0", "T1_1"], w=["JNK", "SSC5"])
                    P.add("act", lambda e: e.activation(out=RSC[:, 5:6], in_=SSC[:, 5:6], func=AF.Ln, bias=EPSC[:], scale=1.0 / D), r=["SSC5"], w=["RSC5"])
                    P.add("act", lambda e: e.activation(out=RSC[:, 5:6], in_=RSC[:, 5:6], func=AF.Exp, scale=-0.5), r=["RSC5"], w=["RSC5"])
                    P.add("dve", lambda e: e.tensor_scalar(out=H2[:], in0=T1[:], scalar1=RSC[:, 5:6], scalar2=None, op0=ALU.mult), r=["T1_0", "T1_1", "RSC5"], w=["H2"])
                    for half in range(2):
                        P.add("pe", (lambda half: _grp([(lambda k: lambda e: e.transpose(PB[5 + half][:, (k % 4) * 128:(k % 4 + 1) * 128], H2[:, k * 128:(k + 1) * 128], IDF[:]))(k)
                                                        for k in range(half * 4, half * 4 + 4)]))(half), r=["H2"], w=bank(5 + half))
                        for k in range(half * 4, half * 4 + 4):
                            eng = "act"
                            src = PB[5 + half][:, (k % 4) * 128:(k % 4 + 1) * 128]
                            if eng == "act":
                                f = (lambda k, src: lambda e: e.activation(out=H2F[:, k, :], in_=src, func=AF.Identity, bias=SH2C[:, k:k + 1], scale=A2C[:, k:k + 1]))(k, src)
                            else:
                                f = (lambda k, src: lambda e: e.tensor_scalar(out=H2F[:, k, :], in0=src, scalar1=A2C[:, k:k + 1], scalar2=SH2C[:, k:k + 1], op0=ALU.mult, op1=ALU.add))(k, src)
                            P.add(eng, f, r=bank(5 + half), w=["H2F%d" % k])
                    H2FK = ["H2F%d" % k for k in range(8)]
                    P.add("pool", lambda e: e.tensor_copy(out=H2B[:], in_=H2F[:]), r=H2FK, w=["H2B"])
                    P.add("sp", (lambda ocs: lambda e: e.dma_start(out=h2s[:, :, ocs], in_=H2B[:]))(ocs), r=["H2B"], w=["h2s"], dma=True)
                    P.add("pe", _grp([(lambda k: lambda e: e.matmul(PB[7][:, 0:NE], lhsT=H2F[:, k, :], rhs=WRF[:, k, :], start=(k == 0), stop=(k == 7)))(k) for k in range(8)]),
                          r=H2FK + ["WRF"], w=qs(7, 0))
                    P.add("dve", (lambda qt_: lambda e: e.tensor_tensor(out=LG[:, qt_, :], in0=PB[7][:, 0:NE], in1=BR[:], op=ALU.add))(qt_), r=qs(7, 0) + ["BR"], w=["LG"])
            fin = ["x1s", "h2s"]
            if dbg and do_b:
                P.add("sp", lambda e: e.dma_start(out=dbg_o["lg"], in_=LG[:]), r=["LG"], w=["dbg_lg"], dma=True)
                fin += ["dbg_lg", "dbg_x1"]
            P.emit(st, final_wait_keys=fin)
            print("phase B ops:", len(P.ops))

        stAB.close()
        with ExitStack() as st:
            S = mk(st)
            P = Prog(nc)
            H2T = S("H2T", [128, 8, NTOK], BF16); Y = S("Y", [128, 16, D]); G = S("G", [128, 16, NE])
            WU = S("WU", [128, 8, 2 * D], BF16); WD = S("WD", [128, 8, D], BF16); ACTT = S("ACTT", [128, 8, 1024], BF16)
            BU = S("BU", [128, NE, 16]); BDN = S("BDN", [NE, D]); GT = S("GT", [NE, 128])
            TG = [S("TG%d" % i, [128, 512]) for i in range(2)]
            TSg = [S("TSg%d" % i, [128, 512], BF16) for i in range(2)]
            TL = [S("TL%d" % i, [128, 512]) for i in range(2)]
            G2B = S("G2B", [128, D]); X1T = S("X1T", [128, D]); JNK = S("JNKc", [128, D], BF16)
            T8 = S("T8", [128, 8]); MSK = S("MSK", [128, NE]); EX = S("EX", [128, NE]); CL = S("CL", [128, 8])
            P.add("sp", lambda e: e.dma_start(out=BU[:], in_=bu_d), w=["BU"], dma=True)
            P.add("dve", lambda e: e.tensor_scalar(out=BU[:, :, 8:16], in0=BU[:, :, 8:16], scalar1=1.0, scalar2=None, op0=ALU.add), r=["BU"], w=["BU"])
            P.add("sp", lambda e: e.dma_start(out=BDN[0:n_exp, :], in_=b_down), w=["BDN"], dma=True)
            for hh in range(2):
                P.add("sp", (lambda hh: lambda e: e.dma_start(out=H2T[:, :, hh * 1024:(hh + 1) * 1024], in_=h2s[:, :, hh * 1024:(hh + 1) * 1024]))(hh), w=["H2T"], dma=True)
            P.add("pool", lambda e: e.dma_start(out=WU[:], in_=w_up[0].rearrange("(k p) f -> p k f", p=128)), w=["WU"], dma=True)
            P.add("pool", lambda e: e.dma_start(out=WD[:], in_=w_down[0].rearrange("(k p) f -> p k f", p=128)), w=["WD"], dma=True)
            P.add("pool", lambda e: e.memset(Y[:], 0.0), w=["Y%d" % i for i in range(16)])
            for half in range(2):
                P.add("pe", (lambda half: lambda e: e.matmul(PB[1][:, :], lhsT=ONF[0:1, :], rhs=G2R[0:1, half * 512:(half + 1) * 512], start=True, stop=True))(half),
                      r=[], w=bank(1))
                P.add("dve", (lambda half: lambda e: e.tensor_copy(out=G2B[:, half * 512:(half + 1) * 512], in_=PB[1][:, :]))(half), r=bank(1), w=["G2B"])
            for tt in range(16):
                L_ = LG[:, tt, :]
                P.add("dve", (lambda L_: lambda e: e.max(out=T8[:], in_=L_))(L_), r=[], w=["T8"])
                P.add("dve", (lambda L_: lambda e: e.tensor_scalar(out=MSK[:], in0=L_, scalar1=T8[:, 3:4], scalar2=None, op0=ALU.is_ge))(L_), r=["T8"], w=["MSK"])
                P.add("dve", lambda e: e.tensor_scalar(out=CL[:, 0:1], in0=T8[:, 0:1], scalar1=-1.0, scalar2=None, op0=ALU.mult), r=["T8"], w=["CL0"])
                P.add("act", (lambda L_: lambda e: e.activation(out=EX[:], in_=L_, func=AF.Exp, bias=CL[:, 0:1]))(L_), r=["CL0"], w=["EX"])
                P.add("dve", lambda e: e.tensor_tensor(out=EX[:], in0=EX[:], in1=MSK[:], op=ALU.mult), r=["EX", "MSK"], w=["EX"])
                P.add("dve", lambda e: e.reduce_sum(out=CL[:, 1:2], in_=EX[:], axis=AX.X), r=["EX"], w=["CL1"])
                P.add("dve", lambda e: e.reciprocal(out=CL[:, 2:3], in_=CL[:, 1:2]), r=["CL1"], w=["CL2"])
                P.add("dve", (lambda tt: lambda e: e.tensor_scalar(out=G[:, tt, :], in0=EX[:], scalar1=CL[:, 2:3], scalar2=None, op0=ALU.mult))(tt), r=["EX", "CL2"], w=["G"])
            x1s_t = x1s.rearrange("(n p) d -> n p d", p=128)
            out_t = out.rearrange("(n p) d -> n p d", p=128)
            for ex in (range(n_exp) if do_c else []):
                for tg in range(4):
                    slot = tg % 2
                    tcs = slice(tg * 512, (tg + 1) * 512)
                    for fc in range(8):
                        pb = (fc % 2) * 2; ss = fc % 2
                        for z, fcc in ((0, fc), (1, fc + 8)):
                            P.add("pe", (lambda fcc, pbz, tcs: _grp([(lambda k: lambda e: e.matmul(PB[pbz][:, :], lhsT=WU[:, k, fcc * 128:(fcc + 1) * 128], rhs=H2T[:, k, tcs],
                                                                                               start=(k == 0), stop=(k == 7)))(k) for k in range(8)]))(fcc, pb + z, tcs),
                                  r=["WU", "H2T"], w=bank(pb + z))
                        P.add("dve", (lambda ex, fc, pb, ss: lambda e: e.tensor_scalar(out=TG[ss][:], in0=PB[pb][:, :], scalar1=BU[:, ex, fc:fc + 1], scalar2=7.0, op0=ALU.add, op1=ALU.min))(ex, fc, pb, ss),
                              r=bank(pb) + ["BU"], w=["TG%d" % ss])
                        P.add("act", (lambda ss: lambda e: e.activation(out=TSg[ss][:], in_=TG[ss][:], func=AF.Sigmoid, scale=1.702))(ss), r=["TG%d" % ss], w=["TSg%d" % ss])
                        P.add("act", (lambda ex, fc, pb, ss: lambda e: e.activation(out=TL[ss][:], in_=PB[pb + 1][:, :], func=AF.Identity, bias=BU[:, ex, 8 + fc:9 + fc]))(ex, fc, pb, ss),
                              r=bank(pb + 1) + ["BU"], w=["TL%d" % ss])
                        P.add("pool", (lambda ss: lambda e: e.tensor_scalar(out=TL[ss][:], in0=TL[ss][:], scalar1=8.0, scalar2=-6.0, op0=ALU.min, op1=ALU.max))(ss), r=["TL%d" % ss], w=["TL%d" % ss])
                        P.add("dve", (lambda ss: lambda e: e.tensor_tensor(out=TG[ss][:], in0=TG[ss][:], in1=TSg[ss][:], op=ALU.mult))(ss), r=["TG%d" % ss, "TSg%d" % ss], w=["TG%d" % ss])
                        P.add("pool", (lambda fc, ss, slot: lambda e: e.tensor_tensor(out=ACTT[:, fc, slot * 512:(slot + 1) * 512], in0=TG[ss][:], in1=TL[ss][:], op=ALU.mult))(fc, ss, slot),
                              r=["TG%d" % ss, "TL%d" % ss], w=["ACTT%d_%d" % (fc, slot)])
                    if tg == 3 and ex + 1 < n_exp:
                        P.add("pool", (lambda ex: lambda e: e.dma_start(out=WU[:], in_=w_up[ex + 1].rearrange("(k p) f -> p k f", p=128)))(ex), w=["WU"], dma=True)
                    for t4 in range(4):
                        tt = tg * 4 + t4
                        acs = slice(slot * 512 + t4 * 128, slot * 512 + (t4 + 1) * 128)
                        for half in range(2):
                            pb = 4 + (tt % 2) * 2 + half
                            fns = [(lambda fc, pb, acs, half: lambda e: e.matmul(PB[pb][:, :], lhsT=ACTT[:, fc, acs], rhs=WD[:, fc, half * 512:(half + 1) * 512],
                                                                                 start=(fc == 0), stop=(fc == 7)))(fc, pb, acs, half) for fc in range(8)]
                            P.add("pe", _grp(fns), r=["WD"] + ["ACTT%d_%d" % (fc, slot) for fc in range(8)], w=bank(pb))
                            P.add("dve", (lambda tt, half, pb, ex: lambda e: e.scalar_tensor_tensor(out=Y[:, tt, half * 512:(half + 1) * 512], in0=PB[pb][:, :],
                                                                                                  scalar=G[:, tt, ex:ex + 1], in1=Y[:, tt, half * 512:(half + 1) * 512],
                                                                                                  op0=ALU.mult, op1=ALU.add))(tt, half, pb, ex),
                                  r=bank(pb) + ["G", "Y%d" % tt], w=["Y%d" % tt])
                if ex + 1 < n_exp:
                    P.add("pool", (lambda ex: lambda e: e.dma_start(out=WD[:], in_=w_down[ex + 1].rearrange("(k p) f -> p k f", p=128)))(ex), w=["WD"], dma=True)
            for tt in (range(16) if do_c else []):
                P.add("pe", (lambda tt: lambda e: e.transpose(PB[2][0:NE, 0:128], G[:, tt, :], IDF[:]))(tt), r=["G", "IDF"], w=bank(2))
                P.add("dve", lambda e: e.tensor_copy(out=GT[:], in_=PB[2][0:NE, 0:128]), r=bank(2), w=["GT"])
                for half in range(2):
                    P.add("pe", (lambda half: lambda e: e.matmul(PB[half][:, :], lhsT=GT[0:n_exp, :], rhs=BDN[0:n_exp, half * 512:(half + 1) * 512], start=True, stop=True))(half),
                          r=["GT", "BDN"], w=bank(half))
                    P.add("dve", (lambda tt, half: lambda e: e.tensor_tensor(out=Y[:, tt, half * 512:(half + 1) * 512], in0=Y[:, tt, half * 512:(half + 1) * 512],
                                                                            in1=PB[half][:, :], op=ALU.add))(tt, half), r=bank(half) + ["Y%d" % tt], w=["Y%d" % tt])
                P.add("sp", (lambda tt: lambda e: e.dma_start(out=X1T[:], in_=x1s_t[tt]))(tt), w=["X1T"], dma=True)
                P.add("act", (lambda tt: lambda e: e.activation(out=JNK[:], in_=Y[:, tt, :], func=AF.Square, accum_out=CL[:, 4:5]))(tt), r=["Y%d" % tt], w=["JNK", "CL4"])
                P.add("act", lambda e: e.activation(out=CL[:, 5:6], in_=CL[:, 4:5], func=AF.Ln, bias=EPSC[:], scale=1.0 / D), r=["CL4"], w=["CL5"])
                P.add("act", lambda e: e.activation(out=CL[:, 5:6], in_=CL[:, 5:6], func=AF.Exp, scale=-0.5), r=["CL5"], w=["CL5"])
                P.add("dve", (lambda tt: lambda e: e.scalar_tensor_tensor(out=Y[:, tt, :], in0=Y[:, tt, :], scalar=CL[:, 5:6], in1=G2B[:], op0=ALU.mult, op1=ALU.mult))(tt),
                      r=["Y%d" % tt, "CL5", "G2B"], w=["Y%d" % tt])
                P.add("dve", (lambda tt: lambda e: e.tensor_tensor(out=X1T[:], in0=X1T[:], in1=Y[:, tt, :], op=ALU.add))(tt), r=["Y%d" % tt, "X1T"], w=["X1T"])
                P.add("sp", (lambda tt: lambda e: e.dma_start(out=out_t[tt], in_=X1T[:]))(tt), r=["X1T"], w=["out"], dma=True)
            P.emit(st, final_wait_keys=["out"] if do_c else [])
            print("phase C ops:", len(P.ops))
    return nc


def _host_inputs(inputs):
    x = np.ascontiguousarray(inputs["x"], dtype=np.float32)
    c = inputs["c"]
    rel = inputs["rel_bias"][0]
    kk = np.arange(128)[:, None, None]; t = np.arange(5)[None, :, None]; qq = np.arange(128)[None, None, :]
    diff = 128 * (4 - t) + qq - kk
    idx = np.clip(diff, -128, 128) + 128
    cd = 8 - 2 * t + qq // 64 - kk // 64
    valid = (cd >= 0) & (cd <= 8)
    bm5 = np.empty((128, 8, 640), np.float32)
    for hb in range(8):
        bm5[:, hb, :] = np.where(valid, rel[hb][idx], np.float32(NEG)).reshape(128, 640)
    ii = np.arange(128)[:, None]; jj = np.arange(128)[None, :]
    imask = np.zeros((128, 5, 128), np.float32)
    imask[:, 0, :] = (ii // 8 == jj // 8)
    for lv, sz in enumerate((8, 16, 32, 64), start=1):
        mk = (ii // (2 * sz) == jj // (2 * sz)) & ((ii % (2 * sz)) >= sz) & ((jj % (2 * sz)) < sz)
        imask[:, lv, :] = mk.T
    cw = np.ascontiguousarray(inputs["conv_w"][0].reshape(4, 12, 128).transpose(2, 1, 0))
    bu = np.ascontiguousarray(inputs["b_up"][0].reshape(NE, 16, 128).transpose(2, 0, 1))
    shared = {
        "w_ada": inputs["w_ada"][0], "b_ada": inputs["b_ada"][0].reshape(1, -1), "norm_w": inputs["norm_w"][0].reshape(1, -1),
        "w_in": inputs["w_in"][0], "cw": cw, "alog": np.ascontiguousarray(np.broadcast_to(inputs["a_log"][0], (128, 4))),
        "dtb": np.ascontiguousarray(np.broadcast_to(inputs["dt_bias"][0], (128, 4))), "anw": inputs["a_norm_w"][0].reshape(128, 1),
        "imask": imask, "bm5": bm5, "w_out": inputs["w_out"][0], "w_router": inputs["w_router"][0],
        "br": np.ascontiguousarray(np.broadcast_to(inputs["b_router"][0], (128, NE))), "w_up": inputs["w_up"][0], "bu": bu,
        "w_down": inputs["w_down"][0], "b_down": inputs["b_down"][0],
    }
    shared = {k: np.ascontiguousarray(v, dtype=np.float32) for k, v in shared.items()}
    maps = []
    for core in range(8):
        b, j = core // 4, core % 4
        xw = np.zeros((WIN, D), np.float32)
        n_real = NTOK * (j + 1)
        xw[WIN - n_real:] = x[b, :n_real]
        qv = np.zeros((128, 3), np.float32)
        for q in range(3):
            qv[:, q] = 1.0 if (j - 3 + q) >= 0 else 0.0
        km = np.full((128, 1), 0.0 if j >= 1 else NEG, np.float32)
        m = dict(shared)
        m.update({"xw": xw, "qvalid": qv, "kmask": km, "ccol": np.ascontiguousarray(c[b].reshape(8, 128).T, dtype=np.float32)})
        maps.append(m)
    return maps


_NC_CACHE = {}


def kernel(**inputs):
    maps = _host_inputs(inputs)
    if "nc" not in _NC_CACHE:
        _NC_CACHE["nc"] = build()
    nc = _NC_CACHE["nc"]
    res = run_bass_kernel_spmd(nc, maps, core_ids=list(range(8)))
    full = np.empty((2, 8192, D), np.float32)
    for core in range(8):
        b, j = core // 4, core % 4
        full[b, j * NTOK:(j + 1) * NTOK] = res.results[core]["out"]
    return full
```

```python
import numpy as np
from contextlib import ExitStack
import concourse.bass as bass
import concourse.mybir as mybir
from concourse.bass_utils import run_bass_kernel_spmd

F32 = mybir.dt.float32
BF16 = mybir.dt.bfloat16
AF = mybir.ActivationFunctionType
ALU = mybir.AluOpType
AX = mybir.AxisListType

ENGS = ("pe", "act", "dve", "pool", "sp")
DS = 1
NEG = -30000.0
EPS = 1e-6


class Prog:
    def __init__(self, nc, n_dma_sems=6):
        self.nc = nc
        self.ops = []
        self.last_w = {}
        self.readers = {}
        self.n_dma_sems = n_dma_sems

    def add(self, eng, fn, r=(), w=(), dma=False, after_all=False):
        banks = set()
        for k in list(r) + list(w):
            if k[0] == "q" and "_" in k and k[1:k.index("_")].isdigit():
                banks.add("BK" + k[1:k.index("_")])
        deps = set()
        for k in r:
            if k in self.last_w:
                deps.add(self.last_w[k])
        for k in w:
            if k in self.last_w:
                deps.add(self.last_w[k])
            deps.update(self.readers.get(k, ()))
        tdeps = set()
        for k in banks:
            if k in self.last_w and self.last_w[k] not in deps:
                tdeps.add(self.last_w[k])
        deps |= tdeps
        w = list(w) + sorted(banks)
        idx = len(self.ops)
        if after_all:
            deps = set(range(idx))
        self.ops.append(dict(eng=eng, fn=fn, deps=deps, dma=dma, sig=False, tdeps=tdeps))
        for k in r:
            self.readers.setdefault(k, []).append(idx)
        for k in w:
            self.last_w[k] = idx
            self.readers[k] = []
        return idx

    def emit(self, stack, final_wait_keys=()):
        nc = self.nc
        ops = self.ops
        final_deps = set()
        for k in final_wait_keys:
            if k in self.last_w:
                final_deps.add(self.last_w[k])
        for o in ops:
            for d in o["deps"]:
                ops[d]["sig"] = True
        for d in final_deps:
            ops[d]["sig"] = True
        esem = {e: stack.enter_context(nc.semaphore("s_" + e)) for e in ENGS}
        dsem = {e: [stack.enter_context(nc.semaphore("d_%s%d" % (e, i))) for i in range(self.n_dma_sems)]
                for e in ENGS if e != "pe"}
        ecount = {e: 0 for e in ENGS}
        dcount = {e: [0] * self.n_dma_sems for e in dsem}
        drr = {e: 0 for e in dsem}
        for o in ops:
            e = o["eng"]
            if o["dma"]:
                j = drr[e]
                drr[e] = (j + 1) % self.n_dma_sems
                o["prev_on_sem"] = (dsem[e][j], dcount[e][j]) if dcount[e][j] else None
                dcount[e][j] += 16
                o["sem"] = dsem[e][j]
                o["val"] = dcount[e][j]
            elif o["sig"]:
                ecount[e] += 1
                o["sem"] = esem[e]
                o["val"] = ecount[e]
        per_eng = {e: [] for e in ENGS}
        for i, o in enumerate(ops):
            per_eng[o["eng"]].append(i)
        block = stack.enter_context(nc.Block())

        def run(e, eng):
            waited = {}

            def wait(sem, val):
                key = id(sem)
                if waited.get(key, 0) >= val:
                    return
                waited[key] = val
                eng.wait_ge(sem, val)

            for i in per_eng[e]:
                o = ops[i]
                for d in sorted(o["deps"]):
                    p = ops[d]
                    if p["eng"] == "pe" and e == "pe" and not p["dma"] and not o["dma"]:
                        continue
                    if d in o["tdeps"] and p["eng"] == e and not p["dma"] and not o["dma"]:
                        continue
                    wait(p["sem"], p["val"])
                if o["dma"] and o["prev_on_sem"] is not None:
                    wait(*o["prev_on_sem"])
                ins = o["fn"](eng)
                if o["dma"]:
                    ins.then_inc(o["sem"], 16)
                elif o["sig"]:
                    ins.then_inc(o["sem"], 1)
            if e == "sp":
                for d in sorted(final_deps):
                    wait(ops[d]["sem"], ops[d]["val"])

        @block.tensor
        def _(eng):
            run("pe", eng)

        @block.scalar
        def _(eng):
            run("act", eng)

        @block.vector
        def _(eng):
            run("dve", eng)

        @block.gpsimd
        def _(eng):
            run("pool", eng)

        @block.sync
        def _(eng):
            run("sp", eng)


D = 1024
NTOK = 2048
WIN = 8192
ST = 256
NST = WIN // ST
OWN0 = (WIN - NTOK) // ST
HALO0 = OWN0 - 2
C_Q, C_K, C_V, C_Z, C_AB, C_QB, C_KB, C_VB = 0, 512, 1024, 1536, 2048, 2056, 2568, 3080
NE = 32


def _grp(fns):
    def f(e):
        ins = None
        for g in fns:
            ins = g(e)
        return ins
    return f


def build(n_exp=NE, dbg=False, a_list=None, do_b=True, do_c=True, stop=99):
    a_list = list(range(NST)) if a_list is None else a_list
    nc = bass.Bass("TRN2", target_bir_lowering=False)
    DI = lambda name, shape, dt=F32: nc.dram_tensor(name, shape, dt, kind="ExternalInput").ap()
    xw = DI("xw", [WIN, D]); qvalid_d = DI("qvalid", [128, 3]); kmask_d = DI("kmask", [128, 1])
    ccol_d = DI("ccol", [128, 8]); w_ada = DI("w_ada", [D, 6 * D]); b_ada = DI("b_ada", [1, 6 * D])
    norm_w = DI("norm_w", [1, 4 * D]); w_in = DI("w_in", [D, 3592]); cw_d = DI("cw", [128, 12, 4])
    imk_d = DI("imask", [128, 5, 128]); alog_d = DI("alog", [128, 4]); dtb_d = DI("dtb", [128, 4]); anw_d = DI("anw", [128, 1])
    bm5_d = DI("bm5", [128, 8, 640]); w_out = DI("w_out", [D, D]); w_router = DI("w_router", [D, NE])
    br_d = DI("br", [128, NE]); w_up = DI("w_up", [n_exp, D, 2 * D]); bu_d = DI("bu", [128, NE, 16])
    w_down = DI("w_down", [n_exp, D, D]); b_down = DI("b_down", [n_exp, D])
    out = nc.dram_tensor("out", [NTOK, D], F32, kind="ExternalOutput").ap()
    x1s = nc.dram_tensor("x1s", [NTOK, D], F32).ap()
    h2s = nc.dram_tensor("h2s", [128, 8, NTOK], BF16).ap()
    dbg_o = {}
    if dbg:
        dbg_o["ota"] = nc.dram_tensor("dbg_ota", [128, 4, NTOK], F32, kind="ExternalOutput").ap()
        dbg_o["otb"] = nc.dram_tensor("dbg_otb", [128, 4, NTOK], F32, kind="ExternalOutput").ap()
        dbg_o["x1"] = nc.dram_tensor("dbg_x1", [NTOK, D], F32, kind="ExternalOutput").ap()
        dbg_o["lg"] = nc.dram_tensor("dbg_lg", [128, 16, NE], F32, kind="ExternalOutput").ap()

    with ExitStack() as st0:
        def mk(st):
            return lambda name, shape, dt=F32: st.enter_context(nc.sbuf_tensor(name, shape, dt))
        S0 = mk(st0)
        IDF = S0("IDF", [128, 128]); IDB = S0("IDB", [128, 128], BF16)
        ONF = S0("ONF", [128, 128]); ONB = S0("ONB", [128, 128], BF16)
        LG = S0("LG", [128, 16, NE])
        G2R = S0("G2R", [1, D])
        A1C = S0("A1C", [128, 8]); SH1C = S0("SH1C", [128, 8]); A2C = S0("A2C", [128, 8]); SH2C = S0("SH2C", [128, 8])
        EPSC = S0("EPSC", [128, 1])
        PB = [st0.enter_context(nc.psum_tensor("pb%d" % i, [128, 512], F32)) for i in range(8)]
        stAB = ExitStack()
        SAB = mk(stAB)
        OTA = SAB("OTA", [128, 4, NTOK], BF16)
        G1R = SAB("G1R", [1, D])

        def bank(b):
            return ["q%d_%d" % (b, i) for i in range(4)]

        def qs(b, q0, n=1):
            return ["q%d_%d" % (b, i) for i in range(q0, q0 + n)]

        with ExitStack() as st:
            S = mk(st)
            P = Prog(nc)
            UF = S("UF", [128, 128]); NM2 = S("NM2", [128, 128]); NMT = S("NMT", [128, 128]); IMK = S("IMK", [128, 5, 128])
            WA = S("WA", [128, 8, 2056], BF16)
            QV = S("QV", [128, 3]); CW = S("CW", [128, 12, 4]); NEGA = S("NEGA", [128, 4]); DTB = S("DTB", [128, 4])
            ANW = S("ANW", [128, 1]); CC = S("CC", [128, 8]); SC = S("SC", [128, 8])
            WST = S("WST", [128, 8, 512])
            BST = S("BST", [1, 512]); NWT = S("NWT", [1, 512]); ROWT = S("ROWT", [1, 512])
            P.add("pool", lambda e: e.memset(IDF[:], 0.0), w=["IDF"])
            P.add("pool", lambda e: e.affine_select(out=IDF[:], in_=IDF[:], pattern=[[-1, 128]], compare_op=ALU.not_equal,
                                                     fill=1.0, base=0, channel_multiplier=1), r=["IDF"], w=["IDF"])
            P.add("pool", lambda e: e.tensor_copy(out=IDB[:], in_=IDF[:]), r=["IDF"], w=["IDB"])
            P.add("pool", lambda e: e.memset(ONF[:], 1.0), w=["ONF"])
            P.add("pool", lambda e: e.memset(ONB[:], 1.0), w=["ONB"])
            P.add("pool", lambda e: e.memset(EPSC[:], EPS), w=["EPSC"])
            P.add("pool", lambda e: e.memset(UF[:], 1.0), w=["UF"])
            P.add("pool", lambda e: e.affine_select(out=UF[:], in_=UF[:], pattern=[[1, 128]], compare_op=ALU.is_ge,
                                                     fill=0.0, base=0, channel_multiplier=-1), r=["UF"], w=["UF"])
            P.add("pool", lambda e: e.memset(NM2[:], -NEG), w=["NM2"])
            P.add("pool", lambda e: e.affine_select(out=NM2[:], in_=NM2[:], pattern=[[1, 128]], compare_op=ALU.is_ge,
                                                     fill=0.0, base=0, channel_multiplier=-1), r=["NM2"], w=["NM2"])
            P.add("pool", lambda e: e.memset(NMT[:], NEG), w=["NMT"])
            P.add("pool", lambda e: e.affine_select(out=NMT[:], in_=NMT[:], pattern=[[-1, 128]], compare_op=ALU.is_gt,
                                                     fill=0.0, base=0, channel_multiplier=1), r=["NMT"], w=["NMT"])
            for dst, src, key in ((IMK, imk_d, "IMK"), (QV, qvalid_d, "QV"), (CW, cw_d, "CW"), (NEGA, alog_d, "NEGA"), (DTB, dtb_d, "DTB"),
                                  (ANW, anw_d, "ANW"), (CC, ccol_d, "CC")):
                P.add("sp", (lambda dst, src: lambda e: e.dma_start(out=dst[:], in_=src))(dst, src), w=[key], dma=True)
            w_in_r = w_in.rearrange("(k p) c -> p k c", p=128)
            P.add("pool", lambda e: e.dma_start(out=WA[:, :, 0:2048], in_=w_in_r[:, :, 0:2048]), w=["WA0"], dma=True)
            P.add("pool", lambda e: e.dma_start(out=WA[:, :, 2048:2056], in_=w_in_r[:, :, 2048:2056]), w=["WA1"], dma=True)
            P.add("act", lambda e: e.activation(out=NEGA[:], in_=NEGA[:], func=AF.Exp), r=["NEGA"], w=["NEGA"])
            P.add("dve", lambda e: e.tensor_scalar(out=NEGA[:], in0=NEGA[:], scalar1=-1.0, scalar2=None, op0=ALU.mult), r=["NEGA"], w=["NEGA"])
            P.add("act", lambda e: e.activation(out=SC[:], in_=CC[:], func=AF.Silu), r=["CC"], w=["SC"])
            w_ada_r = w_ada.rearrange("(k p) n -> p k n", p=128)
            for nb in range(12):
                kind, half = nb // 2, nb % 2
                P.add("sp", (lambda nb: lambda e: e.dma_start(out=WST[:], in_=w_ada_r[:, :, nb * 512:(nb + 1) * 512]))(nb), w=["WST"], dma=True)
                P.add("sp", (lambda nb: lambda e: e.dma_start(out=BST[:], in_=b_ada[0:1, nb * 512:(nb + 1) * 512]))(nb), w=["BST"], dma=True)
                if kind in (1, 2, 4, 5):
                    nwi = {1: 0, 2: 1, 4: 2, 5: 3}[kind]
                    P.add("sp", (lambda o_: lambda e: e.dma_start(out=NWT[:], in_=norm_w[0:1, o_:o_ + 512]))(nwi * D + half * 512), w=["NWT"], dma=True)
                P.add("pe", _grp([(lambda k: lambda e: e.matmul(PB[0][0:1, :], lhsT=SC[:, k:k + 1], rhs=WST[:, k, :], start=(k == 0), stop=(k == 7)))(k) for k in range(8)]),
                      r=["SC", "WST"], w=bank(0))
                P.add("dve", lambda e: e.tensor_tensor(out=ROWT[:], in0=PB[0][0:1, :], in1=BST[:], op=ALU.add), r=bank(0) + ["BST"], w=["ROWT"])
                if kind in (1, 4):
                    P.add("dve", lambda e: e.scalar_tensor_tensor(out=ROWT[:], in0=ROWT[:], scalar=1.0, in1=NWT[:], op0=ALU.add, op1=ALU.mult), r=["ROWT", "NWT"], w=["ROWT"])
                elif kind in (2, 5):
                    P.add("dve", lambda e: e.tensor_tensor(out=ROWT[:], in0=ROWT[:], in1=NWT[:], op=ALU.mult), r=["ROWT", "NWT"], w=["ROWT"])
                if kind in (0, 1, 3, 4):
                    dst, key = {0: (SH1C, "SH1C"), 1: (A1C, "A1C"), 3: (SH2C, "SH2C"), 4: (A2C, "A2C")}[kind]
                    P.add("pe", _grp([(lambda k: lambda e: e.matmul(PB[1][:, k:k + 1], lhsT=ROWT[0:1, k * 128:(k + 1) * 128], rhs=ONF[0:1, 0:1], start=True, stop=True))(k)
                                      for k in range(4)]), r=["ROWT", "ONF"], w=bank(1))
                    P.add("dve", (lambda dst, half: lambda e: e.tensor_copy(out=dst[:, half * 4:half * 4 + 4], in_=PB[1][:, 0:4]))(dst, half), r=bank(1), w=[key])
                else:
                    dst, key = (G1R, "G1R") if kind == 2 else (G2R, "G2R")
                    P.add("dve", (lambda dst, half: lambda e: e.tensor_copy(out=dst[0:1, half * 512:(half + 1) * 512], in_=ROWT[:]))(dst, half), r=["ROWT"], w=[key])

            XT = [S("XT%d" % i, [128, D]) for i in range(2)]
            JNK = S("JNK", [128, D], BF16)
            XS = [S("XS%d" % i, [128, D], BF16) for i in range(2)]
            HT = S("HT", [128, 8, ST], BF16)
            RAW = S("RAW", [128, 12, ST + 3], BF16)
            CV = S("CV", [128, 12, ST])
            SIL = S("SIL", [128, 12, ST], BF16)
            SQ = S("SQ", [128, 8, ST], BF16)
            LNT = S("LNT", [128, 8, ST])
            QKVL = [S("QKV%d" % i, [128, 12, ST], BF16) for i in range(2)]
            ZSL = [S("ZS%d" % i, [128, 4, ST], BF16) for i in range(2)]
            ABTL = [S("ABT%d" % i, [128, 2, 8]) for i in range(2)]
            GSTL = [S("GST%d" % i, [128, 2, 4]) for i in range(2)]; BETL = [S("BET%d" % i, [128, 2, 4]) for i in range(2)]
            NBETL = [S("NBET%d" % i, [128, 2, 4]) for i in range(2)]
            SSC = S("SSC", [128, 4]); RSC = S("RSC", [128, 4])
            Sm = [S("Sm%d" % h, [128, 128]) for h in range(4)]
            Sb = [S("Sb%d" % h, [128, 128], BF16) for h in range(4)]
            NSET = 4
            def tset(i):
                t = {}
                for nm in ("gsb", "decS", "decT", "egb", "o1"):
                    t[nm] = S("%s_%d" % (nm, i), [128, 128])
                for nm in ("A", "AT", "P0", "PT0", "P1", "PT1", "RT", "TM", "BKm", "Ym", "kbg", "kdec", "vb", "nwT", "vnew", "attnT", "qdT", "sq"):
                    t[nm] = S("%s_%d" % (nm, i), [128, 128], BF16)
                t["gc"] = S("gc_%d" % i, [128, 8])
                t["BKall"] = S("BKall_%d" % i, [128, 4, 128], BF16)
                return t
            TS = [tset(i) for i in range(NSET)]
            P.add("pool", lambda e: e.memset(RAW[:], 0.0), w=["RAW%d" % c for c in range(12)])
            for h in range(4):
                P.add("pool", (lambda h: lambda e: e.memset(Sm[h][:], 0.0))(h), w=["Sm%d" % h])
                P.add("pool", (lambda h: lambda e: e.memset(Sb[h][:], 0.0))(h), w=["Sb%d" % h])

            xw_t = xw.rearrange("(n p) d -> n p d", p=128)
            PBb0 = PB[0][:].bitcast(BF16)
            PBbH = [PB[4 + h][:].bitcast(BF16) for h in range(4)]

            def do_st(s):
                pending = []
                FA = lambda *a_, **k_: pending.append((a_, k_))
                own = s >= OWN0
                q = s // 8
                par = s % 2; kp = "p%d" % par
                QKVc, ZSc, ABTc, GSTc, BETc, NBETc = QKVL[par], ZSL[par], ABTL[par], GSTL[par], BETL[par], NBETL[par]
                for u in range(2):
                    ti = 2 * s + u
                    xt = XT[u]; xs_ = XS[u]
                    FA("sp", (lambda xt, ti: lambda e: e.dma_start(out=xt[:], in_=xw_t[ti]))(xt, ti), w=["XT%d" % u], dma=True)
                    if stop <= 0.25:
                        continue
                    FA("act", (lambda xt, u: lambda e: e.activation(out=JNK[:], in_=xt[:], func=AF.Square, accum_out=SSC[:, u:u + 1]))(xt, u),
                          r=["XT%d" % u], w=["JNK", "SSC%d" % u])
                    FA("act", (lambda u: lambda e: e.activation(out=RSC[:, u:u + 1], in_=SSC[:, u:u + 1], func=AF.Ln, bias=EPSC[:], scale=1.0 / D))(u),
                          r=["SSC%d" % u, "EPSC"], w=["RSC%d" % u])
                    FA("act", (lambda u: lambda e: e.activation(out=RSC[:, u:u + 1], in_=RSC[:, u:u + 1], func=AF.Exp, scale=-0.5))(u),
                          r=["RSC%d" % u], w=["RSC%d" % u])
                    FA("dve", (lambda xt, xs_, u: lambda e: e.tensor_scalar(out=xs_[:], in0=xt[:], scalar1=RSC[:, u:u + 1], scalar2=None, op0=ALU.mult))(xt, xs_, u),
                          r=["XT%d" % u, "RSC%d" % u], w=["XS%d" % u])
                    if stop <= 0.5:
                        continue
                    FA("pe", (lambda xs_: _grp([(lambda k: lambda e: e.transpose(PBb0[:, k * 128:(k + 1) * 128], xs_[:, k * 128:(k + 1) * 128], IDB[:]))(k)
                                                   for k in range(8)]))(xs_), r=["XS%d" % u, "IDB"], w=bank(0))
                    for k in range(8):
                        eng = "act"
                        if eng == "act":
                            f = (lambda k, u: lambda e: e.activation(out=HT[:, k, u * 128:(u + 1) * 128], in_=PBb0[:, k * 128:(k + 1) * 128],
                                                                     func=AF.Identity, bias=SH1C[:, k:k + 1], scale=A1C[:, k:k + 1]))(k, u)
                        else:
                            f = (lambda k, u: lambda e: e.tensor_scalar(out=HT[:, k, u * 128:(u + 1) * 128], in0=PBb0[:, k * 128:(k + 1) * 128],
                                                                        scalar1=A1C[:, k:k + 1], scalar2=SH1C[:, k:k + 1], op0=ALU.mult, op1=ALU.add))(k, u)
                        FA(eng, f, r=bank(0) + ["A1C", "SH1C"], w=["HT%d_%d" % (k, u)])
                HTK = ["HT%d_%d" % (k, u) for k in range(8) for u in range(2)]
                if stop <= 1:
                    return pending, None
                chunks = list(range(4, 12)) + (list(range(0, 4)) + list(range(12, 16)) if own else ([0, 1, 2, 3] if s == OWN0 - 1 else []))
                for ci, c in enumerate(chunks):
                    slot_b, slot_q = 1 + (ci % 2), 0
                    pso = PB[slot_b][:, 0:ST]
                    FA("pe", (lambda c, pso: _grp([(lambda k: lambda e: e.matmul(pso, lhsT=WA[:, k, c * 128:(c + 1) * 128], rhs=HT[:, k, :],
                                                                                start=(k == 0), stop=(k == 7)))(k) for k in range(8)]))(c, pso),
                          r=HTK + ["WA0"], w=qs(slot_b, 0, 2))
                    if c < 12:
                        dst = RAW[:, c, 3:3 + ST]
                        if own:
                            FA("act", (lambda dst, pso: lambda e: e.copy(out=dst, in_=pso))(dst, pso), r=qs(slot_b, 0, 2), w=["RAW%d" % c])
                        else:
                            FA("dve", (lambda dst, pso, q: lambda e: e.tensor_scalar(out=dst, in0=pso, scalar1=QV[:, q:q + 1], scalar2=None, op0=ALU.mult))(dst, pso, q),
                                  r=qs(slot_b, 0, 2) + ["QV"], w=["RAW%d" % c])
                    else:
                        FA("act", (lambda c, pso: lambda e: e.activation(out=ZSc[:, c - 12, :], in_=pso, func=AF.Silu))(c, pso),
                              r=qs(slot_b, 0, 2), w=["ZS%d" % (c - 12) + kp])
                if stop <= 2:
                    return pending, None
                for u in range(2):
                    FA("pe", (lambda u: _grp([(lambda k: lambda e: e.matmul(PB[3][:, 0:8], lhsT=HT[:, k, u * 128:(u + 1) * 128], rhs=WA[:, k, 2048:2056],
                                                                            start=(k == 0), stop=(k == 7)))(k) for k in range(8)]))(u),
                          r=HTK + ["WA1"], w=qs(3, 0))
                    if own:
                        FA("dve", (lambda u: lambda e: e.tensor_copy(out=ABTc[:, u, :], in_=PB[3][:, 0:8]))(u), r=qs(3, 0), w=["ABT%d" % u + kp])
                    else:
                        FA("dve", (lambda u, q: lambda e: e.tensor_scalar(out=ABTc[:, u, :], in0=PB[3][:, 0:8], scalar1=QV[:, q:q + 1], scalar2=None, op0=ALU.mult))(u, q),
                              r=qs(3, 0) + ["QV"], w=["ABT%d" % u + kp])
                    FA("dve", (lambda u: lambda e: e.tensor_tensor(out=GSTc[:, u, :], in0=ABTc[:, u, 0:4], in1=DTB[:], op=ALU.add))(u), r=["ABT%d" % u + kp, "DTB"], w=["GST%d" % u + kp])
                    FA("act", (lambda u: lambda e: e.activation(out=GSTc[:, u, :], in_=GSTc[:, u, :], func=AF.Exp))(u), r=["GST%d" % u + kp], w=["GST%d" % u + kp])
                    FA("act", (lambda u: lambda e: e.activation(out=GSTc[:, u, :], in_=GSTc[:, u, :], func=AF.Ln, bias=ONF[:, 0:1]))(u), r=["GST%d" % u + kp, "ONF"], w=["GST%d" % u + kp])
                    FA("dve", (lambda u: lambda e: e.tensor_tensor(out=GSTc[:, u, :], in0=GSTc[:, u, :], in1=NEGA[:], op=ALU.mult))(u), r=["GST%d" % u + kp, "NEGA"], w=["GST%d" % u + kp])
                    FA("act", (lambda u: lambda e: e.activation(out=BETc[:, u, :], in_=ABTc[:, u, 4:8], func=AF.Sigmoid))(u), r=["ABT%d" % u + kp], w=["BET%d" % u + kp])
                    FA("dve", (lambda u: lambda e: e.tensor_scalar(out=NBETc[:, u, :], in0=BETc[:, u, :], scalar1=-1.0, scalar2=None, op0=ALU.mult))(u), r=["BET%d" % u + kp], w=["NBET%d" % u + kp])
                if stop <= 3:
                    return pending, None
                for c in chunks:
                    if c >= 12:
                        continue
                    rk = "RAW%d" % c
                    FA("pool", (lambda c: lambda e: e.tensor_scalar(out=CV[:, c, :], in0=RAW[:, c, 0:ST], scalar1=CW[:, c, 0:1], scalar2=None, op0=ALU.mult))(c),
                          r=[rk, "CW"], w=["CV%d" % c])
                    for tp in range(1, 4):
                        FA("dve", (lambda c, tp: lambda e: e.scalar_tensor_tensor(out=CV[:, c, :], in0=RAW[:, c, tp:tp + ST], scalar=CW[:, c, tp:tp + 1],
                                                                                      in1=CV[:, c, :], op0=ALU.mult, op1=ALU.add))(c, tp),
                              r=[rk, "CW", "CV%d" % c], w=["CV%d" % c])
                    FA("pool", (lambda c: lambda e: e.tensor_copy(out=RAW[:, c, 0:3], in_=RAW[:, c, ST:ST + 3]))(c), r=[rk], w=[rk])
                    if c >= 8:
                        FA("act", (lambda c: lambda e: e.activation(out=QKVc[:, c, :], in_=CV[:, c, :], func=AF.Silu))(c), r=["CV%d" % c], w=["QKV%d" % c + kp])
                    else:
                        FA("act", (lambda c: lambda e: e.activation(out=SIL[:, c, :], in_=CV[:, c, :], func=AF.Silu))(c), r=["CV%d" % c], w=["SIL%d" % c])
                        FA("act", (lambda c: lambda e: e.activation(out=SQ[:, c, :], in_=SIL[:, c, :], func=AF.Square))(c), r=["SIL%d" % c], w=["SQ%d" % c])
                        FA("pe", (lambda c: lambda e: e.matmul(PB[3][:, ST:2 * ST], lhsT=ONB[:], rhs=SQ[:, c, :], start=True, stop=True))(c),
                              r=["SQ%d" % c, "ONB"], w=qs(3, 2, 2))
                        FA("act", (lambda c: lambda e: e.activation(out=LNT[:, c, :], in_=PB[3][:, ST:2 * ST], func=AF.Ln, bias=EPSC[:]))(c),
                              r=qs(3, 2, 2) + ["EPSC"], w=["LNT%d" % c])
                        FA("act", (lambda c: lambda e: e.activation(out=LNT[:, c, :], in_=LNT[:, c, :], func=AF.Exp, scale=-0.5))(c), r=["LNT%d" % c], w=["LNT%d" % c])
                        sc_ = (128.0 ** -0.5) if c < 4 else 1.0
                        FA("dve", (lambda c, sc_: lambda e: e.scalar_tensor_tensor(out=QKVc[:, c, :], in0=SIL[:, c, :], scalar=sc_, in1=LNT[:, c, :],
                                                                                      op0=ALU.mult, op1=ALU.mult))(c, sc_),
                              r=["SIL%d" % c, "LNT%d" % c], w=["QKV%d" % c + kp])
                if stop <= 4:
                    return pending, None
                def chain(u, h):
                    cs = slice(u * 128, (u + 1) * 128)
                    t = TS[h]; tk = "_%d" % h
                    K = lambda n: n + tk
                    hb = 4 + h; Hf = PB[hb]; H16 = PBbH[h]
                    Q = lambda q_: Hf[:, q_ * 128:(q_ + 1) * 128]
                    QK = lambda q_: qs(hb, q_)
                    kT = QKVc[:, 4 + h, cs]; vT = QKVc[:, 8 + h, cs]; qT = QKVc[:, h, cs]
                    kk, vk, qk = "QKV%d" % (4 + h) + kp, "QKV%d" % (8 + h) + kp, "QKV%d" % h + kp
                    gcol = GSTc[:, u, h:h + 1]
                    gk, bk_, nbk = "GST%d" % u + kp, "BET%d" % u + kp, "NBET%d" % u + kp
                    gc = t["gc"]
                    BETl, NBETl, ZSl = BETc, NBETc, ZSc
                    P.add("dve", lambda e: e.tensor_scalar(out=t["gsb"][:], in0=ONF[:], scalar1=gcol, scalar2=None, op0=ALU.mult), r=["ONF", gk], w=[K("gsb")])
                    yield
                    P.add("pe", _grp([lambda e: e.matmul(Q(0), lhsT=t["gsb"][:], rhs=UF[:], start=True, stop=True),
                                      lambda e: e.matmul(Hf[:, 128:129], lhsT=UF[:], rhs=gcol, start=True, stop=True),
                                      lambda e: e.matmul(Hf[:, 129:130], lhsT=ONF[:], rhs=gcol, start=True, stop=True)]),
                          r=[K("gsb"), "UF", "ONF", gk], w=QK(0) + QK(1))
                    P.add("dve", lambda e: e.tensor_copy(out=gc[:, 0:2], in_=Hf[:, 128:130]), r=QK(1), w=[K("gc")])
                    if own:
                        P.add("act", lambda e: e.activation(out=t["egb"][:], in_=Q(0), func=AF.Exp), r=QK(0), w=[K("egb")])
                    yield
                    P.add("dve", lambda e: e.tensor_scalar(out=gc[:, 2:3], in0=gc[:, 0:1], scalar1=-1.0, scalar2=None, op0=ALU.mult), r=[K("gc")], w=[K("gc2")])
                    P.add("dve", lambda e: e.tensor_tensor(out=gc[:, 3:4], in0=gc[:, 1:2], in1=gc[:, 0:1], op=ALU.subtract), r=[K("gc")], w=[K("gc3")])
                    P.add("act", lambda e: e.activation(out=gc[:, 4:5], in_=gc[:, 0:1], func=AF.Exp), r=[K("gc")], w=[K("gc4")])
                    P.add("act", lambda e: e.activation(out=gc[:, 6:7], in_=gc[:, 1:2], func=AF.Exp), r=[K("gc")], w=[K("gc6")])
                    yield
                    P.add("dve", lambda e: e.tensor_tensor(out=gc[:, 4:5], in0=gc[:, 4:5], in1=BETl[:, u, h:h + 1], op=ALU.mult), r=[K("gc4"), bk_], w=[K("gc4")])
                    P.add("act", lambda e: e.activation(out=gc[:, 5:6], in_=gc[:, 3:4], func=AF.Exp), r=[K("gc3")], w=[K("gc5")])
                    P.add("pe", _grp([lambda e: e.matmul(Q(2), lhsT=t["gsb"][:], rhs=UF[:], start=True, stop=False),
                                      lambda e: e.matmul(Q(2), lhsT=IDF[:], rhs=NM2[:], start=False, stop=True)]),
                          r=[K("gsb"), "UF", "IDF", "NM2"], w=QK(2))
                    P.add("act", lambda e: e.activation(out=t["decS"][:], in_=Q(2), func=AF.Exp, bias=gc[:, 0:1], scale=-1.0), r=QK(2) + [K("gc")], w=[K("decS")])
                    yield
                    P.add("pe", _grp([lambda e: e.transpose(H16[:, 768:896], kT, IDB[:]), lambda e: e.transpose(H16[:, 896:1024], vT, IDB[:])]),
                          r=[kk, vk, "IDB"], w=QK(3))
                    P.add("dve", lambda e: e.tensor_scalar(out=t["kbg"][:], in0=H16[:, 768:896], scalar1=gc[:, 4:5], scalar2=None, op0=ALU.mult), r=QK(3) + [K("gc4")], w=[K("kbg")])
                    P.add("dve", lambda e: e.tensor_scalar(out=t["kdec"][:], in0=H16[:, 768:896], scalar1=gc[:, 5:6], scalar2=None, op0=ALU.mult), r=QK(3) + [K("gc5")], w=[K("kdec")])
                    P.add("dve", lambda e: e.tensor_scalar(out=t["vb"][:], in0=H16[:, 896:1024], scalar1=BETl[:, u, h:h + 1], scalar2=None, op0=ALU.mult), r=QK(3) + [bk_], w=[K("vb")])
                    yield
                    P.add("pe", lambda e: e.matmul(Q(2), lhsT=kT, rhs=kT, start=True, stop=True), r=[kk], w=QK(2))
                    P.add("dve", lambda e: e.scalar_tensor_tensor(out=t["A"][:], in0=Q(2), scalar=NBETl[:, u, h:h + 1], in1=t["decS"][:], op0=ALU.mult, op1=ALU.mult),
                          r=QK(2) + [nbk, K("decS")], w=[K("A")])
                    yield
                    P.add("pe", lambda e: e.transpose(H16[:, 768:896], t["A"][:], IDB[:]), r=[K("A"), "IDB"], w=QK(3))
                    P.add("dve", lambda e: e.tensor_copy(out=t["AT"][:], in_=H16[:, 768:896]), r=QK(3), w=[K("AT")])
                    P.add("dve", lambda e: e.tensor_tensor(out=t["P0"][:], in0=t["A"][:], in1=IMK[:, 0, :], op=ALU.mult), r=[K("A"), "IMK"], w=[K("P0")])
                    P.add("pool", lambda e: e.tensor_tensor(out=t["TM"][:], in0=t["P0"][:], in1=IDB[:], op=ALU.add), r=[K("P0"), "IDB"], w=[K("TM")])
                    yield
                    P.add("dve", lambda e: e.tensor_tensor(out=t["PT0"][:], in0=t["AT"][:], in1=IMK[:, 0, :], op=ALU.mult), r=[K("AT"), "IMK"], w=[K("PT0")])
                    P.add("pool", lambda e: e.tensor_tensor(out=t["RT"][:], in0=t["PT0"][:], in1=IDB[:], op=ALU.add), r=[K("PT0"), "IDB"], w=[K("RT")])
                    P.add("dve", lambda e: e.tensor_tensor(out=t["BKall"][:], in0=t["AT"][:].unsqueeze(1).to_broadcast([128, 4, 128]), in1=IMK[:, 1:5, :], op=ALU.mult),
                          r=[K("AT"), "IMK"], w=[K("BKall")])
                    yield

                    def upd(ln, with_T):
                        P.add("pe", lambda e: e.matmul(Q(2), lhsT=t[ln][:], rhs=t["RT"][:], start=True, stop=True), r=[K(ln), K("RT")], w=QK(2))
                        if with_T:
                            P.add("pe", lambda e: e.matmul(Q(3), lhsT=t["RT"][:], rhs=t[ln][:], start=True, stop=True), r=[K(ln), K("RT")], w=QK(3))
                        P.add("dve", lambda e: e.tensor_tensor(out=t["RT"][:], in0=t["RT"][:], in1=Q(2), op=ALU.add), r=QK(2) + [K("RT")], w=[K("RT")])
                        if with_T:
                            P.add("dve", lambda e: e.tensor_tensor(out=t["TM"][:], in0=t["TM"][:], in1=Q(3), op=ALU.add), r=QK(3) + [K("TM")], w=[K("TM")])
                    P.add("pe", lambda e: e.matmul(Q(0), lhsT=t["PT0"][:], rhs=t["P0"][:], start=True, stop=True), r=[K("P0"), K("PT0")], w=QK(0))
                    P.add("pe", lambda e: e.matmul(Q(1), lhsT=t["P0"][:], rhs=t["PT0"][:], start=True, stop=True), r=[K("P0"), K("PT0")], w=QK(1))
                    P.add("act", lambda e: e.copy(out=t["P1"][:], in_=Q(0)), r=QK(0), w=[K("P1")])
                    P.add("act", lambda e: e.copy(out=t["PT1"][:], in_=Q(1)), r=QK(1), w=[K("PT1")])
                    yield
                    upd("P1", True)
                    yield
                    P.add("pe", lambda e: e.matmul(Q(0), lhsT=t["PT1"][:], rhs=t["P1"][:], start=True, stop=True), r=[K("P1"), K("PT1")], w=QK(0))
                    P.add("act", lambda e: e.copy(out=t["P0"][:], in_=Q(0)), r=QK(0), w=[K("P0")])
                    yield
                    upd("P0", True)
                    yield
                    for lv in range(1, 5):
                        P.add("pe", (lambda lv: lambda e: e.matmul(Q(0), lhsT=t["BKall"][:, lv - 1, :], rhs=t["TM"][:], start=True, stop=True))(lv), r=[K("BKall"), K("TM")], w=QK(0))
                        P.add("act", lambda e: e.copy(out=t["Ym"][:], in_=Q(0)), r=QK(0), w=[K("Ym")])
                        yield
                        upd("Ym", lv < 4)
                        yield
                    P.add("pe", lambda e: e.matmul(Q(0), lhsT=t["kbg"][:], rhs=t["RT"][:], start=True, stop=True), r=[K("kbg"), K("RT")], w=QK(0))
                    P.add("act", lambda e: e.activation(out=t["nwT"][:], in_=Q(0), func=AF.Copy, scale=-1.0), r=QK(0), w=[K("nwT")])
                    yield
                    P.add("pe", _grp([lambda e: e.matmul(Q(1), lhsT=t["RT"][:], rhs=t["vb"][:], start=True, stop=False),
                                      lambda e: e.matmul(Q(1), lhsT=t["nwT"][:], rhs=Sb[h][:], start=False, stop=True)]),
                          r=[K("RT"), K("vb"), K("nwT"), "Sb%d" % h], w=QK(1))
                    P.add("act", lambda e: e.copy(out=t["vnew"][:], in_=Q(1)), r=QK(1), w=[K("vnew")])
                    yield
                    if own:
                        qt_ = (s - OWN0) * 2 + u
                        ocs = slice(qt_ * 128, (qt_ + 1) * 128)
                        P.add("pe", _grp([lambda e: e.matmul(Q(2), lhsT=t["gsb"][:], rhs=UF[:], start=True, stop=False),
                                          lambda e: e.matmul(Q(2), lhsT=IDF[:], rhs=NMT[:], start=False, stop=True)]),
                              r=[K("gsb"), "UF", "IDF", "NMT"], w=QK(2))
                        P.add("pe", lambda e: e.matmul(Q(3), lhsT=kT, rhs=qT, start=True, stop=True), r=[kk, qk], w=QK(3))
                        P.add("act", lambda e: e.activation(out=t["decT"][:], in_=Q(2), func=AF.Exp, bias=gc[:, 2:3]), r=QK(2) + [K("gc2")], w=[K("decT")])
                        P.add("pool", lambda e: e.tensor_tensor(out=t["qdT"][:], in0=qT, in1=t["egb"][:], op=ALU.mult), r=[qk, K("egb")], w=[K("qdT")])
                        yield
                        P.add("dve", lambda e: e.tensor_tensor(out=t["attnT"][:], in0=Q(3), in1=t["decT"][:], op=ALU.mult), r=QK(3) + [K("decT")], w=[K("attnT")])
                        yield
                        P.add("pe", _grp([lambda e: e.matmul(Q(0), lhsT=Sb[h][:], rhs=t["qdT"][:], start=True, stop=False),
                                          lambda e: e.matmul(Q(0), lhsT=t["vnew"][:], rhs=t["attnT"][:], start=False, stop=True)]),
                              r=["Sb%d" % h, K("qdT"), K("vnew"), K("attnT")], w=QK(0))
                        P.add("act", lambda e: e.activation(out=t["sq"][:], in_=Q(0), func=AF.Square), r=QK(0), w=[K("sq")])
                        P.add("act", lambda e: e.copy(out=t["o1"][:], in_=Q(0)), r=QK(0), w=[K("o1")])
                        yield
                        P.add("pe", lambda e: e.matmul(Q(1), lhsT=ONB[:], rhs=t["sq"][:], start=True, stop=True), r=[K("sq"), "ONB"], w=QK(1))
                        P.add("act", lambda e: e.activation(out=t["egb"][:], in_=Q(1), func=AF.Ln, bias=EPSC[:], scale=1.0 / 128), r=QK(1) + ["EPSC", K("qdT")], w=[K("egb")])
                        P.add("act", lambda e: e.activation(out=t["egb"][:], in_=t["egb"][:], func=AF.Exp, scale=-0.5), r=[K("egb")], w=[K("egb")])
                        yield
                        P.add("dve", lambda e: e.tensor_tensor(out=t["o1"][:], in0=t["o1"][:], in1=t["egb"][:], op=ALU.mult), r=[K("o1"), K("egb")], w=[K("o1")])
                        P.add("dve", lambda e: e.scalar_tensor_tensor(out=OTA[:, h, ocs], in0=t["o1"][:], scalar=ANW[:, 0:1], in1=ZSl[:, h, cs], op0=ALU.mult, op1=ALU.mult),
                              r=[K("o1"), "ANW", "ZS%d" % h + kp], w=["OTA%d_%d" % (h, qt_)])
                        yield
                    P.add("pe", lambda e: e.matmul(Q(2), lhsT=t["kdec"][:], rhs=t["vnew"][:], start=True, stop=True), r=[K("kdec"), K("vnew")], w=QK(2))
                    P.add("dve", lambda e: e.scalar_tensor_tensor(out=Sm[h][:], in0=Sm[h][:], scalar=gc[:, 6:7], in1=Q(2), op0=ALU.mult, op1=ALU.add),
                          r=QK(2) + [K("gc6"), "Sm%d" % h], w=["Sm%d" % h])
                    P.add("act", lambda e: e.copy(out=Sb[h][:], in_=Sm[h][:]), r=["Sm%d" % h], w=["Sb%d" % h])
                    yield

                return pending, chain

            def run_round(chain_fn, pend):
                pi = 0
                if chain_fn is not None:
                    per = max(1, -(-len(pend) // 60))
                    for u in range(2):
                        gens = [chain_fn(u, h) for h in range(4)]
                        while gens:
                            for g_ in list(gens):
                                try:
                                    next(g_)
                                except StopIteration:
                                    gens.remove(g_)
                            for _ in range(per):
                                if pi < len(pend):
                                    P.add(*pend[pi][0], **pend[pi][1]); pi += 1
                while pi < len(pend):
                    P.add(*pend[pi][0], **pend[pi][1]); pi += 1

            prev_chain = None
            for s_ in list(a_list) + [None]:
                pend, ch = do_st(s_) if s_ is not None else ([], None)
                run_round(prev_chain, pend)
                prev_chain = ch
            fin = []
            if dbg:
                for nm, tsr in (("HT", HT), ("QKV", QKVL[0]), ("RAW", RAW), ("ZS", ZSL[0]), ("GST", GSTL[0]), ("BET", BETL[0]), ("ABT", ABTL[0]), ("CV", CV),
                                ("A1C", A1C), ("SH1C", SH1C), ("Sm0", Sm[0]), ("decS", TS[DS]["decS"]), ("A", TS[DS]["A"]), ("RT", TS[DS]["RT"]),
                                ("vnew", TS[DS]["vnew"]), ("gc", TS[DS]["gc"]), ("kbg", TS[DS]["kbg"]), ("kdec", TS[DS]["kdec"]), ("nwT", TS[DS]["nwT"]),
                                ("decT", TS[DS]["decT"]), ("attnT", TS[DS]["attnT"]), ("qdT", TS[DS]["qdT"]), ("o1", TS[DS]["o1"])):
                    dd = nc.dram_tensor("dump_" + nm, list(tsr.shape), F32, kind="ExternalOutput").ap()
                    P.add("pool", (lambda dd, tsr: lambda e: e.dma_start(out=dd, in_=tsr[:]))(dd, tsr), w=["dump_" + nm], dma=True, after_all=True)
                    fin.append("dump_" + nm)
                P.add("pool", lambda e: e.dma_start(out=dbg_o["ota"], in_=OTA[:]), w=["dbg_ota"], dma=True, after_all=True)
                fin.append("dbg_ota")
            P.emit(st, final_wait_keys=fin)
            print("phase A ops:", len(P.ops))

        with ExitStack() as st:
            S = mk(st)
            P = Prog(nc)
            WB = S("WB", [128, 8, 1536], BF16); WO = S("WO", [128, 8, D], BF16)
            BM5 = S("BM5", [128, 8, 640]); KR = S("KR", [128, 4, 1024], BF16); VR = S("VR", [128, 8, 512], BF16)
            WRF = S("WRF", [128, 8, NE]); BR = S("BR", [128, NE]); KM = S("KM", [128, 1]); G1B = S("G1B", [128, D])
            XT = [S("XTb%d" % i, [128, D]) for i in range(2)]
            JNK = S("JNKb", [128, D], BF16)
            XS = [S("XSb%d" % i, [128, D], BF16) for i in range(2)]
            HT = S("HTb", [128, 8, ST], BF16)
            QB = S("QB", [128, 4, ST], BF16)
            STSL = [S("STS%d" % i, [128, 640]) for i in range(2)]; PTL = [S("PT%d" % i, [128, 640], BF16) for i in range(2)]
            RDENL = [S("RDEN%d" % i, [64, 128]) for i in range(2)]
            OTB = S("OTB", [128, 4, ST], BF16)
            T1 = S("T1", [128, D]); H2 = S("H2", [128, D]); H2F = S("H2F", [128, 8, 128]); H2B = S("H2B", [128, 8, 128], BF16)
            SSC = S("SSCb", [128, 8]); RSC = S("RSCb", [128, 8])
            w_in_r = w_in.rearrange("(k p) c -> p k c", p=128)
            P.add("pool", lambda e: e.dma_start(out=WB[:], in_=w_in_r[:, :, 2056:3592]), w=["WB"], dma=True)
            P.add("pool", lambda e: e.dma_start(out=WO[:], in_=w_out.rearrange("(k p) c -> p k c", p=128)), w=["WO"], dma=True)
            P.add("sp", lambda e: e.dma_start(out=BM5[:], in_=bm5_d), w=["BM5"], dma=True)
            P.add("sp", lambda e: e.dma_start(out=WRF[:], in_=w_router.rearrange("(k p) c -> p k c", p=128)), w=["WRF"], dma=True)
            P.add("sp", lambda e: e.dma_start(out=BR[:], in_=br_d), w=["BR"], dma=True)
            P.add("sp", lambda e: e.dma_start(out=KM[:], in_=kmask_d), w=["KM"], dma=True)
            for half in range(2):
                P.add("pe", (lambda half: lambda e: e.matmul(PB[1][:, :], lhsT=ONF[0:1, :], rhs=G1R[0:1, half * 512:(half + 1) * 512], start=True, stop=True))(half),
                      r=[], w=bank(1))
                P.add("dve", (lambda half: lambda e: e.tensor_copy(out=G1B[:, half * 512:(half + 1) * 512], in_=PB[1][:, :]))(half), r=bank(1), w=["G1B"])
            xw_t = xw.rearrange("(n p) d -> n p d", p=128)
            x1s_t = x1s.rearrange("(n p) d -> n p d", p=128)
            PBb0 = PB[0][:].bitcast(BF16)
            for s in (range(HALO0, NST) if do_b else []):
                own = s >= OWN0
                for u in range(2):
                    ti = 2 * s + u
                    xt = XT[u]; xs_ = XS[u]
                    P.add("sp", (lambda xt, ti: lambda e: e.dma_start(out=xt[:], in_=xw_t[ti]))(xt, ti), w=["XT%d" % u], dma=True)
                    P.add("act", (lambda xt, u: lambda e: e.activation(out=JNK[:], in_=xt[:], func=AF.Square, accum_out=SSC[:, u:u + 1]))(xt, u),
                          r=["XT%d" % u], w=["JNK", "SSC%d" % u])
                    P.add("act", (lambda u: lambda e: e.activation(out=RSC[:, u:u + 1], in_=SSC[:, u:u + 1], func=AF.Ln, bias=EPSC[:], scale=1.0 / D))(u),
                          r=["SSC%d" % u], w=["RSC%d" % u])
                    P.add("act", (lambda u: lambda e: e.activation(out=RSC[:, u:u + 1], in_=RSC[:, u:u + 1], func=AF.Exp, scale=-0.5))(u),
                          r=["RSC%d" % u], w=["RSC%d" % u])
                    P.add("dve", (lambda xt, xs_, u: lambda e: e.tensor_scalar(out=xs_[:], in0=xt[:], scalar1=RSC[:, u:u + 1], scalar2=None, op0=ALU.mult))(xt, xs_, u),
                          r=["XT%d" % u, "RSC%d" % u], w=["XS%d" % u])
                    P.add("pe", (lambda xs_: _grp([(lambda k: lambda e: e.transpose(PBb0[:, k * 128:(k + 1) * 128], xs_[:, k * 128:(k + 1) * 128], IDB[:]))(k)
                                                   for k in range(8)]))(xs_), r=["XS%d" % u], w=bank(0))
                    for k in range(8):
                        eng = "act"
                        if eng == "act":
                            f = (lambda k, u: lambda e: e.activation(out=HT[:, k, u * 128:(u + 1) * 128], in_=PBb0[:, k * 128:(k + 1) * 128],
                                                                     func=AF.Identity, bias=SH1C[:, k:k + 1], scale=A1C[:, k:k + 1]))(k, u)
                        else:
                            f = (lambda k, u: lambda e: e.tensor_scalar(out=HT[:, k, u * 128:(u + 1) * 128], in0=PBb0[:, k * 128:(k + 1) * 128],
                                                                        scalar1=A1C[:, k:k + 1], scalar2=SH1C[:, k:k + 1], op0=ALU.mult, op1=ALU.add))(k, u)
                        P.add(eng, f, r=bank(0), w=["HT%d_%d" % (k, u)])
                HTK = ["HT%d_%d" % (k, u) for k in range(8) for u in range(2)]
                slot0 = (2 * s) % 8
                for p in range(4):
                    P.add("pe", (lambda p: _grp([(lambda k: lambda e: e.matmul(PB[1][:, 0:ST], lhsT=WB[:, k, 512 + p * 128:512 + (p + 1) * 128], rhs=HT[:, k, :],
                                                                            start=(k == 0), stop=(k == 7)))(k) for k in range(8)]))(p),
                          r=HTK + ["WB"], w=qs(1, 0, 2))
                    P.add("act", (lambda p, slot0: lambda e: e.copy(out=KR[:, p, slot0 * 128:slot0 * 128 + ST], in_=PB[1][:, 0:ST]))(p, slot0),
                          r=qs(1, 0, 2), w=["KR%d_%d" % (p, slot0), "KR%d_%d" % (p, slot0 + 1)])
                    if own:
                        P.add("pe", (lambda p: _grp([(lambda k: lambda e: e.matmul(PB[1][:, ST:2 * ST], lhsT=WB[:, k, p * 128:(p + 1) * 128], rhs=HT[:, k, :],
                                                                                start=(k == 0), stop=(k == 7)))(k) for k in range(8)]))(p),
                              r=HTK + ["WB"], w=qs(1, 2, 2))
                        P.add("act", (lambda p: lambda e: e.activation(out=QB[:, p, :], in_=PB[1][:, ST:2 * ST], func=AF.Copy, scale=0.125))(p),
                              r=qs(1, 2, 2), w=["QB%d" % p])
                for u in range(2):
                    P.add("pe", (lambda u: _grp([(lambda k: lambda e: e.matmul(PB[2][:, :], lhsT=HT[:, k, u * 128:(u + 1) * 128], rhs=WB[:, k, 1024:1536],
                                                                            start=(k == 0), stop=(k == 7)))(k) for k in range(8)]))(u),
                          r=HTK + ["WB"], w=bank(2))
                    P.add("dve", (lambda u, slot0: lambda e: e.tensor_copy(out=VR[:, slot0 + u, :], in_=PB[2][:, :]))(u, slot0), r=bank(2), w=["VR%d" % (slot0 + u)])
                if not own:
                    continue
                for u in range(2):
                    qt_ = (s - OWN0) * 2 + u
                    W = 48 + qt_
                    slots = [(W - 4 + t) % 8 for t in range(5)]
                    ucs = slice(u * 128, (u + 1) * 128)
                    nh = max(0, 4 - qt_)
                    def attn(hb, bs, slots, ucs, nh, u):
                        p, r0 = hb // 2, (hb % 2) * 64
                        ba, bb = 3 + 2 * bs, 4 + 2 * bs
                        STSs, PTs, RDENs = STSL[bs], PTL[bs], RDENL[bs]
                        sk = "_%d" % bs
                        fns = []
                        for t in range(5):
                            o_ = PB[ba][:, t * 128:(t + 1) * 128] if t < 4 else PB[bb][:, 0:128]
                            fns.append((lambda o_, sl: lambda e: e.matmul(o_, lhsT=KR[r0:r0 + 64, p, sl * 128:(sl + 1) * 128], rhs=QB[r0:r0 + 64, p, ucs],
                                                                          start=True, stop=True))(o_, slots[t]))
                        P.add("pe", _grp(fns), r=["QB%d" % p] + ["KR%d_%d" % (p, sl) for sl in slots], w=bank(ba) + qs(bb, 0))
                        yield
                        P.add("dve", lambda e: e.tensor_tensor(out=STSs[:, 0:512], in0=PB[ba][:, :], in1=BM5[:, hb, 0:512], op=ALU.add), r=bank(ba) + ["BM5"], w=["STSa" + sk])
                        P.add("dve", lambda e: e.tensor_tensor(out=STSs[:, 512:640], in0=PB[bb][:, 0:128], in1=BM5[:, hb, 512:640], op=ALU.add), r=qs(bb, 0) + ["BM5"], w=["STSb" + sk])
                        yield
                        if nh > 0:
                            P.add("act", lambda e: e.activation(out=PTs[:, 0:nh * 128], in_=STSs[:, 0:nh * 128], func=AF.Exp, bias=KM[:, 0:1]), r=["STSa" + sk, "KM"], w=["PTa" + sk])
                        P.add("act", lambda e: e.activation(out=PTs[:, nh * 128:640], in_=STSs[:, nh * 128:640], func=AF.Exp), r=["STSa" + sk, "STSb" + sk], w=["PTb" + sk])
                        yield
                        fns = [(lambda t, sl: lambda e: e.matmul(PB[bb][0:64, 128:256], lhsT=VR[:, sl, hb * 64:(hb + 1) * 64], rhs=PTs[:, t * 128:(t + 1) * 128],
                                                                 start=(t == 0), stop=(t == 4)))(t, slots[t]) for t in range(5)]
                        P.add("pe", _grp(fns), r=["PTa" + sk, "PTb" + sk] + ["VR%d" % sl for sl in slots], w=qs(bb, 1))
                        fns = [(lambda t: lambda e: e.matmul(PB[bb][0:64, 256:384], lhsT=ONB[:, 0:64], rhs=PTs[:, t * 128:(t + 1) * 128],
                                                             start=(t == 0), stop=(t == 4)))(t) for t in range(5)]
                        P.add("pe", _grp(fns), r=["PTa" + sk, "PTb" + sk], w=qs(bb, 2))
                        yield
                        P.add("dve", lambda e: e.reciprocal(out=RDENs[:], in_=PB[bb][0:64, 256:384]), r=qs(bb, 2), w=["RDEN" + sk])
                        P.add("dve", lambda e: e.tensor_tensor(out=OTB[r0:r0 + 64, p, ucs], in0=PB[bb][0:64, 128:256], in1=RDENs[:], op=ALU.mult),
                              r=qs(bb, 1) + ["RDEN" + sk], w=["OTB%d_%d_%d" % (p, u, hb % 2)])
                        yield

                    for hp in range(4):
                        gens = [attn(2 * hp + z, z, slots, ucs, nh, u) for z in range(2)]
                        while gens:
                            for g_ in list(gens):
                                try:
                                    next(g_)
                                except StopIteration:
                                    gens.remove(g_)
                    ocs = slice(qt_ * 128, (qt_ + 1) * 128)
                    for half in range(2):
                        fns = []
                        for c in range(8):
                            l_ = OTA[:, c, ocs] if c < 4 else OTB[:, c - 4, ucs]
                            fns.append((lambda c, l_, half: lambda e: e.matmul(PB[5 + half][:, :], lhsT=l_, rhs=WO[:, c, half * 512:(half + 1) * 512],
                                                                               start=(c == 0), stop=(c == 7)))(c, l_, half))
                        P.add("pe", _grp(fns), r=["WO"] + ["OTA%d_%d" % (h, qt_) for h in range(4)] + ["OTB%d_%d_%d" % (p, u, z) for p in range(4) for z in range(2)],
                              w=bank(5 + half))
                        P.add("act", (lambda half: lambda e: e.activation(out=JNK[:, half * 512:(half + 1) * 512], in_=PB[5 + half][:, :], func=AF.Square,
                                                                          accum_out=SSC[:, 2 + half:3 + half]))(half), r=bank(5 + half), w=["JNK", "SSC%d" % (2 + half)])
                    P.add("dve", lambda e: e.tensor_tensor(out=SSC[:, 4:5], in0=SSC[:, 2:3], in1=SSC[:, 3:4], op=ALU.add), r=["SSC2", "SSC3"], w=["SSC4"])
                    P.add("act", lambda e: e.activation(out=RSC[:, 4:5], in_=SSC[:, 4:5], func=AF.Ln, bias=EPSC[:], scale=1.0 / D), r=["SSC4"], w=["RSC4"])
                    P.add("act", lambda e: e.activation(out=RSC[:, 4:5], in_=RSC[:, 4:5], func=AF.Exp, scale=-0.5), r=["RSC4"], w=["RSC4"])
                    for half in range(2):
                        hs = slice(half * 512, (half + 1) * 512)
                        P.add("dve", (lambda half, hs: lambda e: e.scalar_tensor_tensor(out=T1[:, hs], in0=PB[5 + half][:, :], scalar=RSC[:, 4:5], in1=G1B[:, hs],
                                                                                        op0=ALU.mult, op1=ALU.mult))(half, hs),
                              r=bank(5 + half) + ["RSC4", "G1B"], w=["T1_%d" % half])
                    P.add("pool", (lambda u: lambda e: e.tensor_tensor(out=T1[:], in0=T1[:], in1=XT[u][:], op=ALU.add))(u), r=["T1_0", "T1_1", "XT%d" % u], w=["T1_0", "T1_1"])
                    P.add("sp", (lambda qt_: lambda e: e.dma_start(out=x1s_t[qt_], in_=T1[:]))(qt_), r=["T1_0", "T1_1"], w=["x1s"], dma=True)
                    if dbg:
                        P.add("sp", (lambda qt_: lambda e: e.dma_start(out=dbg_o["x1"].rearrange("(n p) d -> n p d", p=128)[qt_], in_=T1[:]))(qt_), r=["T1_0", "T1_1"], w=["dbg_x1"], dma=True)
                    P.add("act", lambda e: e.activation(out=JNK[:], in_=T1[:], func=AF.Square, accum_out=SSC[:, 5:6]), r=["T1_0", "T1_1"], w=["JNK", "SSC5"])
                    P.add("act", lambda e: e.activation(out=RSC[:, 5:6], in_=SSC[:, 5:6], func=AF.Ln, bias=EPSC[:], scale=1.0 / D), r=["SSC5"], w=["RSC5"])
                    P.add("act", lambda e: e.activation(out=RSC[:, 5:6], in_=RSC[:, 5:6], func=AF.Exp, scale=-0.5), r=["RSC5"], w=["RSC5"])
                    P.add("dve", lambda e: e.tensor_scalar(out=H2[:], in0=T1[:], scalar1=RSC[:, 5:6], scalar2=None, op0=ALU.mult), r=["T1_0", "T1_1", "RSC5"], w=["H2"])
                    for half in range(2):
                        P.add("pe", (lambda half: _grp([(lambda k: lambda e: e.transpose(PB[5 + half][:, (k % 4) * 128:(k % 4 + 1) * 128], H2[:, k * 128:(k + 1) * 128], IDF[:]))(k)
                                                        for k in range(half * 4, half * 4 + 4)]))(half), r=["H2"], w=bank(5 + half))
                        for k in range(half * 4, half * 4 + 4):
                            eng = "act"
                            src = PB[5 + half][:, (k % 4) * 128:(k % 4 + 1) * 128]
                            if eng == "act":
                                f = (lambda k, src: lambda e: e.activation(out=H2F[:, k, :], in_=src, func=AF.Identity, bias=SH2C[:, k:k + 1], scale=A2C[:, k:k + 1]))(k, src)
                            else:
                                f = (lambda k, src: lambda e: e.tensor_scalar(out=H2F[:, k, :], in0=src, scalar1=A2C[:, k:k + 1], scalar2=SH2C[:, k:k + 1], op0=ALU.mult, op1=ALU.add))(k, src)
                            P.add(eng, f, r=bank(5 + half), w=["H2F%d" % k])
                    H2FK = ["H2F%d" % k for k in range(8)]
                    P.add("pool", lambda e: e.tensor_copy(out=H2B[:], in_=H2F[:]), r=H2FK, w=["H2B"])
                    P.add("sp", (lambda ocs: lambda e: e.dma_start(out=h2s[:, :, ocs], in_=H2B[:]))(ocs), r=["H2B"], w=["h2s"], dma=True)
                    P.add("pe", _grp([(lambda k: lambda e: e.matmul(PB[7][:, 0:NE], lhsT=H2F[:, k, :], rhs=WRF[:, k, :], start=(k == 0), stop=(k == 7)))(k) for k in range(8)]),
                          r=H2FK + ["WRF"], w=qs(7, 0))
                    P.add("dve", (lambda qt_: lambda e: e.tensor_tensor(out=LG[:, qt_, :], in0=PB[7][:, 0:NE], in1=BR[:], op=ALU.add))(qt_), r=qs(7, 0) + ["BR"], w=["LG"])
            fin = ["x1s", "h2s"]
            if dbg and do_b:
                P.add("sp", lambda e: e.dma_start(out=dbg_o["lg"], in_=LG[:]), r=["LG"], w=["dbg_lg"], dma=True)
                fin += ["dbg_lg", "dbg_x1"]
            P.emit(st, final_wait_keys=fin)
            print("phase B ops:", len(P.ops))

        stAB.close()
        with ExitStack() as st:
            S = mk(st)
            P = Prog(nc)
            H2T = S("H2T", [128, 8, NTOK], BF16); Y = S("Y", [128, 16, D]); G = S("G", [128, 16, NE])
            WU = S("WU", [128, 8, 2 * D], BF16); WD = S("WD", [128, 8, D], BF16); ACTT = S("ACTT", [128, 8, 1024], BF16)
            BU = S("BU", [128, NE, 16]); BDN = S("BDN", [NE, D]); GT = S("GT", [NE, 128])
            TG = [S("TG%d" % i, [128, 512]) for i in range(2)]
            TSg = [S("TSg%d" % i, [128, 512], BF16) for i in range(2)]
            TL = [S("TL%d" % i, [128, 512]) for i in range(2)]
            G2B = S("G2B", [128, D]); X1T = S("X1T", [128, D]); JNK = S("JNKc", [128, D], BF16)
            T8 = S("T8", [128, 8]); MSK = S("MSK", [128, NE]); EX = S("EX", [128, NE]); CL = S("CL", [128, 8])
            P.add("sp", lambda e: e.dma_start(out=BU[:], in_=bu_d), w=["BU"], dma=True)
            P.add("dve", lambda e: e.tensor_scalar(out=BU[:, :, 8:16], in0=BU[:, :, 8:16], scalar1=1.0, scalar2=None, op0=ALU.add), r=["BU"], w=["BU"])
            P.add("sp", lambda e: e.dma_start(out=BDN[0:n_exp, :], in_=b_down), w=["BDN"], dma=True)
            for hh in range(2):
                P.add("sp", (lambda hh: lambda e: e.dma_start(out=H2T[:, :, hh * 1024:(hh + 1) * 1024], in_=h2s[:, :, hh * 1024:(hh + 1) * 1024]))(hh), w=["H2T"], dma=True)
            P.add("pool", lambda e: e.dma_start(out=WU[:], in_=w_up[0].rearrange("(k p) f -> p k f", p=128)), w=["WU"], dma=True)
            P.add("pool", lambda e: e.dma_start(out=WD[:], in_=w_down[0].rearrange("(k p) f -> p k f", p=128)), w=["WD"], dma=True)
            P.add("pool", lambda e: e.memset(Y[:], 0.0), w=["Y%d" % i for i in range(16)])
            for half in range(2):
                P.add("pe", (lambda half: lambda e: e.matmul(PB[1][:, :], lhsT=ONF[0:1, :], rhs=G2R[0:1, half * 512:(half + 1) * 512], start=True, stop=True))(half),
                      r=[], w=bank(1))
                P.add("dve", (lambda half: lambda e: e.tensor_copy(out=G2B[:, half * 512:(half + 1) * 512], in_=PB[1][:, :]))(half), r=bank(1), w=["G2B"])
            for tt in range(16):
                L_ = LG[:, tt, :]
                P.add("dve", (lambda L_: lambda e: e.max(out=T8[:], in_=L_))(L_), r=[], w=["T8"])
                P.add("dve", (lambda L_: lambda e: e.tensor_scalar(out=MSK[:], in0=L_, scalar1=T8[:, 3:4], scalar2=None, op0=ALU.is_ge))(L_), r=["T8"], w=["MSK"])
                P.add("dve", lambda e: e.tensor_scalar(out=CL[:, 0:1], in0=T8[:, 0:1], scalar1=-1.0, scalar2=None, op0=ALU.mult), r=["T8"], w=["CL0"])
                P.add("act", (lambda L_: lambda e: e.activation(out=EX[:], in_=L_, func=AF.Exp, bias=CL[:, 0:1]))(L_), r=["CL0"], w=["EX"])
                P.add("dve", lambda e: e.tensor_tensor(out=EX[:], in0=EX[:], in1=MSK[:], op=ALU.mult), r=["EX", "MSK"], w=["EX"])
                P.add("dve", lambda e: e.reduce_sum(out=CL[:, 1:2], in_=EX[:], axis=AX.X), r=["EX"], w=["CL1"])
                P.add("dve", lambda e: e.reciprocal(out=CL[:, 2:3], in_=CL[:, 1:2]), r=["CL1"], w=["CL2"])
                P.add("dve", (lambda tt: lambda e: e.tensor_scalar(out=G[:, tt, :], in0=EX[:], scalar1=CL[:, 2:3], scalar2=None, op0=ALU.mult))(tt), r=["EX", "CL2"], w=["G"])
            x1s_t = x1s.rearrange("(n p) d -> n p d", p=128)
            out_t = out.rearrange("(n p) d -> n p d", p=128)
            def up_grp(ex, tg):
                slot = tg % 2
                tcs = slice(tg * 512, (tg + 1) * 512)
                for fc in range(8):
                    pb = (fc % 2) * 2; ss = fc % 2
                    for z, fcc in ((0, fc), (1, fc + 8)):
                        P.add("pe", (lambda fcc, pbz, tcs: _grp([(lambda k: lambda e: e.matmul(PB[pbz][:, :], lhsT=WU[:, k, fcc * 128:(fcc + 1) * 128], rhs=H2T[:, k, tcs],
                                                                                           start=(k == 0), stop=(k == 7)))(k) for k in range(8)]))(fcc, pb + z, tcs),
                              r=["WU", "H2T"], w=bank(pb + z))
                    P.add("dve", (lambda ex, fc, pb, ss: lambda e: e.tensor_scalar(out=TG[ss][:], in0=PB[pb][:, :], scalar1=BU[:, ex, fc:fc + 1], scalar2=7.0, op0=ALU.add, op1=ALU.min))(ex, fc, pb, ss),
                          r=bank(pb) + ["BU"], w=["TG%d" % ss])
                    P.add("act", (lambda ss: lambda e: e.activation(out=TSg[ss][:], in_=TG[ss][:], func=AF.Sigmoid, scale=1.702))(ss), r=["TG%d" % ss], w=["TSg%d" % ss])
                    P.add("act", (lambda ex, fc, pb, ss: lambda e: e.activation(out=TL[ss][:], in_=PB[pb + 1][:, :], func=AF.Identity, bias=BU[:, ex, 8 + fc:9 + fc]))(ex, fc, pb, ss),
                          r=bank(pb + 1) + ["BU"], w=["TL%d" % ss])
                    P.add("pool", (lambda ss: lambda e: e.tensor_scalar(out=TL[ss][:], in0=TL[ss][:], scalar1=8.0, scalar2=-6.0, op0=ALU.min, op1=ALU.max))(ss), r=["TL%d" % ss], w=["TL%d" % ss])
                    P.add("dve", (lambda ss: lambda e: e.tensor_tensor(out=TG[ss][:], in0=TG[ss][:], in1=TSg[ss][:], op=ALU.mult))(ss), r=["TG%d" % ss, "TSg%d" % ss], w=["TG%d" % ss])
                    P.add("pool", (lambda fc, ss, slot: lambda e: e.tensor_tensor(out=ACTT[:, fc, slot * 512:(slot + 1) * 512], in0=TG[ss][:], in1=TL[ss][:], op=ALU.mult))(fc, ss, slot),
                          r=["TG%d" % ss, "TL%d" % ss], w=["ACTT%d_%d" % (fc, slot)])

            def down_grp(ex, tg):
                slot = tg % 2
                for t4 in range(4):
                    tt = tg * 4 + t4
                    acs = slice(slot * 512 + t4 * 128, slot * 512 + (t4 + 1) * 128)
                    for half in range(2):
                        pb = 4 + (tt % 2) * 2 + half
                        fns = [(lambda fc, pb, acs, half: lambda e: e.matmul(PB[pb][:, :], lhsT=ACTT[:, fc, acs], rhs=WD[:, fc, half * 512:(half + 1) * 512],
                                                                             start=(fc == 0), stop=(fc == 7)))(fc, pb, acs, half) for fc in range(8)]
                        P.add("pe", _grp(fns), r=["WD"] + ["ACTT%d_%d" % (fc, slot) for fc in range(8)], w=bank(pb))
                        P.add("dve", (lambda tt, half, pb, ex: lambda e: e.scalar_tensor_tensor(out=Y[:, tt, half * 512:(half + 1) * 512], in0=PB[pb][:, :],
                                                                                              scalar=G[:, tt, ex:ex + 1], in1=Y[:, tt, half * 512:(half + 1) * 512],
                                                                                              op0=ALU.mult, op1=ALU.add))(tt, half, pb, ex),
                              r=bank(pb) + ["G", "Y%d" % tt], w=["Y%d" % tt])

            for ex in (range(n_exp) if do_c else []):
                up_grp(ex, 0); up_grp(ex, 1); down_grp(ex, 0); up_grp(ex, 2); down_grp(ex, 1); up_grp(ex, 3)
                if ex + 1 < n_exp:
                    P.add("pool", (lambda ex: lambda e: e.dma_start(out=WU[:], in_=w_up[ex + 1].rearrange("(k p) f -> p k f", p=128)))(ex), w=["WU"], dma=True)
                down_grp(ex, 2); down_grp(ex, 3)
                if ex + 1 < n_exp:
                    P.add("pool", (lambda ex: lambda e: e.dma_start(out=WD[:], in_=w_down[ex + 1].rearrange("(k p) f -> p k f", p=128)))(ex), w=["WD"], dma=True)
            for tt in (range(16) if do_c else []):
                P.add("pe", (lambda tt: lambda e: e.transpose(PB[2][0:NE, 0:128], G[:, tt, :], IDF[:]))(tt), r=["G", "IDF"], w=bank(2))
                P.add("dve", lambda e: e.tensor_copy(out=GT[:], in_=PB[2][0:NE, 0:128]), r=bank(2), w=["GT"])
                for half in range(2):
                    P.add("pe", (lambda half: lambda e: e.matmul(PB[half][:, :], lhsT=GT[0:n_exp, :], rhs=BDN[0:n_exp, half * 512:(half + 1) * 512], start=True, stop=True))(half),
                          r=["GT", "BDN"], w=bank(half))
                    P.add("dve", (lambda tt, half: lambda e: e.tensor_tensor(out=Y[:, tt, half * 512:(half + 1) * 512], in0=Y[:, tt, half * 512:(half + 1) * 512],
                                                                            in1=PB[half][:, :], op=ALU.add))(tt, half), r=bank(half) + ["Y%d" % tt], w=["Y%d" % tt])
                P.add("sp", (lambda tt: lambda e: e.dma_start(out=X1T[:], in_=x1s_t[tt]))(tt), w=["X1T"], dma=True)
                P.add("act", (lambda tt: lambda e: e.activation(out=JNK[:], in_=Y[:, tt, :], func=AF.Square, accum_out=CL[:, 4:5]))(tt), r=["Y%d" % tt], w=["JNK", "CL4"])
                P.add("act", lambda e: e.activation(out=CL[:, 5:6], in_=CL[:, 4:5], func=AF.Ln, bias=EPSC[:], scale=1.0 / D), r=["CL4"], w=["CL5"])
                P.add("act", lambda e: e.activation(out=CL[:, 5:6], in_=CL[:, 5:6], func=AF.Exp, scale=-0.5), r=["CL5"], w=["CL5"])
                P.add("dve", (lambda tt: lambda e: e.scalar_tensor_tensor(out=Y[:, tt, :], in0=Y[:, tt, :], scalar=CL[:, 5:6], in1=G2B[:], op0=ALU.mult, op1=ALU.mult))(tt),
                      r=["Y%d" % tt, "CL5", "G2B"], w=["Y%d" % tt])
                P.add("dve", (lambda tt: lambda e: e.tensor_tensor(out=X1T[:], in0=X1T[:], in1=Y[:, tt, :], op=ALU.add))(tt), r=["Y%d" % tt, "X1T"], w=["X1T"])
                P.add("sp", (lambda tt: lambda e: e.dma_start(out=out_t[tt], in_=X1T[:]))(tt), r=["X1T"], w=["out"], dma=True)
            P.emit(st, final_wait_keys=["out"] if do_c else [])
            print("phase C ops:", len(P.ops))
    return nc


def _host_inputs(inputs):
    x = np.ascontiguousarray(inputs["x"], dtype=np.float32)
    c = inputs["c"]
    rel = inputs["rel_bias"][0]
    kk = np.arange(128)[:, None, None]; t = np.arange(5)[None, :, None]; qq = np.arange(128)[None, None, :]
    diff = 128 * (4 - t) + qq - kk
    idx = np.clip(diff, -128, 128) + 128
    cd = 8 - 2 * t + qq // 64 - kk // 64
    valid = (cd >= 0) & (cd <= 8)
    bm5 = np.empty((128, 8, 640), np.float32)
    for hb in range(8):
        bm5[:, hb, :] = np.where(valid, rel[hb][idx], np.float32(NEG)).reshape(128, 640)
    ii = np.arange(128)[:, None]; jj = np.arange(128)[None, :]
    imask = np.zeros((128, 5, 128), np.float32)
    imask[:, 0, :] = (ii // 8 == jj // 8)
    for lv, sz in enumerate((8, 16, 32, 64), start=1):
        mk = (ii // (2 * sz) == jj // (2 * sz)) & ((ii % (2 * sz)) >= sz) & ((jj % (2 * sz)) < sz)
        imask[:, lv, :] = mk.T
    cw = np.ascontiguousarray(inputs["conv_w"][0].reshape(4, 12, 128).transpose(2, 1, 0))
    bu = np.ascontiguousarray(inputs["b_up"][0].reshape(NE, 16, 128).transpose(2, 0, 1))
    shared = {
        "w_ada": inputs["w_ada"][0], "b_ada": inputs["b_ada"][0].reshape(1, -1), "norm_w": inputs["norm_w"][0].reshape(1, -1),
        "w_in": inputs["w_in"][0], "cw": cw, "alog": np.ascontiguousarray(np.broadcast_to(inputs["a_log"][0], (128, 4))),
        "dtb": np.ascontiguousarray(np.broadcast_to(inputs["dt_bias"][0], (128, 4))), "anw": inputs["a_norm_w"][0].reshape(128, 1),
        "imask": imask, "bm5": bm5, "w_out": inputs["w_out"][0], "w_router": inputs["w_router"][0],
        "br": np.ascontiguousarray(np.broadcast_to(inputs["b_router"][0], (128, NE))), "w_up": inputs["w_up"][0], "bu": bu,
        "w_down": inputs["w_down"][0], "b_down": inputs["b_down"][0],
    }
    shared = {k: np.ascontiguousarray(v, dtype=np.float32) for k, v in shared.items()}
    maps = []
    for core in range(8):
        b, j = core // 4, core % 4
        xw = np.zeros((WIN, D), np.float32)
        n_real = NTOK * (j + 1)
        xw[WIN - n_real:] = x[b, :n_real]
        qv = np.zeros((128, 3), np.float32)
        for q in range(3):
            qv[:, q] = 1.0 if (j - 3 + q) >= 0 else 0.0
        km = np.full((128, 1), 0.0 if j >= 1 else NEG, np.float32)
        m = dict(shared)
        m.update({"xw": xw, "qvalid": qv, "kmask": km, "ccol": np.ascontiguousarray(c[b].reshape(8, 128).T, dtype=np.float32)})
        maps.append(m)
    return maps


_NC_CACHE = {}


def kernel(**inputs):
    maps = _host_inputs(inputs)
    if "nc" not in _NC_CACHE:
        _NC_CACHE["nc"] = build()
    nc = _NC_CACHE["nc"]
    res = run_bass_kernel_spmd(nc, maps, core_ids=list(range(8)))
    full = np.empty((2, 8192, D), np.float32)
    for core in range(8):
        b, j = core // 4, core % 4
        full[b, j * NTOK:(j + 1) * NTOK] = res.results[core]["out"]
    return full
```
